# Optimizing a Trainium2 kernel written in Bass

```python
import math
import jax
import jax.numpy as jnp
from jax import lax
import numpy as np

D_MODEL = 1024
BATCH = 16
SEQ = 4096
DEPTH = 4

CTX_LEN = 256
GRID_W = 64
N_MIXERS = 4
N_OCC = tuple((DEPTH - m + N_MIXERS - 1) // N_MIXERS for m in range(N_MIXERS))

HEAD_DIM = 64
ROPE_BASE = 10000.0
NORM_EPS = 1e-6
NEG_INF = -1e30

WIN_HEADS = D_MODEL // HEAD_DIM
WIN_KV_HEADS = WIN_HEADS // 4
WINDOW = 128
WIN_BLOCK = 128
WIN_QKV = (WIN_HEADS + 2 * WIN_KV_HEADS) * HEAD_DIM

DIFF_HEADS = D_MODEL // (2 * HEAD_DIM)
DIFF_Q_BLOCK = 128

POOL_WINDOWS = (2, 4, 8, 16)
POOL_GROUPS = len(POOL_WINDOWS)
POOL_GROUP_DIM = D_MODEL // POOL_GROUPS

RWKV_HEAD = 64
RWKV_HEADS = D_MODEL // RWKV_HEAD
DECAY_LORA = 64
AAA_LORA = 64
GATE_LORA = 128
GN_EPS = 64e-5

N_GROUPS = 4
EXPERTS_PER_GROUP = 8
N_EXPERTS = N_GROUPS * EXPERTS_PER_GROUP
TOP_K_IN_GROUP = 2
EXPERT_FF = D_MODEL // 2
MOE_BLOCK = 256

kernel_name = "hybrid_dit_interleaved_moe"


def rmsnorm(x, g):
    xf = x.astype(jnp.float32)
    y = xf * lax.rsqrt(jnp.mean(xf * xf, axis=-1, keepdims=True) + NORM_EPS)
    return (y * g.astype(jnp.float32)).astype(x.dtype)


def axial_rope_tables(rows, dim):
    row = jnp.repeat(jnp.arange(rows), GRID_W).astype(jnp.float32)
    col = jnp.tile(jnp.arange(GRID_W), rows).astype(jnp.float32)
    nf = dim // 4
    inv = ROPE_BASE ** (-jnp.arange(nf, dtype=jnp.float32) / nf)
    ar, ac = row[:, None] * inv, col[:, None] * inv
    ang = jnp.concatenate([ar, ar, ac, ac], axis=-1)
    return jnp.cos(ang), jnp.sin(ang)


def apply_rope(x, cos, sin):
    shp = x.shape
    x4 = x.reshape(shp[:-1] + (2, 2, shp[-1] // 4))
    rot = jnp.concatenate([-x4[..., 1:, :], x4[..., :1, :]], axis=-2).reshape(shp)
    bshape = (shp[1],) + (1,) * (x.ndim - 3) + (shp[-1],)
    return x * cos.reshape(bshape).astype(x.dtype) + rot * sin.reshape(bshape).astype(x.dtype)


def window_attention(h, hc, cos, sin, w_qkv, sink, w_o, ctx_out):
    B, S, D = h.shape
    G = WIN_HEADS // WIN_KV_HEADS
    nq, nkv = WIN_HEADS * HEAD_DIM, WIN_KV_HEADS * HEAD_DIM

    def proj(t):
        qkv = t @ w_qkv
        lead = t.shape[:2]
        q = qkv[..., :nq].reshape(lead + (WIN_KV_HEADS, G, HEAD_DIM)) * HEAD_DIM ** -0.5
        k = qkv[..., nq:nq + nkv].reshape(lead + (WIN_KV_HEADS, HEAD_DIM))
        v = qkv[..., nq + nkv:].reshape(lead + (WIN_KV_HEADS, HEAD_DIM))
        return q, k, v

    q, k, v = proj(h)
    qc, kc, vc = proj(hc)
    q, k = apply_rope(q, cos, sin), apply_rope(k, cos, sin)
    sink_f = sink.astype(jnp.float32).reshape(1, WIN_KV_HEADS, G, 1, 1)

    def sink_softmax(parts):
        s = jnp.concatenate(parts + [jnp.broadcast_to(sink_f, parts[0].shape[:-1] + (1,))], axis=-1)
        return jax.nn.softmax(s, axis=-1)

    pad = ((0, 0), (WIN_BLOCK, WIN_BLOCK), (0, 0), (0, 0))
    kp, vp = jnp.pad(k, pad), jnp.pad(v, pad)
    nb = S // WIN_BLOCK
    span = 3 * WIN_BLOCK
    rel = jnp.arange(span)[None, :] - WIN_BLOCK - jnp.arange(WIN_BLOCK)[:, None]
    in_window = jnp.abs(rel) <= WINDOW
    qb = jnp.moveaxis(q.reshape((B, nb, WIN_BLOCK) + q.shape[2:]), 1, 0)

    def block(args):
        n, qblk = args
        start = n * WIN_BLOCK
        kblk = lax.dynamic_slice_in_dim(kp, start, span, axis=1)
        vblk = lax.dynamic_slice_in_dim(vp, start, span, axis=1)
        key_pos = start - WIN_BLOCK + jnp.arange(span)
        mask = in_window & ((key_pos >= 0) & (key_pos < S))[None, :]
        s_loc = jnp.einsum('bqhgd,bkhd->bhgqk', qblk, kblk, preferred_element_type=jnp.float32)
        s_loc = jnp.where(mask, s_loc, NEG_INF)
        s_ctx = jnp.einsum('bqhgd,bchd->bhgqc', qblk, kc, preferred_element_type=jnp.float32)
        p = sink_softmax([s_loc, s_ctx]).astype(v.dtype)
        return (jnp.einsum('bhgqk,bkhd->bqhgd', p[..., :span], vblk)
                + jnp.einsum('bhgqc,bchd->bqhgd', p[..., span:-1], vc))

    o = lax.map(block, (jnp.arange(nb), qb))
    y = jnp.moveaxis(o, 0, 1).reshape(B, S, D) @ w_o
    yc = None
    if ctx_out:
        s = jnp.einsum('bqhgd,bkhd->bhgqk', qc, kc, preferred_element_type=jnp.float32)
        pc = sink_softmax([s]).astype(v.dtype)
        yc = jnp.einsum('bhgqk,bkhd->bqhgd', pc[..., :-1], vc).reshape(hc.shape) @ w_o
    return y, yc


def diff_attention(h, hc, cos, sin, w_qkv, lam, subln_g, w_o, lambda_init, ctx_out):
    B, S, D = h.shape
    dv = 2 * HEAD_DIM

    def proj(t):
        qkv = t @ w_qkv
        lead = t.shape[:2]
        q = qkv[..., :D].reshape(lead + (DIFF_HEADS, 2, HEAD_DIM)) * HEAD_DIM ** -0.5
        k = qkv[..., D:2 * D].reshape(lead + (DIFF_HEADS, 2, HEAD_DIM))
        v = qkv[..., 2 * D:].reshape(lead + (DIFF_HEADS, dv))
        return q, k, v

    q, k, v = proj(h)
    qc, kc, vc = proj(hc)
    q, k = apply_rope(q, cos, sin), apply_rope(k, cos, sin)
    lf = lam.astype(jnp.float32)
    lam_full = jnp.exp(jnp.sum(lf[0] * lf[1])) - jnp.exp(jnp.sum(lf[2] * lf[3])) + lambda_init

    def attend(qblk, keys, vals):
        s = jnp.einsum('bqhmd,bkhmd->bhmqk', qblk, keys, preferred_element_type=jnp.float32)
        p = jax.nn.softmax(s, axis=-1)
        a = (p[:, :, 0] - lam_full * p[:, :, 1]).astype(vals.dtype)
        return jnp.einsum('bhqk,bkhd->bqhd', a, vals)

    def post(o):
        o = rmsnorm(o, subln_g) * (1.0 - lambda_init)
        return o.reshape(o.shape[:2] + (D,)) @ w_o

    k_all = jnp.concatenate([k, kc], axis=1)
    v_all = jnp.concatenate([v, vc], axis=1)
    nb = S // DIFF_Q_BLOCK
    qb = jnp.moveaxis(q.reshape((B, nb, DIFF_Q_BLOCK) + q.shape[2:]), 1, 0)
    o = lax.map(lambda qblk: attend(qblk, k_all, v_all), qb)
    y = post(jnp.moveaxis(o, 0, 1).reshape(B, S, DIFF_HEADS, dv))
    yc = post(attend(qc, kc, vc)) if ctx_out else None
    return y, yc


def multiscale_pool(t, w_group, b_group, layer_scale):
    B, L, D = t.shape
    tf = t.astype(jnp.float32)
    cs = jnp.concatenate([jnp.zeros((B, 1, D), jnp.float32), jnp.cumsum(tf, axis=1)], axis=1)
    pos = jnp.arange(L)
    means = []
    for gi, w in enumerate(POOL_WINDOWS):
        lo = jnp.clip(pos - w // 2, 0, L)
        hi = jnp.clip(pos + w - w // 2, 0, L)
        csg = cs[..., gi * POOL_GROUP_DIM:(gi + 1) * POOL_GROUP_DIM]
        means.append((csg[:, hi] - csg[:, lo]) / (hi - lo).astype(jnp.float32)[None, :, None])
    y = (jnp.concatenate(means, axis=-1) - tf).astype(t.dtype).reshape(B, L, POOL_GROUPS, POOL_GROUP_DIM)
    y = jnp.einsum('blgc,gcd->blgd', y, w_group).reshape(B, L, D) + b_group
    return y * layer_scale


def rwkv_features(t, p, with_out):
    mu, w_rkv, w0, w_a1, w_a2, a0, a_a1, a_a2, g1, g2, k_k, k_a = p
    B, L, D = t.shape
    prev = jnp.pad(t, ((0, 0), (1, 0), (0, 0)))[:, :-1]
    nxt = jnp.pad(t, ((0, 0), (0, 1), (0, 0)))[:, 1:]
    dp, dn = prev - t, nxt - t

    def mix(i):
        return t + dp * mu[0, i] + dn * mu[1, i]

    def heads(u):
        return u.astype(jnp.float32).reshape(B, L, RWKV_HEADS, RWKV_HEAD)

    k = (mix(2) @ w_rkv[1]).astype(jnp.float32)
    v = heads(mix(3) @ w_rkv[2])
    kk = heads(k * k_k.astype(jnp.float32))
    kk = kk / jnp.maximum(jnp.sqrt(jnp.sum(kk * kk, axis=-1, keepdims=True)), 1e-12)
    xw, xa = mix(1), mix(4)
    dirs = []
    for d in range(2):
        w = -jax.nn.softplus(-(w0[d] + jnp.tanh(xw @ w_a1[d]) @ w_a2[d]).astype(jnp.float32)) - 0.5
        a = jax.nn.sigmoid((a0[d] + (xa @ a_a1[d]) @ a_a2[d]).astype(jnp.float32))
        kd = k * (1.0 + (a - 1.0) * k_a.astype(jnp.float32))
        dirs.append((heads(jnp.exp(-jnp.exp(w))), heads(a), heads(kd)))
    r = heads(mix(0) @ w_rkv[0]) if with_out else None
    g = (jax.nn.sigmoid(mix(5) @ g1) @ g2).astype(jnp.float32) if with_out else None
    return r, v, kk, g, dirs


def rwkv_scan(state0, decay, k, v, kk, a, r, reverse):
    emit = r is not None
    seq = (decay, k, v, kk, a) + ((r,) if emit else ())
    xs = tuple(jnp.moveaxis(u, 1, 0) for u in seq)

    def step(S, inp):
        w_t, k_t, v_t, kk_t, a_t = inp[:5]
        sa = jnp.einsum('bhvk,bhk->bhv', S, kk_t)
        S = (S * w_t[:, :, None, :] - sa[..., None] * (kk_t * a_t)[:, :, None, :]
             + v_t[..., None] * k_t[:, :, None, :])
        return S, (jnp.einsum('bhvk,bhk->bhv', S, inp[5]) if emit else None)

    S_fin, out = lax.scan(step, state0, xs, reverse=reverse)
    return S_fin, (jnp.moveaxis(out, 0, 1) if emit else None)


def rwkv_output(o, r, v, kds, g, r_k, ln_w, ln_b, w_o, dtype):
    B, L = o.shape[:2]
    mean = jnp.mean(o, axis=-1, keepdims=True)
    var = jnp.mean(jnp.square(o - mean), axis=-1, keepdims=True)
    on = ((o - mean) * lax.rsqrt(var + GN_EPS)).reshape(B, L, D_MODEL) * ln_w.astype(jnp.float32) + ln_b.astype(jnp.float32)
    rk = r_k.astype(jnp.float32)
    bonus = (jnp.sum(r * kds[0] * rk, axis=-1, keepdims=True) * v
             + jnp.sum(r * kds[1] * rk, axis=-1, keepdims=True) * v).reshape(B, L, D_MODEL)
    return ((on + bonus) * g).astype(dtype) @ w_o


def rwkv_mix(h, hc, feat_p, out_p, ctx_out):
    r_k, ln_w, ln_b, w_o = out_p
    r, v, kk, g, dirs = rwkv_features(h, feat_p, True)
    rc, vc, kkc, gc, dirs_c = rwkv_features(hc, feat_p, ctx_out)
    B = h.shape[0]
    outs, outs_c = [], []
    for d, rev in enumerate((False, True)):
        S0 = jnp.zeros((B, RWKV_HEADS, RWKV_HEAD, RWKV_HEAD), jnp.float32)
        dec_c, a_c, k_c = dirs_c[d]
        S_c, o_c = rwkv_scan(S0, dec_c, k_c, vc, kkc, a_c, rc, rev)
        dec, a, kd = dirs[d]
        _, o_l = rwkv_scan(S_c, dec, kd, v, kk, a, r, rev)
        outs.append(o_l)
        outs_c.append(o_c)
    y = rwkv_output(outs[0] + outs[1], r, v, [dd[2] for dd in dirs], g, r_k, ln_w, ln_b, w_o, h.dtype)
    yc = None
    if ctx_out:
        yc = rwkv_output(outs_c[0] + outs_c[1], rc, vc, [dd[2] for dd in dirs_c], gc, r_k, ln_w, ln_b, w_o, hc.dtype)
    return y, yc


def expert_dispatch(t, eid, wts, w_up, w_down):
    T, D = t.shape
    A = T * TOP_K_IN_GROUP
    flat_e = eid.reshape(A)
    flat_tok = jnp.arange(A, dtype=jnp.int32) // TOP_K_IN_GROUP
    flat_w = wts.reshape(A)
    order = jnp.argsort(flat_e)
    se, stok, sw = flat_e[order], flat_tok[order], flat_w[order]
    counts = jnp.zeros((N_EXPERTS,), jnp.int32).at[flat_e].add(1)
    start = jnp.cumsum(counts) - counts
    nblk = (counts + MOE_BLOCK - 1) // MOE_BLOCK
    blk_end = jnp.cumsum(nblk)
    blk_start = blk_end - nblk
    dest = blk_start[se] * MOE_BLOCK + (jnp.arange(A, dtype=jnp.int32) - start[se])
    NB = -(-A // MOE_BLOCK) + N_EXPERTS
    src = jnp.full((NB * MOE_BLOCK,), T, jnp.int32).at[dest].set(stok)
    xb = jnp.concatenate([t, jnp.zeros((1, D), t.dtype)], axis=0)[src].reshape(NB, MOE_BLOCK, D)
    blk_expert = jnp.minimum(jnp.searchsorted(blk_end, jnp.arange(NB), side='right'), N_EXPERTS - 1)

    def run(args):
        xblk, e = args
        u = xblk @ w_up[e]
        return (jax.nn.silu(u[:, :EXPERT_FF]) * u[:, EXPERT_FF:]) @ w_down[e]

    yb = lax.map(run, (xb, blk_expert)).reshape(NB * MOE_BLOCK, D)
    return jnp.zeros((T, D), t.dtype).at[stok].add(yb[dest] * sw[:, None].astype(t.dtype))


def hier_moe(t, wg, bg, we, be, w_up, w_down):
    T, D = t.shape
    lg = (t @ wg).astype(jnp.float32) + bg.astype(jnp.float32)
    gsel = jnp.argmax(lg, axis=-1)
    p_grp = jnp.max(jax.nn.softmax(lg, axis=-1), axis=-1)
    le = ((t @ we).astype(jnp.float32) + be.astype(jnp.float32)).reshape(T, N_GROUPS, EXPERTS_PER_GROUP)
    le_sel = jnp.einsum('tge,tg->te', le, jax.nn.one_hot(gsel, N_GROUPS, dtype=jnp.float32))
    top_p, top_i = lax.top_k(jax.nn.softmax(le_sel, axis=-1), TOP_K_IN_GROUP)
    wts = p_grp[:, None] * top_p / jnp.sum(top_p, axis=-1, keepdims=True)
    eid = (gsel[:, None] * EXPERTS_PER_GROUP + top_i).astype(jnp.int32)
    return expert_dispatch(t, eid, wts, w_up, w_down)


def setup_inputs(seed: int = 0) -> dict:
    key = jax.random.key(seed)
    keys = iter(jax.random.split(key, 64))
    f32 = jnp.float32

    def nrm(shape, scale):
        return jax.random.normal(next(keys), shape, f32) * scale

    def near_one(shape):
        return 1.0 + nrm(shape, 0.02)

    NA, NBD, NC, ND = N_OCC
    D = D_MODEL
    return {
        "x": nrm((BATCH, SEQ, D), 1.0),
        "c": nrm((BATCH, D), 1.0),
        "ctx": nrm((BATCH, CTX_LEN, D), 1.0),
        "c_ctx": nrm((D,), 1.0),
        "mod_w": nrm((DEPTH, D, 6 * D), 0.5 * D ** -0.5),
        "mod_b": nrm((DEPTH, 6 * D), 0.02),
        "norm_g": near_one((DEPTH, 2, D)),
        "final_g": near_one((D,)),
        "win_w_qkv": nrm((NA, D, WIN_QKV), D ** -0.5),
        "win_sink": nrm((NA, WIN_HEADS), 0.5),
        "win_w_o": nrm((NA, D, D), D ** -0.5),
        "diff_w_qkv": nrm((NBD, D, 3 * D), D ** -0.5),
        "diff_lambda": nrm((NBD, 4, HEAD_DIM), 0.1),
        "diff_subln_g": near_one((NBD, 2 * HEAD_DIM)),
        "diff_w_o": nrm((NBD, D, D), D ** -0.5),
        "pool_w_group": nrm((NC, POOL_GROUPS, POOL_GROUP_DIM, POOL_GROUP_DIM), POOL_GROUP_DIM ** -0.5),
        "pool_b_group": nrm((NC, D), 0.02),
        "pool_scale": near_one((NC, D)),
        "rwkv_mu": jax.random.uniform(next(keys), (ND, 2, 6, D), f32, 0.0, 0.5),
        "rwkv_w_rkv": nrm((ND, 3, D, D), D ** -0.5),
        "rwkv_w0": jax.random.uniform(next(keys), (ND, 2, D), f32, -5.0, -0.5),
        "rwkv_w_a1": nrm((ND, 2, D, DECAY_LORA), D ** -0.5),
        "rwkv_w_a2": nrm((ND, 2, DECAY_LORA, D), 0.1 * DECAY_LORA ** -0.5),
        "rwkv_a0": nrm((ND, 2, D), 0.1),
        "rwkv_a_a1": nrm((ND, 2, D, AAA_LORA), D ** -0.5),
        "rwkv_a_a2": nrm((ND, 2, AAA_LORA, D), 0.1 * AAA_LORA ** -0.5),
        "rwkv_g1": nrm((ND, D, GATE_LORA), D ** -0.5),
        "rwkv_g2": nrm((ND, GATE_LORA, D), GATE_LORA ** -0.5),
        "rwkv_k_k": 0.85 + nrm((ND, D), 0.02),
        "rwkv_k_a": near_one((ND, D)),
        "rwkv_r_k": nrm((ND, RWKV_HEADS, RWKV_HEAD), 0.1),
        "rwkv_ln_w": near_one((ND, D)),
        "rwkv_ln_b": nrm((ND, D), 0.02),
        "rwkv_w_o": nrm((ND, D, D), D ** -0.5),
        "moe_wg": nrm((DEPTH, D, N_GROUPS), D ** -0.5),
        "moe_bg": nrm((DEPTH, N_GROUPS), 0.01),
        "moe_we": nrm((DEPTH, D, N_EXPERTS), D ** -0.5),
        "moe_be": nrm((DEPTH, N_EXPERTS), 0.01),
        "moe_w_up": nrm((DEPTH, N_EXPERTS, D, 2 * EXPERT_FF), D ** -0.5),
        "moe_w_down": nrm((DEPTH, N_EXPERTS, EXPERT_FF, D), EXPERT_FF ** -0.5),
    }


def reference(x, c, ctx, c_ctx, mod_w, mod_b, norm_g, final_g,
              win_w_qkv, win_sink, win_w_o,
              diff_w_qkv, diff_lambda, diff_subln_g, diff_w_o,
              pool_w_group, pool_b_group, pool_scale,
              rwkv_mu, rwkv_w_rkv, rwkv_w0, rwkv_w_a1, rwkv_w_a2, rwkv_a0, rwkv_a_a1, rwkv_a_a2,
              rwkv_g1, rwkv_g2, rwkv_k_k, rwkv_k_a, rwkv_r_k, rwkv_ln_w, rwkv_ln_b, rwkv_w_o,
              moe_wg, moe_bg, moe_we, moe_be, moe_w_up, moe_w_down):
    B, S, D = x.shape
    C = ctx.shape[1]
    ROWS = S // GRID_W
    cos, sin = axial_rope_tables(ROWS, HEAD_DIM)
    xc = ctx
    for i in range(DEPTH):
        m, occ = i % N_MIXERS, i // N_MIXERS
        last = i == DEPTH - 1
        mod = (jax.nn.silu(c) @ mod_w[i] + mod_b[i]).reshape(B, 6, 1, D)
        modc = (jax.nn.silu(c_ctx) @ mod_w[i] + mod_b[i]).reshape(6, D)
        h = rmsnorm(x, norm_g[i, 0]) * (1 + mod[:, 1]) + mod[:, 0]
        need_ctx = (not last) or m != 2
        hc = rmsnorm(xc, norm_g[i, 0]) * (1 + modc[1]) + modc[0] if need_ctx else None
        if m == 0:
            y, yc = window_attention(h, hc, cos, sin, win_w_qkv[occ], win_sink[occ], win_w_o[occ], not last)
        elif m == 1:
            lambda_init = 0.8 - 0.6 * math.exp(-0.3 * i)
            y, yc = diff_attention(h, hc, cos, sin, diff_w_qkv[occ], diff_lambda[occ], diff_subln_g[occ],
                                   diff_w_o[occ], lambda_init, not last)
        elif m == 2:
            y = multiscale_pool(h, pool_w_group[occ], pool_b_group[occ], pool_scale[occ])
            yc = None if last else multiscale_pool(hc, pool_w_group[occ], pool_b_group[occ], pool_scale[occ])
        else:
            feat_p = (rwkv_mu[occ], rwkv_w_rkv[occ], rwkv_w0[occ], rwkv_w_a1[occ], rwkv_w_a2[occ],
                      rwkv_a0[occ], rwkv_a_a1[occ], rwkv_a_a2[occ], rwkv_g1[occ], rwkv_g2[occ],
                      rwkv_k_k[occ], rwkv_k_a[occ])
            out_p = (rwkv_r_k[occ], rwkv_ln_w[occ], rwkv_ln_b[occ], rwkv_w_o[occ])
            y, yc = rwkv_mix(h, hc, feat_p, out_p, not last)
        x = x + mod[:, 2] * y
        h = rmsnorm(x, norm_g[i, 1]) * (1 + mod[:, 4]) + mod[:, 3]
        moe_p = (moe_wg[i], moe_bg[i], moe_we[i], moe_be[i], moe_w_up[i], moe_w_down[i])
        if last:
            x = x + mod[:, 5] * hier_moe(h.reshape(B * S, D), *moe_p).reshape(B, S, D)
        else:
            xc = xc + modc[2] * yc
            hc = rmsnorm(xc, norm_g[i, 1]) * (1 + modc[4]) + modc[3]
            out = hier_moe(jnp.concatenate([h.reshape(B * S, D), hc.reshape(B * C, D)], axis=0), *moe_p)
            x = x + mod[:, 5] * out[:B * S].reshape(B, S, D)
            xc = xc + modc[5] * out[B * S:].reshape(B, C, D)
    return rmsnorm(x, final_g)
```

```python
import math
import os
import numpy as np
import ml_dtypes
import concourse.bass as bass
import concourse.mybir as mybir
from concourse.bass_utils import run_bass_kernel_spmd
from contextlib import ExitStack, contextmanager

F32 = mybir.dt.float32
BF16 = mybir.dt.bfloat16
ALU = mybir.AluOpType
AF = mybir.ActivationFunctionType
AX = mybir.AxisListType

D = 1024
HD = 64
NEXP = 32
EFF = 512
NEG = -30000.0
NORM_EPS = 1e-6


class Rec:
    __slots__ = ("w", "r", "dsem")

    def __init__(self):
        self.w = None
        self.r = {}
        self.dsem = None


class T:
    def __init__(self, k, t, name, is_dram=False):
        self.k = k
        self.t = t
        self.name = name
        self.is_dram = is_dram
        self.is_psum = False
        self.recs = {None: Rec()}

    def key(self, *keys):
        return TK(self, tuple(keys))

    def __getitem__(self, sl):
        return V(self.t[sl], self, None)


class TK:
    def __init__(self, tile, keys):
        self.tile = tile
        self.keys = keys

    def __getitem__(self, sl):
        return V(self.tile.t[sl], self.tile, self.keys)


class V:
    def __init__(self, ap, tile, keys):
        self.ap = ap
        self.tile = tile
        self.keys = keys

    def w(self, fn):
        return V(fn(self.ap), self.tile, self.keys)

    def rearrange(self, s, **kw):
        return V(self.ap.rearrange(s, **kw), self.tile, self.keys)

    def __getitem__(self, sl):
        return V(self.ap[sl], self.tile, self.keys)

    def bitcast(self, dt):
        return V(self.ap.bitcast(dt), self.tile, self.keys)


WRITE_KW = ("out", "accum_out")


class K:
    def __init__(self, nc, es, same_engine_sync=True):
        self.nc = nc
        self.es = es
        self.same = same_engine_sync
        self.eng = {"pe": nc.tensor, "act": nc.scalar, "dve": nc.vector, "pool": nc.gpsimd, "sp": nc.sync}
        self.sem = {}
        self.cnt = {}
        self.EPOCH = 30000
        for e in self.eng:
            self.sem[e] = [es.enter_context(nc.semaphore("s_" + e + "_0"))]
            self.cnt[e] = 0
        self.seen = {e: {} for e in self.eng}
        self.ndsem = 0
        self.ninstr = 0
        self.nwait = 0
        self.free_dsems = []
        self.alloc_es = es
        self.phase_tiles = None

    def sb(self, name, shape, dt):
        self.uid = getattr(self, "uid", 0) + 1
        name = "%s_u%d" % (name, self.uid)
        t = T(self, self.alloc_es.enter_context(self.nc.sbuf_tensor(name, list(shape), dt)), name)
        if self.phase_tiles is not None:
            self.phase_tiles.append(t)
        return t

    def ps(self, name, shape, dt=F32):
        self.uid = getattr(self, "uid", 0) + 1
        name = "%s_u%d" % (name, self.uid)
        t = T(self, self.alloc_es.enter_context(self.nc.psum_tensor(name, list(shape), dt)), name)
        t.is_psum = True
        if self.phase_tiles is not None:
            self.phase_tiles.append(t)
        return t

    def dram(self, name, shape, dt, kind="Internal"):
        return T(self, self.nc.dram_tensor(name, list(shape), dt, kind=kind).ap(), name, is_dram=True)

    @contextmanager
    def phase(self, name=""):
        old_es, old_tiles = self.alloc_es, self.phase_tiles
        with ExitStack() as pes:
            self.alloc_es = pes
            self.phase_tiles = []
            yield
            self.barrier()
            for t in self.phase_tiles:
                for r in t.recs.values():
                    if r.dsem is not None:
                        if self.cnt[r.dsem] < 40000:
                            self.free_dsems.append(r.dsem)
                        r.dsem = None
        self.alloc_es, self.phase_tiles = old_es, old_tiles

    def barrier(self):
        for e in self.eng:
            needs = {sk: v for sk, v in self.cnt.items() if v > 0}
            self._emit_waits(e, needs, force_same=False)

    def _recs_for(self, v):
        tile, keys = v.tile, v.keys
        if keys is None:
            return list(tile.recs.values())
        out = [tile.recs[None]]
        for kk in keys:
            if kk not in tile.recs:
                tile.recs[kk] = Rec()
            out.append(tile.recs[kk])
        return out

    def _own(self, v):
        tile, keys = v.tile, v.keys
        if keys is None:
            return [tile.recs[None]]
        out = []
        for kk in keys:
            if kk not in tile.recs:
                tile.recs[kk] = Rec()
            out.append(tile.recs[kk])
        return out

    @staticmethod
    def _need(needs, semkey, val):
        if val > needs.get(semkey, 0):
            needs[semkey] = val

    def _collect(self, reads, writes):
        needs = {}
        for v in reads:
            for r in self._recs_for(v):
                if r.w is not None:
                    self._need(needs, *r.w)
                if v.tile.is_psum:
                    for sk, val in r.r.items():
                        self._need(needs, sk, val)
        for v in writes:
            for r in self._recs_for(v):
                if r.w is not None:
                    self._need(needs, *r.w)
                for sk, val in r.r.items():
                    self._need(needs, sk, val)
        return needs

    def _emit_waits(self, e, needs, force_same=None):
        eng = self.eng[e]
        seen = self.seen[e]
        for sk, val in needs.items():
            if sk == e and (e == "pe" or not self.same or force_same is False):
                continue
            if seen.get(sk, 0) >= val:
                continue
            if sk in self.eng:
                p = (val - 1) // self.EPOCH
                eng.wait_ge(self.sem[sk][p], val - p * self.EPOCH)
            else:
                eng.wait_ge(self.sem[sk], val)
            self.nwait += 1
            seen[sk] = val

    def _commit(self, reads, writes, semkey, val):
        for v in writes:
            for r in self._own(v):
                r.w = (semkey, val)
                r.r = {}
            if v.keys is None:
                for kk, rr in v.tile.recs.items():
                    if kk is not None:
                        rr.w = (semkey, val)
                        rr.r = {}
        for v in reads:
            for r in self._own(v):
                if val > r.r.get(semkey, 0):
                    r.r[semkey] = val

    def op(self, e, method, *args, extra_reads=(), extra_writes=(), **kw):
        reads = list(extra_reads)
        writes = list(extra_writes)
        real = {}
        for name, a in kw.items():
            if isinstance(a, V):
                (writes if name in WRITE_KW else reads).append(a)
                real[name] = a.ap
            else:
                real[name] = a
        needs = self._collect(reads, writes)
        self._emit_waits(e, needs)
        ins = getattr(self.eng[e], method)(*args, **real)
        self.cnt[e] += 1
        p = (self.cnt[e] - 1) // self.EPOCH
        if p >= len(self.sem[e]):
            self.sem[e].append(self.es.enter_context(self.nc.semaphore("s_%s_%d" % (e, p))))
        ins.then_inc(self.sem[e][p], 1)
        self.ninstr += 1
        self._commit(reads, writes, e, self.cnt[e])
        return ins

    def _dsem_for(self, v):
        r = self._own(v)[0]
        if r.dsem is not None and self.cnt[r.dsem] > 50000:
            r.dsem = None
        if r.dsem is None:
            if self.free_dsems:
                r.dsem = self.free_dsems.pop()
            else:
                name = "d%d" % self.ndsem
                self.ndsem += 1
                self.sem[name] = self.es.enter_context(self.nc.semaphore(name))
                self.cnt[name] = 0
                r.dsem = name
        return r.dsem

    def dma(self, q, out, in_, **kw):
        reads = [in_]
        writes = [out]
        needs = self._collect(reads, writes)
        self._emit_waits(q, needs)
        owner = out if not out.tile.is_dram else (in_ if not in_.tile.is_dram else out)
        sk = self._dsem_for(owner)
        ins = self.eng[q].dma_start(out=out.ap, in_=in_.ap, **kw)
        self.cnt[sk] += 16
        ins.then_inc(self.sem[sk], 16)
        self.ninstr += 1
        self._commit(reads, writes, sk, self.cnt[sk])
        return ins

    def wait_all(self, e, views):
        needs = self._collect(views, [])
        self._emit_waits(e, needs)

    def mm(self, out, lhsT, rhs, start=True, stop=True, skip=False):
        if skip:
            return self.op("pe", "matmul", out=out, lhsT=lhsT, rhs=rhs, start=start, stop=stop, skip_group_check=True)
        return self.op("pe", "matmul", out=out, lhsT=lhsT, rhs=rhs, start=start, stop=stop)

    def tr(self, out, in_, ident):
        return self.op("pe", "transpose", out=out, in_=in_, identity=ident)

    def act(self, out, in_, func, **kw):
        return self.op("act", "activation", out=out, in_=in_, func=func, **kw)

    def tt(self, e, out, in0, in1, op):
        return self.op(e, "tensor_tensor", out=out, in0=in0, in1=in1, op=op)

    def ts(self, e, out, in0, s1, op0, s2=None, op1=None, **kw):
        if op1 is None:
            return self.op(e, "tensor_scalar", out=out, in0=in0, scalar1=s1, scalar2=None, op0=op0, **kw)
        return self.op(e, "tensor_scalar", out=out, in0=in0, scalar1=s1, scalar2=s2, op0=op0, op1=op1, **kw)

    def stt(self, e, out, in0, scalar, in1, op0, op1):
        return self.op(e, "scalar_tensor_tensor", out=out, in0=in0, scalar=scalar, in1=in1, op0=op0, op1=op1)

    def copy(self, e, out, in_):
        if e == "act":
            return self.act(out, in_, AF.Copy)
        return self.op(e, "tensor_copy", out=out, in_=in_)

    def memset(self, e, out, val):
        return self.op(e, "memset", out.ap, val, extra_writes=[out])


def rope_tables(S):
    GRID_W = 64
    rows = S // GRID_W
    row = np.repeat(np.arange(rows), GRID_W).astype(np.float32)
    col = np.tile(np.arange(GRID_W), rows).astype(np.float32)
    nf = HD // 4
    inv = (10000.0 ** (-np.arange(nf, dtype=np.float32) / nf)).astype(np.float32)
    ar, ac = row[:, None] * inv, col[:, None] * inv
    ang = np.concatenate([ar, ar, ac, ac], axis=-1).astype(np.float32)
    cos, sin = np.cos(ang).astype(np.float32), np.sin(ang).astype(np.float32)
    sgn = np.ones((64,), np.float32)
    sgn[0:16] = -1.0
    sgn[32:48] = -1.0
    return cos, (sin * sgn).astype(np.float32)


def make_consts(S):
    c = {}
    cos, ssin = rope_tables(S)
    c["rope_cos"] = cos
    c["rope_sin"] = ssin
    c["ident"] = np.eye(128, dtype=np.float32)
    kp = np.arange(128)[:, None]
    qp = np.arange(128)[None, :]
    mP = np.where(qp <= kp, 0.0, NEG).astype(np.float32)
    mN = np.where(kp <= qp, 0.0, NEG).astype(np.float32)
    c["maskP"] = np.tile(mP, (1, 4))
    c["maskN"] = np.tile(mN, (1, 4))
    c["pool_band"] = pool_band_consts()
    c.update(rwkv_consts())
    return c


class Prog:
    def __init__(self, NB, S, C, layers, depth_total=4):
        self.NB, self.S, self.C = NB, S, C
        self.TS, self.TC = S // 128, C // 128
        self.TB = self.TS + self.TC
        self.NT = NB * self.TB
        self.layers = layers
        self.lmap = {l: i for i, l in enumerate(layers)}
        self.depth_total = depth_total

    def cond_of(self, tt):
        b, j = divmod(tt, self.TB)
        return b if j < self.TS else 2

    def is_lat(self, tt):
        return (tt % self.TB) < self.TS

    def build(self, weight_shapes, const_shapes):
        nc = bass.Bass("TRN2", target_bir_lowering=False)
        self.nc = nc
        es = ExitStack()
        with es:
            k = K(nc, es)
            self.k = k
            NT = self.NT
            self.xin = k.dram("xin", [NT * 128, D], F32, kind="ExternalInput")
            self.cc = k.dram("cc", [3, D], F32, kind="ExternalInput")
            self.W = {n: k.dram(n, list(s), F32, kind="ExternalInput") for n, s in weight_shapes.items()}
            self.Cn = {n: k.dram(n, list(s), F32, kind="ExternalInput") for n, s in const_shapes.items()}
            self.y = k.dram("y", [self.NB * self.S, D], F32, kind="ExternalOutput")
            self.xs = k.dram("xs", [NT * 128, D], F32)
            self.attn_d = k.dram("attn_d", [NT * 128, D], BF16)
            self.modrows = k.dram("modrows", [self.depth_total, 3, 6 * D], F32)
            self.kstop = os.environ.get("KSTOP", "")
            self.mod_prep()
            src = self.xin
            for li, l in enumerate(self.layers):
                if self.kstop == "mod":
                    break
                m = l % 4
                last = l == self.depth_total - 1
                final = li == len(self.layers) - 1
                if m == 0:
                    self.win_attention(l, src, last)
                elif m == 1:
                    self.diff_attention(l, src, last)
                elif m == 2:
                    self.pool_mixer(l, src, last)
                else:
                    self.rwkv_features(l, src)
                    if self.kstop in ("rwA", "rwB"):
                        break
                    self.rwkv_scan(l, src)
                    if self.kstop in ("rwC", "rwD"):
                        break
                if self.kstop in ("p1", "p2"):
                    break
                self.post_and_moe(l, src, last, final, pool=(m == 2))
                src = self.xs
            self.dbg = []
            if os.environ.get("KDBG", ""):
                dl = [("attn_d", self.attn_d, [NT * 128, D], BF16), ("xs", self.xs, [NT * 128, D], F32),
                      ("modrows", self.modrows, [self.depth_total * 3, 6 * D], F32)]
                if hasattr(self, "o_d"):
                    dl += [("rw_v", self.v_d, [NT * 128, D], BF16), ("rw_g", self.g_d, [NT * 128, D], BF16),
                           ("rw_bs", self.bs_d, [NT * 128, 16], F32), ("rw_o0", self.o_d, [NT * 128, D], F32), ("rw_o1", self.o_d, [NT * 128, D], F32)]
                if hasattr(self, "o_d"):
                    dl += [("rw_F", self.F_d, [self.NB * 2 * self.TB * 128, 6 * 1024], BF16), ("rw_Wc", self.Wc_d, [self.NB * 2 * self.TB * 128, 8], F32)]
                for nm, t, shp, dt in dl:
                    o = k.dram("dbg_" + nm, shp, dt, kind="ExternalOutput")
                    if nm == "modrows":
                        src_v = t[:, :, :].rearrange("a b c -> (a b) c")
                    elif nm in ("rw_F", "rw_Wc"):
                        src_v = t[:, :, :, :, :].rearrange("a b c p f -> (a b c p) f")
                    elif nm in ("rw_o0", "rw_o1"):
                        src_v = t[int(nm[-1]), :, :]
                    else:
                        src_v = t[:, :]
                    k.dma("sp", o[:, :], src_v)
                    self.dbg.append(("dbg_" + nm, o))
                k.wait_all("sp", [o[:, :] for _, o in self.dbg])
            k.wait_all("sp", [self.y[:, :]])
            print("build: ninstr", k.ninstr, "nwait", k.nwait, "ndsem", k.ndsem)
        return nc

    def mod_prep(self):
        k = self.k
        with k.phase("modprep"):
            csT = k.sb("csT", [128, 8, 3], F32)
            for c_ in range(3):
                k.dma("sp", csT[:, :, c_:c_ + 1], self.cc[c_:c_ + 1, :].rearrange("c (k p) -> p k c", p=128), allow_slow_non_contiguous=True)
            k.act(csT[:], csT[:], AF.Silu)
            wbuf = [k.sb("modw%d" % i, [128, 8, 512], F32) for i in range(2)]
            bb = [k.sb("modb%d" % i, [3, 512], F32) for i in range(2)]
            ob = [k.sb("modo%d" % i, [3, 512], F32) for i in range(2)]
            ps = [k.ps("modps%d" % i, [128, 512], F32) for i in range(2)]
            it = 0
            for l in self.layers:
                for n in range(12):
                    i = it % 2
                    it += 1
                    k.dma("sp", wbuf[i][:], self.W["mod_w"][self.lmap[l], :, n * 512:(n + 1) * 512].rearrange("(k p) n -> p k n", p=128))
                    k.dma("sp", bb[i][:], self.W["mod_b"][self.lmap[l]:self.lmap[l] + 1, n * 512:(n + 1) * 512].w(lambda a: a.partition_broadcast(3)))
                    for kc in range(8):
                        k.mm(ps[i][0:3, :], csT[:, kc, :], wbuf[i][:, kc, :], start=(kc == 0), stop=(kc == 7))
                    k.tt("dve", ob[i][:], ps[i][0:3, :], bb[i][:], ALU.add)
                    k.dma("sp", self.modrows[l, :, n * 512:(n + 1) * 512], ob[i][:])

    def alloc_mod(self, l, sub, norm=True, gate=True):
        k = self.k
        st = {"cond": None, "l": l, "sub": sub}
        if norm:
            st["Gp"] = k.sb("modGp%d" % sub, [128, D], F32)
            st["sh"] = k.sb("modsh%d" % sub, [128, D], F32)
            st["g"] = k.sb("modg%d" % sub, [128, D], F32)
            k.dma("sp", st["g"][:], self.W["norm_g"][self.lmap[l], sub:sub + 1, :].w(lambda a: a.partition_broadcast(128)))
        if gate:
            st["gate"] = k.sb("modgt%d" % sub, [128, D], F32)
        return st

    def set_cond(self, st, cond, need_gate=True, need_norm=True):
        k = self.k
        l, sub = st["l"], st["sub"]
        need_norm = need_norm and st.get("cond_n") != cond
        need_gate = need_gate and st.get("cond_g") != cond
        if need_norm:
            st["cond_n"] = cond
        if need_gate:
            st["cond_g"] = cond
        base = sub * 3 * D

        def row(i):
            return self.modrows[l, cond:cond + 1, base + i * D: base + (i + 1) * D].w(lambda a: a.partition_broadcast(128))
        if need_norm:
            k.dma("sp", st["sh"][:], row(0))
            k.dma("sp", st["Gp"][:], row(1))
            k.stt("dve", st["Gp"][:], st["Gp"][:], 1.0, st["g"][:], ALU.add, ALU.mult)
        if need_gate:
            k.dma("sp", st["gate"][:], row(2))

    def norm_mod(self, xt, Gp, sh, out, scr, ss, eng2="pool"):
        k = self.k
        k.act(scr, xt, AF.Square, accum_out=ss)
        k.ts("dve", ss, ss, 1.0 / D, ALU.mult, NORM_EPS, ALU.add)
        k.act(ss, ss, AF.Sqrt)
        k.op("dve", "reciprocal", out=ss, in_=ss)
        if sh is None:
            k.stt("dve", out, xt, ss, Gp, ALU.mult, ALU.mult)
        else:
            k.stt("dve", scr, xt, ss, Gp, ALU.mult, ALU.mult)
            k.tt(eng2, out, scr, sh, ALU.add)

    def load_w_bf16(self, name, src_v, kchunks, n):
        k = self.k
        t = k.sb(name, [128, kchunks, n], BF16)
        for kc in range(kchunks):
            k.dma("pool", t[:, kc, :], src_v[kc * 128:(kc + 1) * 128, :])
        return t

    def transpose_tile(self, src, dst, ps, ident, nblk=8, eng="act"):
        k = self.k
        for kc in range(nblk):
            k.tr(ps[:, kc * 128:(kc + 1) * 128], src[:, kc * 128:(kc + 1) * 128], ident)
        k.copy(eng, dst, ps)

    def rope(self, ps_v, nh, cos, sin, scr_v, out_v):
        k = self.k
        X3 = ps_v.rearrange("p (h d) -> p h d", d=64)
        S3 = scr_v.rearrange("p (h d) -> p h d", d=64)
        cb = cos.w(lambda a: a.unsqueeze(1).to_broadcast([128, nh, 64]))
        k.tt("dve", S3, X3, cb, ALU.mult)
        X5 = ps_v.rearrange("p (h a r i) -> p h a r i", a=2, r=2, i=16)
        s4 = sin.rearrange("p (a r i) -> p a r i", a=2, r=2, i=16)
        O5 = out_v.rearrange("p h (a r i) -> p h a r i", a=2, r=2, i=16)
        for r in range(2):
            sb_ = s4[:, :, r, :].w(lambda a: a.unsqueeze(1).to_broadcast([128, nh, 2, 16]))
            k.tt("dve", O5[:, :, :, r, :], X5[:, :, :, 1 - r, :], sb_, ALU.mult)
        k.tt("pool", out_v, out_v, S3, ALU.add)

    def win_attention(self, l, src, last):
        k = self.k
        NB, TS, TC, TB, NT = self.NB, self.TS, self.TC, self.TB, self.NT
        W_ = TB * 128
        G = 4
        qT_d = k.dram("win_qT", [NB, 16, 65, W_], BF16)
        kT_d = k.dram("win_kT", [NB, 4, 65, W_], BF16)
        with k.phase("win"):
            v_all = k.sb("v_all", [128, NT, 4, 65], BF16)
            qn_all = k.sb("qn_all", [128, NT, 16], F32)
            nk8 = k.sb("nk8", [128, 1], F32)
            ident_bf = k.sb("ident_bf", [128, 128], BF16)
            k.dma("pool", ident_bf[:], self.Cn["ident"][:, :])
            k.memset("pool", v_all[:, :, :, 64:65], 1.0)
            with k.phase("winP1"):
                ident_f = k.sb("ident_f", [128, 128], F32)
                k.dma("sp", ident_f[:], self.Cn["ident"][:, :])
                wqkv = self.load_w_bf16("wqkv", self.W["win_w_qkv"][0], 8, 1536)
                st1 = self.alloc_mod(l, 0, gate=False)
                xt = [k.sb("xt%d" % i, [128, D], F32) for i in range(2)]
                cs = [k.sb("cos%d" % i, [128, 64], F32) for i in range(2)]
                sn = [k.sb("sin%d" % i, [128, 64], F32) for i in range(2)]
                scr = k.sb("scr", [128, D], F32)
                scr2 = k.sb("scr2", [128, D], F32)
                ss = k.sb("ss", [128, 1], F32)
                hb = k.sb("hb", [128, D], BF16)
                hT = k.sb("hT", [128, 8, 128], BF16)
                q_aug = [k.sb("q_aug%d" % i, [128, 16, 65], BF16) for i in range(2)]
                k_aug = [k.sb("k_aug%d" % i, [128, 4, 65], BF16) for i in range(2)]
                for i in range(2):
                    k.memset("pool", k_aug[i][:, :, 64:65], 0.0)
                qn2 = k.sb("qn2", [128, 16], F32)
                qnb = k.sb("qnb", [128, 16], BF16)
                k2 = k.sb("k2", [128, 4], F32)
                kmax2 = k.sb("kmax2", [128, 4], F32)
                k.memset("dve", kmax2[:], 0.0)
                qT_st = [k.sb("qT_st%d" % i, [65, 16, 128], BF16) for i in range(2)]
                kT_st = [k.sb("kT_st%d" % i, [65, 4, 128], BF16) for i in range(2)]
                ps_tr = k.ps("ps_tr", [128, D], BF16)
                ps_q = k.ps("ps_q", [128, D], F32)
                ps_kv = k.ps("ps_kv", [128, 512], F32)
                ps_qT = k.ps("ps_qT", [128, 2048], BF16)
                ps_kT = k.ps("ps_kT", [128, 1024], BF16)
                def p1_load(tt):
                    b, j = divmod(tt, TB)
                    i = tt % 2
                    k.dma("sp", xt[i][:], src.key(tt)[tt * 128:(tt + 1) * 128, :])
                    if j < TS:
                        k.dma("sp", cs[i][:], self.Cn["rope_cos"][j * 128:(j + 1) * 128, :])
                        k.dma("sp", sn[i][:], self.Cn["rope_sin"][j * 128:(j + 1) * 128, :])
                p1_load(0)
                for tt in range(NT):
                    b, j = divmod(tt, TB)
                    lat = j < TS
                    i = tt % 2
                    if tt + 1 < NT:
                        p1_load(tt + 1)
                    self.set_cond(st1, self.cond_of(tt), need_gate=False)
                    self.norm_mod(xt[i][:], st1["Gp"][:], st1["sh"][:], hb[:], scr[:], ss[:])
                    self.transpose_tile(hb[:], hT[:].rearrange("p k t -> p (k t)"), ps_tr[:], ident_bf[:])
                    for n in range(3):
                        o = ps_q[:, n * 512:(n + 1) * 512] if n < 2 else ps_kv[:, :]
                        for kc in range(8):
                            k.mm(o, hT[:, kc, :], wqkv[:, kc, n * 512:(n + 1) * 512], start=(kc == 0), stop=(kc == 7))
                    k.act(scr[:], ps_q[:], AF.Square)
                    k.op("dve", "tensor_reduce", out=qn2[:], in_=scr[:].rearrange("p (h d) -> p h d", d=64), axis=AX.X, op=ALU.add)
                    k.act(qn2[:], qn2[:], AF.Sqrt)
                    k.copy("dve", qnb[:], qn2[:])
                    k.copy("dve", qn_all[:, tt, :], qnb[:])
                    k.ts("dve", q_aug[i][:, :, 64:65], qnb[:].w(lambda a: a.unsqueeze(2)), -1.0, ALU.mult)
                    k.act(scr2[:, 0:256], ps_kv[:, 0:256], AF.Square)
                    k.op("dve", "tensor_reduce", out=k2[:], in_=scr2[:, 0:256].rearrange("p (h d) -> p h d", d=64), axis=AX.X, op=ALU.add)
                    k.tt("dve", kmax2[:], kmax2[:], k2[:], ALU.max)
                    if lat:
                        self.rope(ps_q[:], 16, cs[i][:], sn[i][:], scr[:], q_aug[i][:, :, 0:64])
                        self.rope(ps_kv[:, 0:256], 4, cs[i][:], sn[i][:], scr2[:, 0:256], k_aug[i][:, :, 0:64])
                    else:
                        k.copy("dve", q_aug[i][:, :, 0:64], ps_q[:].rearrange("p (h d) -> p h d", d=64))
                        k.copy("dve", k_aug[i][:, :, 0:64], ps_kv[:, 0:256].rearrange("p (h d) -> p h d", d=64))
                    k.copy("act", v_all[:, tt, :, 0:64], ps_kv[:, 256:512].rearrange("p (h d) -> p h d", d=64))
                    for h in range(16):
                        k.tr(ps_qT[0:65, h * 128:(h + 1) * 128], q_aug[i][:, h, :], ident_bf[:])
                    k.copy("act", qT_st[i][:].rearrange("r h t -> r (h t)"), ps_qT[0:65, :])
                    for g in range(4):
                        k.tr(ps_kT[0:65, g * 128:(g + 1) * 128], k_aug[i][:, g, :], ident_bf[:])
                    k.copy("dve", kT_st[i][:].rearrange("r h t -> r (h t)"), ps_kT[0:65, 0:512])
                    k.dma("sp", qT_d.key(tt)[b, :, :, j * 128:(j + 1) * 128].rearrange("h r t -> r h t"), qT_st[i][:])
                    k.dma("sp", kT_d.key(tt)[b, :, :, j * 128:(j + 1) * 128].rearrange("h r t -> r h t"), kT_st[i][:])
                km1 = k.sb("km1", [128, 1], F32)
                k.op("dve", "tensor_reduce", out=km1[:], in_=kmax2[:], axis=AX.X, op=ALU.max)
                k.tr(ps_kv[0:1, 0:128], km1[:], ident_f[:])
                kmx = k.sb("kmx", [1, 1], F32)
                kmxb = k.sb("kmxb", [1, 1], BF16)
                k.op("dve", "tensor_reduce", out=kmx[:], in_=ps_kv[0:1, 0:128], axis=AX.X, op=ALU.max)
                k.act(kmx[:], kmx[:], AF.Sqrt)
                k.copy("dve", kmxb[:], kmx[:])
                k.copy("dve", kmx[:], kmxb[:])
                ones_f = k.sb("ones_f", [1, 128], F32)
                k.memset("dve", ones_f[:], 1.0)
                k.mm(ps_kv[:, 128:129], ones_f[0:1, :], kmx[0:1, 0:1])
                k.ts("dve", nk8[:], ps_kv[:, 128:129], -0.125, ALU.mult)
                kmrow = k.sb("kmrow", [1, W_], BF16)
                k.memset("pool", kmrow[:], 1.0)
                k.ts("dve", kmrow[:], kmrow[:], kmx[0:1, 0:1], ALU.mult)
                for b in range(NB):
                    for g in range(4):
                        k.dma("sp", kT_d[b, g, 64:65, :], kmrow[:])
            if self.kstop == "p1":
                return
            with k.phase("winP2"):
                maskP = k.sb("maskP", [128, 512], BF16)
                maskN = k.sb("maskN", [128, 512], BF16)
                k.dma("pool", maskP[:], self.Cn["maskP"][:, :])
                k.dma("pool", maskN[:], self.Cn["maskN"][:, :])
                masks = {"P": maskP, "N": maskN}
                sinkb = k.sb("sinkb", [128, 16], F32)
                k.dma("sp", sinkb[:], self.W["win_sink"][0:1, :].w(lambda a: a.partition_broadcast(128)))
                qT_g = [k.sb("qT_g%d" % i, [65, 4, W_], BF16) for i in range(2)]
                kT_g = [k.sb("kT_g%d" % i, [65, W_], BF16) for i in range(2)]
                sk = [k.sb("sk%d" % i, [128, TB, 4], F32) for i in range(2)]
                ET = [k.sb("ET%d" % i, [128, 512], BF16) for i in range(3)]
                den = [k.sb("den%d" % i, [128, 4], F32) for i in range(2)]
                ao = [k.sb("ao%d" % i, [128, 4, 64], BF16) for i in range(2)]
                ps_s = [k.ps("ps_s%d" % i, [128, 512], F32) for i in range(3)]
                ps_o = [k.ps("ps_o%d" % i, [128, 512], F32) for i in range(2)]
                it = 0
                def w2_load(b, g):
                    bi = (b * G + g) % 2
                    k.dma("sp", qT_g[bi][:], qT_d[b, 4 * g:4 * g + 4, :, :].rearrange("h r t -> r h t"))
                    k.dma("sp", kT_g[bi][:], kT_d[b, g, :, :])
                bgs = [(b, g) for b in range(NB) for g in range(G)]
                w2_load(*bgs[0])
                for bgi, (b, g) in enumerate(bgs):
                    if True:
                        bi = (b * G + g) % 2
                        if bgi + 1 < len(bgs):
                            w2_load(*bgs[bgi + 1])
                        k.ts("dve", sk[bi][:], qn_all[:, b * TB:(b + 1) * TB, 4 * g:4 * g + 4], nk8[:, 0:1], ALU.mult)
                        k.tt("dve", sk[bi][:], sk[bi][:], sinkb[:, 4 * g:4 * g + 4].w(lambda a: a.unsqueeze(1).to_broadcast([128, TB, 4])), ALU.add)
                        k.act(sk[bi][:], sk[bi][:], AF.Exp)
                        for n in range(TB):
                            lat = n < TS
                            kbs = []
                            if lat:
                                if n >= 1:
                                    kbs.append((n - 1, "P"))
                                kbs.append((n, None))
                                if n + 1 < TS:
                                    kbs.append((n + 1, "N"))
                            kbs += [(TS + c, None) for c in range(TC)]
                            po = ps_o[n % 2]
                            for idx, (jb, mk) in enumerate(kbs):
                                pss = ps_s[it % 3]
                                et = ET[it % 3]
                                it += 1
                                k.mm(pss[:], kT_g[bi][:, jb * 128:(jb + 1) * 128], qT_g[bi][:, :, n * 128:(n + 1) * 128], start=True, stop=(mk is None))
                                if mk is not None:
                                    k.mm(pss[:], ident_bf[:], masks[mk][:], start=False, stop=True)
                                k.act(et[:], pss[:], AF.Exp, scale=0.125)
                                for h in range(4):
                                    k.mm(po[:, h * 65:(h + 1) * 65], et[:, h * 128:(h + 1) * 128], v_all[:, b * TB + jb, g, :],
                                         start=(idx == 0 and h == 0), stop=(idx == len(kbs) - 1), skip=True)
                            po3 = po[:, 0:260].rearrange("p (h c) -> p h c", c=65)
                            dn = den[n % 2]
                            k.tt("dve", dn[:], po3[:, :, 64:65].rearrange("p h c -> p (h c)"), sk[bi][:, n, :], ALU.add)
                            k.op("dve", "reciprocal", out=dn[:], in_=dn[:])
                            k.tt("dve", ao[n % 2][:], po3[:, :, 0:64], dn[:].w(lambda a: a.unsqueeze(2).to_broadcast([128, 4, 64])), ALU.mult)
                            tt = b * TB + n
                            k.dma("sp", self.attn_d.key(tt)[tt * 128:(tt + 1) * 128, g * 256:(g + 1) * 256], ao[n % 2][:].rearrange("p h d -> p (h d)"))

    def post_and_moe(self, l, src, last, final, pool=False, wo_name=None):
        k = self.k
        NB, TS, TC, TB, NT = self.NB, self.TS, self.TC, self.TB, self.NT
        m = l % 4
        wo_src = {0: "win_w_o", 1: "diff_w_o", 3: "rwkv_w_o"}.get(m)
        tiles = [tt for tt in range(NT) if (not last) or self.is_lat(tt)]
        GSZ = 12
        groups = [tiles[i:i + GSZ] for i in range(0, len(tiles), GSZ)]
        with k.phase("post"):
            ident_bf = k.sb("ident_bf", [128, 128], BF16)
            k.dma("pool", ident_bf[:], self.Cn["ident"][:, :])
            ident_f = k.sb("ident_f", [128, 128], F32)
            k.dma("sp", ident_f[:], self.Cn["ident"][:, :])
            wo = None if pool else self.load_w_bf16("wo", self.W[wo_src][0], 8, D)
            yt_ = [k.sb("yt%d" % i, [128, D], F32) for i in range(2)] if pool else None
            wr = k.sb("wr", [128, 8, 36], F32)
            k.dma("sp", wr[:, :, 0:4], self.W["moe_wg"][self.lmap[l]].rearrange("(k p) n -> p k n", p=128))
            k.dma("sp", wr[:, :, 4:36], self.W["moe_we"][self.lmap[l]].rearrange("(k p) n -> p k n", p=128))
            rb = k.sb("rb", [128, 36], F32)
            k.dma("sp", rb[:, 0:4], self.W["moe_bg"][self.lmap[l]:self.lmap[l] + 1, :].w(lambda a: a.partition_broadcast(128)))
            k.dma("sp", rb[:, 4:36], self.W["moe_be"][self.lmap[l]:self.lmap[l] + 1, :].w(lambda a: a.partition_broadcast(128)))
            st1 = self.alloc_mod(l, 0, norm=False)
            st2 = self.alloc_mod(l, 1)
            if final:
                fg = k.sb("fg", [128, D], F32)
                k.dma("sp", fg[:], self.W["final_g"][:].w(lambda a: a.unsqueeze(0).partition_broadcast(128)))
            at = [k.sb("at%d" % i, [128, D], BF16) for i in range(2)]
            xt = [k.sb("xt%d" % i, [128, D], F32) for i in range(2)]
            aT = k.sb("aT", [128, 8, 128], BF16)
            scr = k.sb("scr", [128, D], F32)
            h2 = k.sb("h2", [128, D], F32)
            ss = k.sb("ss", [128, 1], F32)
            h2T32 = k.sb("h2T32", [128, 8, 128], F32)
            h2T = k.sb("h2T", [128, 8, GSZ * 128], BF16)
            wt = k.sb("wt", [128, GSZ, 32], F32)
            acc = k.sb("acc", [128, GSZ, D], F32)
            lg = k.sb("lg", [128, 36], F32)
            sm = {n: k.sb("r_" + n, [128, w], F32) for n, w in
                  [("gmax", 1), ("ngmax", 1), ("oh", 4), ("eg", 4), ("gsum", 1), ("les", 32), ("lsel", 8), ("emax", 1), ("nemax", 1),
                   ("ee", 8), ("mk1", 8), ("ee2", 8), ("m2", 1), ("mk2", 8), ("wsel", 8), ("fac", 1)]}
            wup = [k.sb("wup%d" % i, [128, 8, D], BF16) for i in range(2)]
            wdn = [k.sb("wdn%d" % i, [128, 4, D], BF16) for i in range(2)]
            sa = [k.sb("sa%d" % i, [128, 512], F32) for i in range(2)]
            actT = [k.sb("actT%d" % i, [128, 4, 512], BF16) for i in range(2)]
            ps_tr = k.ps("ps_tr", [128, D], BF16)
            ps_y = k.ps("ps_y", [128, D], F32)
            ps_ab = [k.ps("ps_ab%d" % i, [128, 512], F32) for i in range(3)]
            ps_d = [k.ps("ps_d%d" % i, [128, 512], F32) for i in range(2)]

            def load_expert(e):
                wu, wd = wup[e % 2], wdn[e % 2]
                for kc in range(8):
                    k.dma("pool", wu[:, kc, :], self.W["moe_w_up"][self.lmap[l], e, kc * 128:(kc + 1) * 128, :])
                for kc in range(4):
                    k.dma("pool", wd[:, kc, :], self.W["moe_w_down"][self.lmap[l], e, kc * 128:(kc + 1) * 128, :])

            for G in groups:
                ng = len(G)
                if self.kstop == "post0":
                    return
                load_expert(0)

                def g_load(ti):
                    tt = G[ti]
                    if pool:
                        k.dma("sp", yt_[ti % 2][:], self.y_d.key(tt)[tt * 128:(tt + 1) * 128, :])
                    else:
                        k.dma("sp", at[ti % 2][:], self.attn_d.key(tt)[tt * 128:(tt + 1) * 128, :])
                    k.dma("sp", xt[ti % 2][:], src.key(tt)[tt * 128:(tt + 1) * 128, :])
                g_load(0)
                for ti, tt in enumerate(G):
                    if ti + 1 < ng:
                        g_load(ti + 1)
                    cond = self.cond_of(tt)
                    self.set_cond(st1, cond, need_norm=False)
                    self.set_cond(st2, cond, need_gate=False)
                    a_ = at[ti % 2]
                    x_ = xt[ti % 2]
                    if pool:
                        k.tt("dve", scr[:], yt_[ti % 2][:], st1["gate"][:], ALU.mult)
                    else:
                        self.transpose_tile(a_[:], aT[:].rearrange("p k t -> p (k t)"), ps_tr[:], ident_bf[:])
                        for n in range(2):
                            for kc in range(8):
                                k.mm(ps_y[:, n * 512:(n + 1) * 512], aT[:, kc, :], wo[:, kc, n * 512:(n + 1) * 512], start=(kc == 0), stop=(kc == 7))
                        k.tt("dve", scr[:], ps_y[:], st1["gate"][:], ALU.mult)
                    k.tt("pool", x_[:], scr[:], x_[:], ALU.add)
                    k.dma("sp", self.xs.key(tt)[tt * 128:(tt + 1) * 128, :], x_[:])
                    if self.kstop == "post1a":
                        return
                    self.norm_mod(x_[:], st2["Gp"][:], st2["sh"][:], h2[:], scr[:], ss[:])
                    if self.kstop == "post1b1":
                        return
                    for kc in range(8):
                        k.tr(ps_y[:, kc * 128:(kc + 1) * 128], h2[:, kc * 128:(kc + 1) * 128], ident_f[:])
                    if self.kstop == "post1b2":
                        return
                    k.copy("act", h2T32[:].rearrange("p k t -> p (k t)"), ps_y[:])
                    if self.kstop == "post1b3":
                        return
                    k.copy("pool", h2T[:, :, ti * 128:(ti + 1) * 128], h2T32[:])
                    if self.kstop == "post1b":
                        return
                    pr = ps_ab[2]
                    for kc in range(8):
                        k.mm(pr[:, 0:36], h2T32[:, kc, :], wr[:, kc, :], start=(kc == 0), stop=(kc == 7))
                    if self.kstop == "post1c":
                        return
                    self.router(pr[:, 0:36], rb, lg, sm, wt[:, ti, :])
                if self.kstop == "post1":
                    return
                ntok = ng * 128
                nsb = (ng + 3) // 4
                cnt_ab = 0
                cnt_d = 0
                for e in range(NEXP):
                    wu, wd = wup[e % 2], wdn[e % 2]
                    if e + 1 < NEXP:
                        load_expert(e + 1)
                    for sb_ in range(nsb):
                        t0 = sb_ * 4
                        nt_ = min(4, ng - t0)
                        w_ = nt_ * 128
                        aT_ = actT[(e * nsb + sb_) % 2]
                        for j in range(4):
                            pa = ps_ab[cnt_ab % 3]
                            pb = ps_ab[(cnt_ab + 1) % 3]
                            cnt_ab += 2
                            for kc in range(8):
                                k.mm(pa[:, 0:w_], wu[:, kc, j * 128:(j + 1) * 128], h2T[:, kc, t0 * 128:t0 * 128 + w_], start=(kc == 0), stop=(kc == 7))
                            for kc in range(8):
                                k.mm(pb[:, 0:w_], wu[:, kc, 512 + j * 128:512 + (j + 1) * 128], h2T[:, kc, t0 * 128:t0 * 128 + w_], start=(kc == 0), stop=(kc == 7))
                            s_ = sa[j % 2]
                            k.act(s_[:, 0:w_], pa[:, 0:w_], AF.Silu)
                            k.tt("dve", aT_[:, j, 0:w_], s_[:, 0:w_], pb[:, 0:w_], ALU.mult)
                        for t in range(nt_):
                            ti = t0 + t
                            for half in range(2):
                                pd = ps_d[cnt_d % 2]
                                cnt_d += 1
                                for j in range(4):
                                    k.mm(pd[:], aT_[:, j, t * 128:(t + 1) * 128], wd[:, j, half * 512:(half + 1) * 512], start=(j == 0), stop=(j == 3))
                                av = acc[:, ti, half * 512:(half + 1) * 512]
                                if e == 0:
                                    k.ts("dve", av, pd[:], wt[:, ti, e:e + 1], ALU.mult)
                                else:
                                    k.stt("dve", av, pd[:], wt[:, ti, e:e + 1], av, ALU.mult, ALU.add)
                if self.kstop == "post2":
                    return
                def f_load(ti):
                    tt = G[ti]
                    k.dma("sp", xt[ti % 2][:], self.xs.key(tt)[tt * 128:(tt + 1) * 128, :])
                f_load(0)
                for ti, tt in enumerate(G):
                    if ti + 1 < ng:
                        f_load(ti + 1)
                    cond = self.cond_of(tt)
                    self.set_cond(st2, cond, need_norm=False)
                    x_ = xt[ti % 2]
                    k.tt("pool", scr[:], acc[:, ti, :], st2["gate"][:], ALU.mult)
                    k.tt("pool", x_[:], scr[:], x_[:], ALU.add)
                    if not final:
                        k.dma("sp", self.xs.key(tt)[tt * 128:(tt + 1) * 128, :], x_[:])
                    elif self.is_lat(tt):
                        b, j = divmod(tt, TB)
                        self.norm_mod(x_[:], fg[:], None, h2[:], scr[:], ss[:])
                        r0 = (b * TS + j) * 128
                        k.dma("sp", self.y.key(tt)[r0:r0 + 128, :], h2[:])

    def router(self, pr, rb, lg, sm, wt_out):
        k = self.k
        k.tt("dve", lg[:], pr, rb[:], ALU.add)
        lgg = lg[:, 0:4]
        le = lg[:, 4:36]
        k.op("dve", "tensor_reduce", out=sm["gmax"][:], in_=lgg, axis=AX.X, op=ALU.max)
        k.ts("dve", sm["ngmax"][:], sm["gmax"][:], -1.0, ALU.mult)
        k.ts("dve", sm["oh"][:], lgg, sm["gmax"][:, 0:1], ALU.is_equal)
        k.act(sm["eg"][:], lgg, AF.Exp, bias=sm["ngmax"][:, 0:1], accum_out=sm["gsum"][:])
        k.tt("dve", sm["les"][:].rearrange("p (g e) -> p g e", e=8), le.rearrange("p (g e) -> p g e", e=8),
             sm["oh"][:].w(lambda a: a.unsqueeze(2).to_broadcast([128, 4, 8])), ALU.mult)
        k.op("dve", "tensor_reduce", out=sm["lsel"][:], in_=sm["les"][:].rearrange("p (g e) -> p e g", e=8), axis=AX.X, op=ALU.add)
        k.op("dve", "tensor_reduce", out=sm["emax"][:], in_=sm["lsel"][:], axis=AX.X, op=ALU.max)
        k.ts("dve", sm["nemax"][:], sm["emax"][:], -1.0, ALU.mult)
        k.act(sm["ee"][:], sm["lsel"][:], AF.Exp, bias=sm["nemax"][:, 0:1])
        k.ts("dve", sm["mk1"][:], sm["lsel"][:], sm["emax"][:, 0:1], ALU.is_equal)
        k.stt("dve", sm["ee2"][:], sm["mk1"][:], -2.0, sm["ee"][:], ALU.mult, ALU.add)
        k.op("dve", "tensor_reduce", out=sm["m2"][:], in_=sm["ee2"][:], axis=AX.X, op=ALU.max)
        k.ts("dve", sm["mk2"][:], sm["ee2"][:], sm["m2"][:, 0:1], ALU.is_equal)
        k.stt("dve", sm["wsel"][:], sm["mk2"][:], sm["m2"][:, 0:1], sm["mk1"][:], ALU.mult, ALU.add)
        k.ts("dve", sm["fac"][:], sm["m2"][:], 1.0, ALU.add, sm["gsum"][:, 0:1], ALU.mult)
        k.op("dve", "reciprocal", out=sm["fac"][:], in_=sm["fac"][:])
        k.ts("dve", sm["wsel"][:], sm["wsel"][:], sm["fac"][:, 0:1], ALU.mult)
        k.tt("dve", wt_out.rearrange("p (g e) -> p g e", e=8),
             sm["oh"][:].w(lambda a: a.unsqueeze(2).to_broadcast([128, 4, 8])),
             sm["wsel"][:].w(lambda a: a.unsqueeze(1).to_broadcast([128, 4, 8])), ALU.mult)


WEIGHT_NAMES = ["mod_w", "mod_b", "norm_g", "final_g", "win_w_qkv", "win_sink", "win_w_o",
                "diff_w_qkv", "diff_lambda", "diff_subln_g", "diff_w_o",
                "pool_w_group", "pool_b_group", "pool_scale",
                "rwkv_mu", "rwkv_w_rkv", "rwkv_w0", "rwkv_w_a1", "rwkv_w_a2", "rwkv_a0", "rwkv_a_a1", "rwkv_a_a2",
                "rwkv_g1", "rwkv_g2", "rwkv_k_k", "rwkv_k_a", "rwkv_r_k", "rwkv_ln_w", "rwkv_ln_b", "rwkv_w_o",
                "moe_wg", "moe_bg", "moe_we", "moe_be", "moe_w_up", "moe_w_down"]


def run_model(inputs, layers, n_cores, NB):
    x = np.asarray(inputs["x"], np.float32)
    ctx = np.asarray(inputs["ctx"], np.float32)
    c = np.asarray(inputs["c"], np.float32)
    c_ctx = np.asarray(inputs["c_ctx"], np.float32)
    B, S, _ = x.shape
    C = ctx.shape[1]
    assert B == n_cores * NB
    weights = {n: np.asarray(inputs[n], np.float32) for n in WEIGHT_NAMES}
    if list(layers) != [0, 1, 2, 3]:
        for n in ["mod_w", "mod_b", "norm_g", "moe_wg", "moe_we", "moe_bg", "moe_be", "moe_w_up", "moe_w_down"]:
            weights[n] = weights[n][list(layers)]
    weights = {n: np.ascontiguousarray(w) for n, w in weights.items()}
    consts = make_consts(S)
    prog = Prog(NB, S, C, layers)
    nc = prog.build({n: w.shape for n, w in weights.items()}, {n: v.shape for n, v in consts.items()})
    in_maps = []
    for i in range(n_cores):
        bs = range(i * NB, (i + 1) * NB)
        xin = np.concatenate([np.concatenate([x[b], ctx[b]], axis=0) for b in bs], axis=0)
        cc = np.stack([c[b] for b in bs] + [c_ctx] * (3 - NB), axis=0) if NB == 2 else None
        m = {"xin": np.ascontiguousarray(xin), "cc": np.ascontiguousarray(cc)}
        m.update(weights)
        m.update(consts)
        in_maps.append(m)
    res = run_bass_kernel_spmd(nc, in_maps, core_ids=list(range(n_cores)))
    global LAST_DBG
    LAST_DBG = [{n: np.asarray(r[n]) for n, _ in (prog.dbg + getattr(prog, "dbg2", []))} for r in res.results]
    out = np.concatenate([r["y"].reshape(NB, S, D) for r in res.results], axis=0)
    return out.astype(np.float32)


def kernel(**inputs):
    return run_model(inputs, [0, 1, 2, 3], 8, 2)


def diff_attention(self, l, src, last):
    k = self.k
    NB, TS, TC, TB, NT = self.NB, self.TS, self.TC, self.TB, self.NT
    W_ = TB * 128
    lam_init = 0.8 - 0.6 * math.exp(-0.3 * l)
    qT_d = k.dram("dif_qT", [NB, 16, 65, W_], BF16)
    kT_d = k.dram("dif_kT", [NB, 16, 65, W_], BF16)
    v_d = k.dram("dif_v", [NB, 8, 128, TB, 129], BF16)
    with k.phase("diffP1"):
        ident_bf = k.sb("ident_bf", [128, 128], BF16)
        k.dma("pool", ident_bf[:], self.Cn["ident"][:, :])
        ident_f = k.sb("ident_f", [128, 128], F32)
        k.dma("sp", ident_f[:], self.Cn["ident"][:, :])
        wqkv = self.load_w_bf16("wqkv", self.W["diff_w_qkv"][0], 8, 3072)
        st1 = self.alloc_mod(l, 0, gate=False)
        xt = [k.sb("xt%d" % i, [128, D], F32) for i in range(2)]
        cs = [k.sb("cos%d" % i, [128, 64], F32) for i in range(2)]
        sn = [k.sb("sin%d" % i, [128, 64], F32) for i in range(2)]
        scr = k.sb("scr", [128, D], F32)
        ss = k.sb("ss", [128, 1], F32)
        hb = k.sb("hb", [128, D], BF16)
        hT = k.sb("hT", [128, 8, 128], BF16)
        qk_aug = [k.sb("qk_aug%d" % i, [128, 16, 65], BF16) for i in range(2)]
        k.memset("pool", qk_aug[1][:, :, 64:65], 0.0)
        v_aug = [k.sb("v_aug%d" % i, [128, 8, 129], BF16) for i in range(2)]
        for i in range(2):
            k.memset("pool", v_aug[i][:, :, 128:129], 1.0)
        n2 = k.sb("n2", [128, 16], F32)
        nb_ = k.sb("nb_", [128, 16], BF16)
        kmax2 = k.sb("kmax2", [128, 16], F32)
        k.memset("dve", kmax2[:], 0.0)
        T_st = [k.sb("T_st%d" % i, [65, 16, 128], BF16) for i in range(2)]
        ps_tr = k.ps("ps_tr", [128, D], BF16)
        ps_a = [k.ps("ps_a%d" % i, [128, D], F32) for i in range(2)]
        ps_T = k.ps("ps_T", [128, 2048], BF16)

        def p1_load(tt):
            b, j = divmod(tt, TB)
            i = tt % 2
            k.dma("sp", xt[i][:], src.key(tt)[tt * 128:(tt + 1) * 128, :])
            if j < TS:
                k.dma("sp", cs[i][:], self.Cn["rope_cos"][j * 128:(j + 1) * 128, :])
                k.dma("sp", sn[i][:], self.Cn["rope_sin"][j * 128:(j + 1) * 128, :])
        p1_load(0)
        for tt in range(NT):
            b, j = divmod(tt, TB)
            lat = j < TS
            i = tt % 2
            if tt + 1 < NT:
                p1_load(tt + 1)
            self.set_cond(st1, self.cond_of(tt), need_gate=False)
            self.norm_mod(xt[i][:], st1["Gp"][:], st1["sh"][:], hb[:], scr[:], ss[:])
            self.transpose_tile(hb[:], hT[:].rearrange("p k t -> p (k t)"), ps_tr[:], ident_bf[:])
            for part in range(3):
                pa = ps_a[part % 2]
                for n in range(2):
                    c0 = part * 1024 + n * 512
                    for kc in range(8):
                        k.mm(pa[:, n * 512:(n + 1) * 512], hT[:, kc, :], wqkv[:, kc, c0:c0 + 512], start=(kc == 0), stop=(kc == 7))
                if part == 2:
                    k.copy("act", v_aug[i][:, :, 0:128], pa[:].rearrange("p (h d) -> p h d", d=128))
                    k.dma("sp", v_d.key(tt)[b, :, :, j, :].rearrange("h p c -> p h c"), v_aug[i][:])
                    continue
                aug = qk_aug[part]
                k.act(scr[:], pa[:], AF.Square)
                k.op("dve", "tensor_reduce", out=n2[:], in_=scr[:].rearrange("p (h d) -> p h d", d=64), axis=AX.X, op=ALU.add)
                if part == 0:
                    k.act(n2[:], n2[:], AF.Sqrt)
                    k.copy("dve", nb_[:], n2[:])
                    k.ts("dve", aug[:, :, 64:65], nb_[:].w(lambda a: a.unsqueeze(2)), -1.0, ALU.mult)
                else:
                    k.tt("dve", kmax2[:], kmax2[:], n2[:], ALU.max)
                if lat:
                    self.rope(pa[:], 16, cs[i][:], sn[i][:], scr[:], aug[:, :, 0:64])
                else:
                    k.copy("dve", aug[:, :, 0:64], pa[:].rearrange("p (h d) -> p h d", d=64))
                for h in range(16):
                    k.tr(ps_T[0:65, h * 128:(h + 1) * 128], aug[:, h, :], ident_bf[:])
                ts_ = T_st[part]
                k.copy("act", ts_[:].rearrange("r h t -> r (h t)"), ps_T[0:65, :])
                dst = qT_d if part == 0 else kT_d
                k.dma("sp", dst.key(tt)[b, :, :, j * 128:(j + 1) * 128].rearrange("h r t -> r h t"), ts_[:])
        km1 = k.sb("km1", [128, 1], F32)
        k.op("dve", "tensor_reduce", out=km1[:], in_=kmax2[:], axis=AX.X, op=ALU.max)
        k.tr(ps_a[0][0:1, 0:128], km1[:], ident_f[:])
        kmx = k.sb("kmx", [1, 1], F32)
        kmxb = k.sb("kmxb", [1, 1], BF16)
        k.op("dve", "tensor_reduce", out=kmx[:], in_=ps_a[0][0:1, 0:128], axis=AX.X, op=ALU.max)
        k.act(kmx[:], kmx[:], AF.Sqrt)
        k.copy("dve", kmxb[:], kmx[:])
        k.copy("dve", kmx[:], kmxb[:])
        kmrow = k.sb("kmrow", [1, W_], BF16)
        k.memset("pool", kmrow[:], 1.0)
        k.ts("dve", kmrow[:], kmrow[:], kmx[0:1, 0:1], ALU.mult)
        for b in range(NB):
            for g in range(16):
                k.dma("sp", kT_d[b, g, 64:65, :], kmrow[:])
    if self.kstop == "p1":
        return
    with k.phase("diffP2"):
        lamt = k.sb("lamt", [128, 256], F32)
        k.dma("sp", lamt[:], self.W["diff_lambda"][0:1, :, :].rearrange("o a d -> o (a d)").w(lambda a: a.partition_broadcast(128)))
        lp = k.sb("lp", [128, 128], F32)
        l2 = k.sb("l2", [128, 2], F32)
        l4 = lamt[:].rearrange("p (a d) -> p a d", d=64)
        k.tt("dve", lp[:, 0:64], l4[:, 0, :], l4[:, 1, :], ALU.mult)
        k.tt("dve", lp[:, 64:128], l4[:, 2, :], l4[:, 3, :], ALU.mult)
        k.op("dve", "tensor_reduce", out=l2[:], in_=lp[:].rearrange("p (a d) -> p a d", d=64), axis=AX.X, op=ALU.add)
        k.act(l2[:], l2[:], AF.Exp)
        nlam = k.sb("nlam", [128, 1], F32)
        k.tt("dve", nlam[:], l2[:, 1:2], l2[:, 0:1], ALU.subtract)
        k.ts("dve", nlam[:], nlam[:], -lam_init, ALU.add)
        gsub = k.sb("gsub", [128, 128], F32)
        k.dma("sp", gsub[:], self.W["diff_subln_g"][0:1, :].w(lambda a: a.partition_broadcast(128)))
        k.ts("dve", gsub[:], gsub[:], 1.0 - lam_init, ALU.mult)
        qT_g = [k.sb("qT_g%d" % i, [65, 2, W_], BF16) for i in range(2)]
        kT_g = [k.sb("kT_g%d" % i, [65, 2, W_], BF16) for i in range(2)]
        v_g = [k.sb("v_g%d" % i, [128, TB, 129], BF16) for i in range(2)]
        ET = [k.sb("ET%d" % i, [128, 512], BF16) for i in range(3)]
        o1 = [k.sb("o1_%d" % i, [128, 128], F32) for i in range(2)]
        osq = k.sb("osq", [128, 128], F32)
        rr = [k.sb("rr%d" % i, [128, 2], F32) for i in range(2)]
        sso = [k.sb("sso%d" % i, [128, 1], F32) for i in range(2)]
        ao = [k.sb("ao%d" % i, [128, 128], BF16) for i in range(2)]
        ps_s = [k.ps("ps_s%d" % i, [128, 512], F32) for i in range(3)]
        ps_o = [[k.ps("ps_o%d_%d" % (m, i), [128, 512], F32) for i in range(2)] for m in range(2)]
        sblocks = [(q0, min(4, TS - q0), list(range(TB))) for q0 in range(0, TS, 4)]
        sblocks += [(TS + q0, min(4, TC - q0), list(range(TS, TB))) for q0 in range(0, TC, 4)]
        it = 0
        cnt = 0
        def d2_load(b, h):
            bi = (b * 8 + h) % 2
            k.dma("sp", qT_g[bi][:], qT_d[b, 2 * h:2 * h + 2, :, :].rearrange("m r t -> r m t"))
            k.dma("sp", kT_g[bi][:], kT_d[b, 2 * h:2 * h + 2, :, :].rearrange("m r t -> r m t"))
            k.dma("sp", v_g[bi][:], v_d[b, h, :, :, :])
        bhs = [(b, h) for b in range(NB) for h in range(8)]
        d2_load(*bhs[0])
        for bhi, (b, h) in enumerate(bhs):
            if True:
                bi = (b * 8 + h) % 2
                if bhi + 1 < len(bhs):
                    d2_load(*bhs[bhi + 1])
                for (q0, nq, kbs) in sblocks:
                    wq = nq * 128
                    for m in range(2):
                        for idx, jb in enumerate(kbs):
                            pss = ps_s[it % 3]
                            et = ET[it % 3]
                            it += 1
                            k.mm(pss[:, 0:wq], kT_g[bi][:, m, jb * 128:(jb + 1) * 128], qT_g[bi][:, m, q0 * 128:q0 * 128 + wq])
                            k.act(et[:, 0:wq], pss[:, 0:wq], AF.Exp, scale=0.125)
                            for qb in range(nq):
                                po = ps_o[m][qb // 2]
                                c0 = (qb % 2) * 129
                                k.mm(po[:, c0:c0 + 129], et[:, qb * 128:(qb + 1) * 128], v_g[bi][:, jb, :],
                                     start=(idx == 0 and qb % 2 == 0), stop=(idx == len(kbs) - 1), skip=True)
                    for qb in range(nq):
                        c0 = (qb % 2) * 129
                        p1 = ps_o[0][qb // 2]
                        p2 = ps_o[1][qb // 2]
                        ci = cnt % 2
                        cnt += 1
                        r_ = rr[ci]
                        k.copy("dve", r_[:, 0:1], p1[:, c0 + 128:c0 + 129])
                        k.copy("dve", r_[:, 1:2], p2[:, c0 + 128:c0 + 129])
                        k.op("dve", "reciprocal", out=r_[:], in_=r_[:])
                        k.ts("dve", r_[:, 1:2], r_[:, 1:2], nlam[:, 0:1], ALU.mult)
                        k.ts("dve", o1[ci][:], p1[:, c0:c0 + 128], r_[:, 0:1], ALU.mult)
                        k.stt("dve", o1[ci][:], p2[:, c0:c0 + 128], r_[:, 1:2], o1[ci][:], ALU.mult, ALU.add)
                        k.act(osq[:], o1[ci][:], AF.Square, accum_out=sso[ci][:])
                        k.ts("dve", sso[ci][:], sso[ci][:], 1.0 / 128, ALU.mult, NORM_EPS, ALU.add)
                        k.act(sso[ci][:], sso[ci][:], AF.Sqrt)
                        k.op("dve", "reciprocal", out=sso[ci][:], in_=sso[ci][:])
                        k.stt("dve", ao[ci][:], o1[ci][:], sso[ci][:, 0:1], gsub[:], ALU.mult, ALU.mult)
                        tt = b * TB + q0 + qb
                        k.dma("sp", self.attn_d.key(tt)[tt * 128:(tt + 1) * 128, h * 128:(h + 1) * 128], ao[ci][:])


Prog.diff_attention = diff_attention


POOL_WINDOWS = (2, 4, 8, 16)


def pool_band_consts():
    out = np.zeros((20, 128, 128), np.float32)
    Lbig = 128 * 8
    for wi, w in enumerate(POOL_WINDOWS):
        for vi, tile in enumerate([0, 3, 7]):
            for i in range(128):
                p = tile * 128 + i
                lo = min(max(p - w // 2, 0), Lbig)
                hi = min(max(p + w - w // 2, 0), Lbig)
                cnt = hi - lo
                for q in range(lo, hi):
                    j = q - tile * 128
                    if 0 <= j < 128:
                        out[wi * 3 + vi, j, i] += 1.0 / cnt
                    elif j < 0 and vi == 1:
                        out[12 + wi, j + 128, i] += 1.0 / cnt
                    elif j >= 128 and vi == 1:
                        out[16 + wi, j - 128, i] += 1.0 / cnt
                out[wi * 3 + vi, i, i] -= 1.0
    return out


def pool_mixer(self, l, src, last):
    k = self.k
    NB, TS, TC, TB, NT = self.NB, self.TS, self.TC, self.TB, self.NT
    self.y_d = getattr(self, "y_d", None) or k.dram("pool_y", [NT * 128, D], F32)
    with k.phase("pool"):
        band = k.sb("band", [128, 20, 128], F32)
        k.dma("sp", band[:], self.Cn["pool_band"][:, :, :].rearrange("n j i -> j n i"))
        wg = k.sb("wg", [128, 4, 2, 256], BF16)
        for g in range(4):
            for cc in range(2):
                k.dma("pool", wg[:, g, cc, :], self.W["pool_w_group"][0, g, cc * 128:(cc + 1) * 128, :])
        bgb = k.sb("bgb", [128, D], F32)
        lsb = k.sb("lsb", [128, D], F32)
        k.dma("sp", bgb[:], self.W["pool_b_group"][0:1, :].w(lambda a: a.partition_broadcast(128)))
        k.dma("sp", lsb[:], self.W["pool_scale"][0:1, :].w(lambda a: a.partition_broadcast(128)))
        st1 = self.alloc_mod(l, 0, gate=False)
        xt = [k.sb("xt%d" % i, [128, D], F32) for i in range(2)]
        hh = [k.sb("hh%d" % i, [128, D], F32) for i in range(3)]
        scr = k.sb("scr", [128, D], F32)
        ss = k.sb("ss", [128, 1], F32)
        yT = k.sb("yT", [128, 8, 128], BF16)
        yo = [k.sb("yo%d" % i, [128, D], F32) for i in range(2)]
        ps_p = k.ps("ps_p", [128, D], F32)
        ps_y = k.ps("ps_y", [128, D], F32)
        seqs = []
        for b in range(NB):
            seqs.append([b * TB + j for j in range(TS)])
            seqs.append([b * TB + TS + j for j in range(TC)])
        cnt = 0
        for seq in seqs:
            n = len(seq)
            assert n >= 2

            def mk_h(t):
                nonlocal cnt
                tt = seq[t]
                x_ = xt[cnt % 2]
                cnt += 1
                k.dma("sp", x_[:], src.key(tt)[tt * 128:(tt + 1) * 128, :])
                self.set_cond(st1, self.cond_of(tt), need_gate=False)
                self.norm_mod(x_[:], st1["Gp"][:], st1["sh"][:], hh[t % 3][:], scr[:], ss[:])
            mk_h(0)
            for t in range(n):
                tt = seq[t]
                if t + 1 < n:
                    mk_h(t + 1)
                vi = 0 if t == 0 else (2 if t == n - 1 else 1)
                for c in range(8):
                    wi = c // 2
                    srcs = [(hh[t % 3], band[:, wi * 3 + vi, :])]
                    if t > 0:
                        srcs.append((hh[(t - 1) % 3], band[:, 12 + wi, :]))
                    if t + 1 < n:
                        srcs.append((hh[(t + 1) % 3], band[:, 16 + wi, :]))
                    for si, (hsrc, bm) in enumerate(srcs):
                        k.mm(ps_p[:, c * 128:(c + 1) * 128], hsrc[:, c * 128:(c + 1) * 128], bm, start=(si == 0), stop=(si == len(srcs) - 1))
                k.copy("act", yT[:].rearrange("p c t -> p (c t)"), ps_p[:])
                for g in range(4):
                    for cc in range(2):
                        k.mm(ps_y[:, g * 256:(g + 1) * 256], yT[:, 2 * g + cc, :], wg[:, g, cc, :], start=(cc == 0), stop=(cc == 1))
                yo_ = yo[t % 2]
                k.tt("dve", yo_[:], ps_y[:], bgb[:], ALU.add)
                k.tt("pool", yo_[:], yo_[:], lsb[:], ALU.mult)
                k.dma("sp", self.y_d.key(tt)[tt * 128:(tt + 1) * 128, :], yo_[:])


Prog.pool_mixer = pool_mixer


def rwkv_consts():
    c = {}
    row = np.arange(128)[:, None]
    col = np.arange(128)[None, :]
    ms = {"U": row < col, "Lo": row > col, "UI": row <= col, "LI": row >= col}
    c["rw_masks"] = np.stack([np.tile(ms["U"], (1, 4)), np.tile(ms["Lo"], (1, 4)),
                              -np.tile(ms["UI"], (1, 4)).astype(np.float32), -np.tile(ms["LI"], (1, 4)).astype(np.float32),
                              np.tile(ms["UI"], (1, 4)), np.tile(ms["LI"], (1, 4))], axis=0).astype(np.float32)
    bo = np.zeros((128, 128), np.float32)
    bo[:64, :64] = 1.0
    bo[64:, 64:] = 1.0
    c["rw_bones"] = bo
    sel = np.zeros((128, 2), np.float32)
    sel[:64, 0] = 1.0
    sel[64:, 1] = 1.0
    c["rw_sel2"] = sel
    c["rw_i2"] = np.concatenate([np.eye(64), np.eye(64)], axis=0).astype(np.float32)
    return c


def rwkv_features(self, l, src):
    k = self.k
    NB, TS, TC, TB, NT = self.NB, self.TS, self.TC, self.TB, self.NT
    W_ = TB * 128
    hT_d = k.dram("rw_hT", [NB, D, W_], F32)
    self.F_d = k.dram("rw_F", [NB, 2, TB, 128, 6 * 1024], BF16)
    self.Wc_d = k.dram("rw_Wc", [NB, 2, TB, 128, 8], F32)
    self.v_d = k.dram("rw_v", [NT * 128, D], BF16)
    self.g_d = k.dram("rw_g", [NT * 128, D], BF16)
    self.bs_d = k.dram("rw_bs", [NT * 128, 16], F32)
    with k.phase("rwA"):
        ident_f = k.sb("ident_f", [128, 128], F32)
        k.dma("sp", ident_f[:], self.Cn["ident"][:, :])
        st1 = self.alloc_mod(l, 0, gate=False)
        xt = [k.sb("xt%d" % i, [128, D], F32) for i in range(2)]
        scr = k.sb("scr", [128, D], F32)
        ss = k.sb("ss", [128, 1], F32)
        h = k.sb("h", [128, D], F32)
        hT = [k.sb("hT%d" % i, [128, 8, 128], F32) for i in range(2)]
        ps = [k.ps("psA%d" % i, [128, D], F32) for i in range(2)]
        k.dma("sp", xt[0][:], src.key(0)[0:128, :])
        for tt in range(NT):
            b, j = divmod(tt, TB)
            i = tt % 2
            if tt + 1 < NT:
                k.dma("sp", xt[1 - i][:], src.key(tt + 1)[(tt + 1) * 128:(tt + 2) * 128, :])
            self.set_cond(st1, self.cond_of(tt), need_gate=False)
            self.norm_mod(xt[i][:], st1["Gp"][:], st1["sh"][:], h[:], scr[:], ss[:])
            for kc in range(8):
                k.tr(ps[i][:, kc * 128:(kc + 1) * 128], h[:, kc * 128:(kc + 1) * 128], ident_f[:])
            k.copy("act", hT[i][:].rearrange("p c t -> p (c t)"), ps[i][:])
            k.dma("sp", hT_d.key(tt)[b, :, j * 128:(j + 1) * 128].rearrange("(c p) t -> p c t", p=128), hT[i][:])
    if self.kstop == "rwA":
        return
    with k.phase("rwB"):
        def wload(name, srcv, kch, n):
            return self.load_w_bf16(name, srcv, kch, n)
        Wr = wload("Wr", self.W["rwkv_w_rkv"][0, 0], 8, D)
        Wk = wload("Wk", self.W["rwkv_w_rkv"][0, 1], 8, D)
        Wv = wload("Wv", self.W["rwkv_w_rkv"][0, 2], 8, D)
        wa1 = k.sb("wa1", [128, 8, 128], BF16)
        aa1 = k.sb("aa1", [128, 8, 128], BF16)
        wa2 = k.sb("wa2", [128, D], BF16)
        aa2 = k.sb("aa2", [128, D], BF16)
        for d in range(2):
            for kc in range(8):
                k.dma("pool", wa1[:, kc, d * 64:(d + 1) * 64], self.W["rwkv_w_a1"][0, d, kc * 128:(kc + 1) * 128, :])
                k.dma("pool", aa1[:, kc, d * 64:(d + 1) * 64], self.W["rwkv_a_a1"][0, d, kc * 128:(kc + 1) * 128, :])
            k.dma("pool", wa2[d * 64:(d + 1) * 64, :], self.W["rwkv_w_a2"][0, d, :, :])
            k.dma("pool", aa2[d * 64:(d + 1) * 64, :], self.W["rwkv_a_a2"][0, d, :, :])
        g1 = wload("g1", self.W["rwkv_g1"][0], 8, 128)
        g2 = k.sb("g2", [128, D], BF16)
        k.dma("pool", g2[:], self.W["rwkv_g2"][0, :, :])
        sc = k.sb("sc", [128, 8, 24], F32)

        def scload(col, v1d):
            k.dma("sp", sc[:, :, col:col + 1], v1d.rearrange("(c p o) -> p c o", p=128, o=1), allow_slow_non_contiguous=True)
        for d in range(2):
            for i in range(6):
                scload(d * 6 + i, self.W["rwkv_mu"][0, d, i, :])
        scload(12, self.W["rwkv_k_k"][0, :])
        scload(13, self.W["rwkv_k_a"][0, :])
        scload(14, self.W["rwkv_r_k"][0, :, :].rearrange("h d -> (h d)"))
        scload(15, self.W["rwkv_w0"][0, 0, :])
        scload(16, self.W["rwkv_w0"][0, 1, :])
        scload(17, self.W["rwkv_a0"][0, 0, :])
        scload(18, self.W["rwkv_a0"][0, 1, :])
        c0 = k.sb("c0", [128, 8, 6], F32)
        k.tt("dve", c0[:], sc[:, :, 0:6], sc[:, :, 6:12], ALU.add)
        k.ts("dve", c0[:], c0[:], -1.0, ALU.mult, 1.0, ALU.add)
        oka = k.sb("oka", [128, 8, 1], F32)
        k.ts("dve", oka[:], sc[:, :, 13:14], -1.0, ALU.mult, 1.0, ALU.add)
        bones = k.sb("bones", [128, 128], F32)
        k.dma("sp", bones[:], self.Cn["rw_bones"][:, :])
        sel2 = k.sb("sel2", [128, 2], F32)
        k.dma("sp", sel2[:], self.Cn["rw_sel2"][:, :])
        WB = 256
        blk = k.sb("blk", [128, 8, WB + 2], F32)
        mix = [k.sb("mix%d" % i, [128, 8, WB], BF16) for i in range(2)]
        t_a = k.sb("t_a", [128, 8, WB], F32)
        t_b = k.sb("t_b", [128, 8, WB], F32)
        kT = k.sb("kT", [128, 8, WB], F32)
        rT = k.sb("rT", [128, 8, WB], F32)
        kkT = k.sb("kkT", [128, 8, WB], F32)
        aT = k.sb("aT_", [128, 8, WB], F32)
        kdT = k.sb("kdT", [128, 8, WB], F32)
        bT = k.sb("bT", [128, 8, WB], F32)
        lwT = k.sb("lwT", [128, 8, WB], F32)
        cA = k.sb("cA", [128, 8, WB], F32)
        cB = k.sb("cB", [128, 8, WB], F32)
        ex = k.sb("ex", [128, 8, WB], F32)
        fo = [k.sb("fo%d" % i, [128, 2, 6 * 1024], BF16) for i in range(1)]
        wc = k.sb("wc", [128, 2, 8], F32)
        hid = k.sb("hid", [128, WB], BF16)
        vt = [k.sb("vt%d" % i, [128, D], BF16) for i in range(2)]
        bsv = k.sb("bsv", [128, 16], F32)
        ps_m = [k.ps("ps_m%d" % i, [128, 512], F32) for i in range(4)]
        ps_v = k.ps("ps_v", [128, D], F32)
        ps_b = k.ps("ps_b", [128, 512], F32)
        pc = [0]

        def nps():
            pc[0] += 1
            return ps_m[pc[0] % 4]

        def bc(v, n):
            return v.w(lambda a: a.to_broadcast([128, 8, n]))

        blocks = []
        for b in range(NB):
            for (s0, n_) in [(0, TS), (TS, TC)]:
                for t0 in range(0, n_, 2):
                    blocks.append((b, s0, n_, t0, min(2, n_ - t0)))
        mi = [0]
        for (b, s0, n_, t0, nt) in blocks:
            wb = nt * 128
            lat = s0 == 0
            g0 = (s0 + t0) * 128
            first, lastb = t0 == 0, t0 + nt == n_
            lo = g0 - (0 if first else 1)
            hi = g0 + wb + (0 if lastb else 1)
            if first:
                k.memset("pool", blk[:, :, 0:1], 0.0)
            if lastb:
                k.memset("pool", blk[:, :, wb + 1:wb + 2], 0.0)
            k.dma("sp", blk[:, :, (1 if first else 0):(1 if first else 0) + (hi - lo)],
                  hT_d[b, :, lo:hi].rearrange("(c p) t -> p c t", p=128))
            cur, prv, nxt = blk[:, :, 1:wb + 1], blk[:, :, 0:wb], blk[:, :, 2:wb + 2]

            def mk_mix(i):
                m_ = mix[mi[0] % 2]
                mi[0] += 1
                k.tt("dve", t_a[:, :, 0:wb], prv, bc(sc[:, :, i:i + 1], wb), ALU.mult)
                k.tt("pool", t_b[:, :, 0:wb], nxt, bc(sc[:, :, 6 + i:7 + i], wb), ALU.mult)
                k.tt("pool", t_a[:, :, 0:wb], t_a[:, :, 0:wb], t_b[:, :, 0:wb], ALU.add)
                k.tt("dve", t_b[:, :, 0:wb], cur, bc(c0[:, :, i:i + 1], wb), ALU.mult)
                k.tt("pool", m_[:, :, 0:wb], t_a[:, :, 0:wb], t_b[:, :, 0:wb], ALU.add)
                return m_

            def proj_fm(m_, Wt, dst):
                for oc in range(8):
                    p_ = nps()
                    for kc in range(8):
                        k.mm(p_[:, 0:wb], Wt[:, kc, oc * 128:(oc + 1) * 128], m_[:, kc, 0:wb], start=(kc == 0), stop=(kc == 7))
                    k.copy("act", dst[:, oc, 0:wb], p_[:, 0:wb])
            m2 = mk_mix(2)
            proj_fm(m2, Wk, kT)
            k.tt("dve", kkT[:, :, 0:wb], kT[:, :, 0:wb], bc(sc[:, :, 12:13], wb), ALU.mult)
            k.tt("pool", t_a[:, :, 0:wb], kkT[:, :, 0:wb], kkT[:, :, 0:wb], ALU.mult)
            for oc in range(8):
                p_ = nps()
                k.mm(p_[:, 0:wb], bones[:], t_a[:, oc, 0:wb])
                k.act(t_b[:, oc, 0:wb], p_[:, 0:wb], AF.Sqrt)
            k.ts("dve", t_b[:, :, 0:wb], t_b[:, :, 0:wb], 1e-12, ALU.max)
            k.op("dve", "reciprocal", out=t_b[:, :, 0:wb], in_=t_b[:, :, 0:wb])
            k.tt("dve", kkT[:, :, 0:wb], kkT[:, :, 0:wb], t_b[:, :, 0:wb], ALU.mult)
            m3 = mk_mix(3)
            for t in range(nt):
                tt = b * TB + s0 + t0 + t
                for n in range(2):
                    for kc in range(8):
                        k.mm(ps_v[:, n * 512:(n + 1) * 512], m3[:, kc, t * 128:(t + 1) * 128], Wv[:, kc, n * 512:(n + 1) * 512], start=(kc == 0), stop=(kc == 7))
                k.copy("act", vt[t % 2][:], ps_v[:])
                k.dma("sp", self.v_d.key(tt)[tt * 128:(tt + 1) * 128, :], vt[t % 2][:])
            if lat:
                m0 = mk_mix(0)
                proj_fm(m0, Wr, rT)
                m5 = mk_mix(5)
                p_ = nps()
                for kc in range(8):
                    k.mm(p_[:, 0:wb], g1[:, kc, :], m5[:, kc, 0:wb], start=(kc == 0), stop=(kc == 7))
                k.act(hid[:, 0:wb], p_[:, 0:wb], AF.Sigmoid)
                for t in range(nt):
                    tt = b * TB + s0 + t0 + t
                    for n in range(2):
                        k.mm(ps_v[:, n * 512:(n + 1) * 512], hid[:, t * 128:(t + 1) * 128], g2[:, n * 512:(n + 1) * 512])
                    k.copy("act", vt[t % 2][:], ps_v[:])
                    k.dma("sp", self.g_d.key(tt)[tt * 128:(tt + 1) * 128, :], vt[t % 2][:])
            m1 = mk_mix(1)
            p_ = nps()
            for kc in range(8):
                k.mm(p_[:, 0:wb], wa1[:, kc, :], m1[:, kc, 0:wb], start=(kc == 0), stop=(kc == 7))
            hw = k_hw = hid
            k.act(hw[:, 0:wb], p_[:, 0:wb], AF.Tanh)
            lws = []
            for d in range(2):
                dst = lwT if d == 0 else cA
                for oc in range(8):
                    p_ = nps()
                    k.mm(p_[:, 0:wb], wa2[d * 64:(d + 1) * 64, oc * 128:(oc + 1) * 128], hw[d * 64:(d + 1) * 64, 0:wb])
                    k.act(dst[:, oc, 0:wb], p_[:, 0:wb], AF.Sigmoid, bias=sc[:, oc, 15 + d:16 + d])
            m4 = mk_mix(4)
            p_ = nps()
            for kc in range(8):
                k.mm(p_[:, 0:wb], aa1[:, kc, :], m4[:, kc, 0:wb], start=(kc == 0), stop=(kc == 7))
            ha = k.sb if False else None
            k.copy("act", hid[:, 0:wb], p_[:, 0:wb])
            first_bs = True
            for d in range(2):
                for oc in range(8):
                    p_ = nps()
                    k.mm(p_[:, 0:wb], aa2[d * 64:(d + 1) * 64, oc * 128:(oc + 1) * 128], hid[d * 64:(d + 1) * 64, 0:wb])
                    k.act(aT[:, oc, 0:wb], p_[:, 0:wb], AF.Sigmoid, bias=sc[:, oc, 17 + d:18 + d])
                k.tt("dve", t_a[:, :, 0:wb], aT[:, :, 0:wb], bc(sc[:, :, 13:14], wb), ALU.mult)
                k.tt("pool", t_a[:, :, 0:wb], t_a[:, :, 0:wb], bc(oka[:, :, 0:1], wb), ALU.add)
                k.tt("dve", kdT[:, :, 0:wb], kT[:, :, 0:wb], t_a[:, :, 0:wb], ALU.mult)
                k.tt("pool", bT[:, :, 0:wb], kkT[:, :, 0:wb], aT[:, :, 0:wb], ALU.mult)
                if lat:
                    k.tt("dve", t_a[:, :, 0:wb], rT[:, :, 0:wb], kdT[:, :, 0:wb], ALU.mult)
                    k.tt("pool", t_a[:, :, 0:wb], t_a[:, :, 0:wb], bc(sc[:, :, 14:15], wb), ALU.mult)
                    for t in range(nt):
                        for oc in range(8):
                            k.mm(ps_b[:, t * 16 + 2 * oc:t * 16 + 2 * oc + 2], t_a[:, oc, t * 128:(t + 1) * 128], sel2[:],
                                 start=first_bs, stop=(d == 1), skip=True)
                            first_bs = False
                lsrc = lwT if d == 0 else cA
                if d == 1:
                    k.copy("act", lwT[:, :, 0:wb], cA[:, :, 0:wb])
                k.ts("dve", lwT[:, :, 0:wb], lwT[:, :, 0:wb], -0.6065306597126334, ALU.mult)
                src_c, dst_c = lwT, cB
                bufs = [cB, ex]
                bi_ = 0
                cur_c = lwT
                for s_ in (1, 2, 4, 8, 16, 32, 64):
                    nb_ = bufs[bi_ % 2]
                    bi_ += 1
                    c4 = cur_c[:, :, 0:wb].rearrange("p c (t k) -> p c t k", k=128)
                    n4 = nb_[:, :, 0:wb].rearrange("p c (t k) -> p c t k", k=128)
                    if d == 0:
                        k.copy("act", n4[:, :, :, 0:s_], c4[:, :, :, 0:s_])
                        k.tt("dve", n4[:, :, :, s_:128], c4[:, :, :, s_:128], c4[:, :, :, 0:128 - s_], ALU.add)
                    else:
                        k.copy("act", n4[:, :, :, 128 - s_:128], c4[:, :, :, 128 - s_:128])
                        k.tt("dve", n4[:, :, :, 0:128 - s_], c4[:, :, :, 0:128 - s_], c4[:, :, :, s_:128], ALU.add)
                    cur_c = nb_
                cum = cur_c
                cum4 = cum[:, :, 0:wb].rearrange("p c (t k) -> p c t k", k=128)
                cend = cum4[:, :, :, 127:128] if d == 0 else cum4[:, :, :, 0:1]
                fo_ = fo[0]

                def fq(q):
                    return fo_[:, 0:nt, q * 1024:(q + 1) * 1024].rearrange("p t (c k) -> p c t k", k=128)

                def v4(x):
                    return x[:, :, 0:wb].rearrange("p c (t k) -> p c t k", k=128)
                k.tt("pool", t_a[:, :, 0:wb], cum[:, :, 0:wb], lwT[:, :, 0:wb], ALU.subtract)
                k.act(ex[:, :, 0:wb], t_a[:, :, 0:wb], AF.Exp)
                k.tt("dve", fq(0), v4(kkT), v4(ex), ALU.mult)
                k.act(ex[:, :, 0:wb], cum[:, :, 0:wb], AF.Exp)
                if lat:
                    k.tt("dve", fq(1), v4(rT), v4(ex), ALU.mult)
                k.act(ex[:, :, 0:wb], cum[:, :, 0:wb], AF.Exp, scale=-1.0)
                k.tt("dve", fq(2), v4(bT), v4(ex), ALU.mult)
                k.tt("pool", fq(3), v4(kdT), v4(ex), ALU.mult)
                k.tt("dve", v4(t_a), cend.w(lambda a: a.to_broadcast([128, 8, nt, 128])), v4(cum), ALU.subtract)
                k.act(ex[:, :, 0:wb], t_a[:, :, 0:wb], AF.Exp)
                k.tt("dve", fq(4), v4(bT), v4(ex), ALU.mult)
                k.tt("pool", fq(5), v4(kdT), v4(ex), ALU.mult)
                k.act(wc[:, 0:nt, :].rearrange("p t c -> p c t"), cend.rearrange("p c t o -> p c (t o)"), AF.Exp)
                tl0 = s0 + t0
                k.dma("sp", self.F_d.key((b, d, tl0))[b, d, tl0:tl0 + nt, :, :].rearrange("t p f -> p t f"), fo_[:, 0:nt, :])
                k.dma("sp", self.Wc_d.key((b, d, tl0))[b, d, tl0:tl0 + nt, :, :].rearrange("t p c -> p t c"), wc[:, 0:nt, :])
            if lat:
                for t in range(nt):
                    tt = b * TB + s0 + t0 + t
                    k.copy("dve", bsv[:], ps_b[:, t * 16:(t + 1) * 16])
                    k.dma("sp", self.bs_d.key(tt)[tt * 128:(tt + 1) * 128, :], bsv[:])


Prog.rwkv_features = rwkv_features


def rwkv_scan(self, l, src):
    k = self.k
    NB, TS, TC, TB, NT = self.NB, self.TS, self.TC, self.TB, self.NT
    o_d = k.dram("rw_o", [2, NT * 128, D], F32)
    self.o_d = o_d
    with k.phase("rwC"):
        ident_bf = k.sb("ident_bf", [128, 128], BF16)
        k.dma("pool", ident_bf[:], self.Cn["ident"][:, :])
        i2 = k.sb("i2", [128, 64], F32)
        k.dma("sp", i2[:], self.Cn["rw_i2"][:, :])
        masks = k.sb("masks", [128, 6, 512], F32)
        k.dma("sp", masks[:], self.Cn["rw_masks"][:, :, :].rearrange("m p f -> p m f"))
        f = [k.sb("f%d" % i, [128, 6 * 1024], BF16) for i in range(2)]
        wct = [k.sb("wct%d" % i, [128, 8], F32) for i in range(2)]
        vv = [k.sb("vv%d" % i, [128, D], BF16) for i in range(2)]
        Z = k.sb("Z", [128, 8, 64], BF16)
        NBh = k.sb("NBh", [128, 16, 64], BF16)
        Kh = k.sb("Kh", [128, 16, 64], BF16)
        LT = [k.sb("LT%d" % i, [128, 16, 128], F32) for i in range(2)]
        Lm = [k.sb("Lm%d" % i, [128, 16, 128], F32) for i in range(2)]
        LqkT = k.sb("LqkT", [128, 16, 128], BF16)
        NLrbT = k.sb("NLrbT", [128, 16, 128], BF16)
        LrkT = k.sb("LrkT", [128, 16, 128], BF16)
        Y32 = k.sb("Y32", [128, 16, 128], F32)
        Ybf = k.sb("Ybf", [128, 16, 128], BF16)
        RhT = k.sb("RhT", [64, 16, 128], BF16)
        Pt = k.sb("Pt", [64, 16, 64], BF16)
        G32 = k.sb("G32", [64, 16, 64], F32)
        M32 = k.sb("M32", [64, 16, 64], F32)
        Mbf = k.sb("Mbf", [64, 16, 64], BF16)
        ot = [k.sb("ot%d" % i, [128, D], F32) for i in range(2)]
        psw = [k.ps("psw%d" % i, [128, 512], F32) for i in range(7)]
        pst = k.ps("pst", [128, D], BF16)
        pc = [0]

        def nps():
            pc[0] += 1
            return psw[pc[0] % 7]
        lc = 0
        for b in range(NB):
            for d in range(2):
                order = (list(range(TS, TB)) + list(range(TS))) if d == 0 else (list(range(TB - 1, TS - 1, -1)) + list(range(TS - 1, -1, -1)))
                mA, mB, mNAI, mAI = (0, 1, 2, 4) if d == 0 else (1, 0, 3, 5)
                k.memset("dve", M32[:], 0.0)
                k.memset("pool", Mbf[:], 0.0)

                def c_load(tl, i):
                    tt = b * TB + tl
                    k.dma("sp", f[i][:], self.F_d[b, d, tl, :, :])
                    k.dma("sp", wct[i][:], self.Wc_d[b, d, tl, :, :])
                    k.dma("sp", vv[i][:], self.v_d[tt * 128:(tt + 1) * 128, :])
                c_load(order[0], lc % 2)
                for oi, tl in enumerate(order):
                    tt = b * TB + tl
                    lat = tl < TS
                    fi = f[lc % 2]
                    wc_ = wct[lc % 2]
                    V = vv[lc % 2]
                    lc += 1
                    if oi + 1 < len(order):
                        c_load(order[oi + 1], lc % 2)

                    def X(q, h):
                        c_, hp = divmod(h, 2)
                        return fi[hp * 64:(hp + 1) * 64, q * 1024 + c_ * 128:q * 1024 + (c_ + 1) * 128]

                    def Vh(h):
                        return V[:, h * 64:(h + 1) * 64]
                    def hsel4(x, gg, hp):
                        return x.rearrange("p (g i q) t -> p g i q t", g=2, i=4, q=2)[:, gg, :, hp, :]

                    def hsel8(x, hp):
                        return x.rearrange("p (i q) t -> p i q t", q=2)[:, :, hp, :]
                    G4 = [(gg, hp) for hp in range(2) for gg in range(2)]
                    for q, dst in ((0, None), (4, NBh), (5, Kh)):
                        for c_ in range(8):
                            k.tr(pst[:, c_ * 128:(c_ + 1) * 128], fi[:, q * 1024 + c_ * 128:q * 1024 + (c_ + 1) * 128], ident_bf[:])
                        pv = pst[:].rearrange("p (h k) -> p h k", k=64)
                        if q == 0:
                            k.copy("act", Y32[:, :, 0:64], pv)
                        elif q == 4:
                            k.ts("dve", dst[:], pv, -1.0, ALU.mult)
                        else:
                            k.copy("act", dst[:], pv)
                    k.tt("dve", Z[:], i2[:].w(lambda a: a.unsqueeze(1).to_broadcast([128, 8, 64])),
                         wc_[:].w(lambda a: a.unsqueeze(2).to_broadcast([128, 8, 64])), ALU.mult)
                    if self.kstop == "rwC0":
                        return
                    jobs = [(2, 0, LT[0], mA), (0, 2, Lm[0], mB), (3, 0, LqkT, mA)]
                    if lat:
                        jobs += [(2, 1, NLrbT, mNAI), (3, 1, LrkT, mAI)]
                    ev = 0
                    for (qa, qb, dst, mi_) in jobs:
                        for (gg, hp) in G4:
                            p_ = nps()
                            for hh in range(4):
                                h = 8 * gg + 2 * hh + hp
                                k.mm(p_[:, hh * 128:(hh + 1) * 128], X(qa, h), X(qb, h))
                            k.tt("dve", hsel4(dst[:], gg, hp), p_[:].rearrange("p (h t) -> p h t", t=128),
                                 masks[:, mi_, :].rearrange("p (h t) -> p h t", t=128), ALU.mult)
                    if self.kstop == "rwC1":
                        return
                    for g8 in range(2):
                        p_ = nps()
                        for hh in range(8):
                            h = 8 * g8 + hh
                            k.mm(p_[:, hh * 64:(hh + 1) * 64], LqkT[:, h, :], Vh(h))
                        k.copy("act", Y32[:, 8 * g8:8 * g8 + 8, 64:128], p_[:].rearrange("p (h k) -> p h k", k=64))
                    if self.kstop == "rwC2":
                        return
                    cur = 0
                    for lev in range(7):
                        if lev > 0:
                            nxt = 1 - cur
                            for g4 in range(4):
                                if lev < 6:
                                    p_ = nps()
                                    for hh in range(4):
                                        h = 4 * g4 + hh
                                        k.mm(p_[:, hh * 128:(hh + 1) * 128], LT[cur][:, h, :], Lm[cur][:, h, :])
                                    k.copy("act", Lm[nxt][:, 4 * g4:4 * g4 + 4, :].rearrange("p h t -> p (h t)"), p_[:])
                                p_ = nps()
                                for hh in range(4):
                                    h = 4 * g4 + hh
                                    k.mm(p_[:, hh * 128:(hh + 1) * 128], Lm[cur][:, h, :], LT[cur][:, h, :])
                                k.copy("dve", LT[nxt][:, 4 * g4:4 * g4 + 4, :].rearrange("p h t -> p (h t)"), p_[:])
                            cur = nxt
                        for g4 in range(4):
                            p_ = nps()
                            for hh in range(4):
                                h = 4 * g4 + hh
                                k.mm(p_[:, hh * 128:(hh + 1) * 128], LT[cur][:, h, :], Y32[:, h, :])
                            ysl = Y32[:, 4 * g4:4 * g4 + 4, :].rearrange("p h t -> p (h t)")
                            k.tt("dve", ysl, ysl, p_[:], ALU.subtract if lev == 0 else ALU.add)
                            if lev == 6:
                                k.copy("act", Ybf[:, 4 * g4:4 * g4 + 4, :].rearrange("p h t -> p (h t)"), ysl)
                    if self.kstop == "rwC3":
                        return
                    if lat:
                        for (gg, hp) in G4:
                            p_ = nps()
                            for hh in range(4):
                                h = 8 * gg + 2 * hh + hp
                                o_ = p_[0:64, hh * 128:(hh + 1) * 128]
                                k.mm(o_, Ybf[:, h, 0:64], NLrbT[:, h, :], start=True, stop=False)
                                k.mm(o_, ident_bf[hp * 64:(hp + 1) * 64, hp * 64:(hp + 1) * 64], X(1, h), start=False, stop=True)
                            k.copy("act", hsel4(RhT[:], gg, hp), p_[0:64, :].rearrange("p (h t) -> p h t", t=128))
                    for hp in range(2):
                        p_ = nps()
                        for hh in range(8):
                            h = 2 * hh + hp
                            c_ = hh
                            o_ = p_[0:64, hh * 64:(hh + 1) * 64]
                            k.mm(o_, ident_bf[hp * 64:(hp + 1) * 64, hp * 64:(hp + 1) * 64], Z[hp * 64:(hp + 1) * 64, c_, :], start=True, stop=False)
                            k.mm(o_, Ybf[:, h, 0:64], NBh[:, h, :], start=False, stop=True)
                        k.copy("act", hsel8(Pt[:], hp), p_[0:64, :].rearrange("p (h t) -> p h t", t=64))
                    for g8 in range(2):
                        p_ = nps()
                        for hh in range(8):
                            h = 8 * g8 + hh
                            o_ = p_[0:64, hh * 64:(hh + 1) * 64]
                            k.mm(o_, Kh[:, h, :], Vh(h), start=True, stop=False)
                            k.mm(o_, NBh[:, h, :], Ybf[:, h, 64:128], start=False, stop=True)
                        k.copy("dve", G32[:, 8 * g8:8 * g8 + 8, :].rearrange("p h t -> p (h t)"), p_[0:64, :])
                    if self.kstop == "rwC4":
                        self.dbg2 = []
                        for nm, tl_, shp, dt in [("LT0", LT[0], [128, 2048], BF16), ("Lm0", Lm[0], [128, 2048], BF16), ("LqkT", LqkT, [128, 2048], BF16),
                                                 ("Y32", Y32, [128, 2048], F32), ("Pt", Pt, [64, 1024], BF16), ("G32", G32, [64, 1024], F32),
                                                 ("NBh", NBh, [128, 1024], BF16), ("Kh", Kh, [128, 1024], BF16), ("LTc", LT[cur], [128, 2048], BF16)]:
                            o = k.dram("dbg2_" + nm, shp, dt, kind="ExternalOutput")
                            k.dma("sp", o[:, :], tl_[:].rearrange("p h t -> p (h t)"))
                            self.dbg2.append(("dbg2_" + nm, o))
                        k.wait_all("sp", [o[:, :] for _, o in self.dbg2])
                        return
                    if lat:
                        o_t = ot[oi % 2]
                        for g8 in range(2):
                            p_ = nps()
                            for hh in range(8):
                                h = 8 * g8 + hh
                                o_ = p_[:, hh * 64:(hh + 1) * 64]
                                k.mm(o_, LrkT[:, h, :], Vh(h), start=True, stop=False)
                                k.mm(o_, NLrbT[:, h, :], Ybf[:, h, 64:128], start=False, stop=False)
                                k.mm(o_, RhT[:, h, :], Mbf[:, h, :], start=False, stop=True)
                            k.copy("act", o_t[:, g8 * 512:(g8 + 1) * 512], p_[:])
                        k.dma("sp", o_d.key((d, tt))[d, tt * 128:(tt + 1) * 128, :], o_t[:])
                    for g8 in range(2):
                        p_ = nps()
                        for hh in range(8):
                            h = 8 * g8 + hh
                            k.mm(p_[0:64, hh * 64:(hh + 1) * 64], Pt[:, h, :], Mbf[:, h, :])
                        msl = M32[:, 8 * g8:8 * g8 + 8, :].rearrange("p h t -> p (h t)")
                        k.tt("dve", msl, p_[0:64, :], G32[:, 8 * g8:8 * g8 + 8, :].rearrange("p h t -> p (h t)"), ALU.add)
                        k.copy("act", Mbf[:, 8 * g8:8 * g8 + 8, :].rearrange("p h t -> p (h t)"), msl)
    if self.kstop == "rwC":
        return
    with k.phase("rwD"):
        lnw = k.sb("lnw", [128, D], F32)
        lnb = k.sb("lnb", [128, D], F32)
        k.dma("sp", lnw[:], self.W["rwkv_ln_w"][0:1, :].w(lambda a: a.partition_broadcast(128)))
        k.dma("sp", lnb[:], self.W["rwkv_ln_b"][0:1, :].w(lambda a: a.partition_broadcast(128)))
        o0 = [k.sb("o0_%d" % i, [128, D], F32) for i in range(2)]
        o1 = [k.sb("o1_%d" % i, [128, D], F32) for i in range(2)]
        vb = [k.sb("vb%d" % i, [128, D], BF16) for i in range(2)]
        gb = [k.sb("gb%d" % i, [128, D], BF16) for i in range(2)]
        bs = [k.sb("bs%d" % i, [128, 16], F32) for i in range(2)]
        sq = k.sb("sq", [128, D], F32)
        mean = k.sb("mean", [128, 16], F32)
        var = k.sb("var", [128, 16], F32)
        res = [k.sb("res%d" % i, [128, D], BF16) for i in range(2)]
        lat_tiles = [tt for tt in range(NT) if self.is_lat(tt)]

        def d_load(ix):
            tt = lat_tiles[ix]
            i = ix % 2
            k.dma("sp", o0[i][:], o_d[0, tt * 128:(tt + 1) * 128, :])
            k.dma("sp", o1[i][:], o_d[1, tt * 128:(tt + 1) * 128, :])
            k.dma("sp", vb[i][:], self.v_d[tt * 128:(tt + 1) * 128, :])
            k.dma("sp", gb[i][:], self.g_d[tt * 128:(tt + 1) * 128, :])
            k.dma("sp", bs[i][:], self.bs_d[tt * 128:(tt + 1) * 128, :])
        d_load(0)
        for ix, tt in enumerate(lat_tiles):
            i = ix % 2
            if ix + 1 < len(lat_tiles):
                d_load(ix + 1)
            o = o0[i]

            def h3(x):
                return x.rearrange("p (h k) -> p h k", k=64)

            def b16(x):
                return x.w(lambda a: a.unsqueeze(2).to_broadcast([128, 16, 64]))
            k.tt("pool", o[:], o[:], o1[i][:], ALU.add)
            k.op("dve", "tensor_reduce", out=mean[:], in_=h3(o[:]), axis=AX.X, op=ALU.add)
            k.ts("dve", mean[:], mean[:], 1.0 / 64, ALU.mult)
            k.tt("dve", h3(o[:]), h3(o[:]), b16(mean[:]), ALU.subtract)
            k.tt("pool", sq[:], o[:], o[:], ALU.mult)
            k.op("dve", "tensor_reduce", out=var[:], in_=h3(sq[:]), axis=AX.X, op=ALU.add)
            k.ts("dve", var[:], var[:], 1.0 / 64, ALU.mult, 64e-5, ALU.add)
            k.act(var[:], var[:], AF.Sqrt)
            k.op("dve", "reciprocal", out=var[:], in_=var[:])
            k.tt("dve", h3(o[:]), h3(o[:]), b16(var[:]), ALU.mult)
            k.tt("pool", o[:], o[:], lnw[:], ALU.mult)
            k.tt("pool", o[:], o[:], lnb[:], ALU.add)
            k.tt("dve", h3(sq[:]), h3(vb[i][:]), b16(bs[i][:]), ALU.mult)
            k.tt("pool", o[:], o[:], sq[:], ALU.add)
            k.tt("dve", res[i][:], o[:], gb[i][:], ALU.mult)
            k.dma("sp", self.attn_d.key(tt)[tt * 128:(tt + 1) * 128, :], res[i][:])


Prog.rwkv_scan = rwkv_scan
```

```python
import math
import os
import numpy as np
import ml_dtypes
import concourse.bass as bass
import concourse.mybir as mybir
from concourse.bass_utils import run_bass_kernel_spmd
from contextlib import ExitStack, contextmanager

F32 = mybir.dt.float32
BF16 = mybir.dt.bfloat16
ALU = mybir.AluOpType
AF = mybir.ActivationFunctionType
AX = mybir.AxisListType

D = 1024
HD = 64
NEXP = 32
EFF = 512
NEG = -30000.0
NORM_EPS = 1e-6


class Rec:
    __slots__ = ("w", "r", "dsem")

    def __init__(self):
        self.w = None
        self.r = {}
        self.dsem = None


class T:
    def __init__(self, k, t, name, is_dram=False):
        self.k = k
        self.t = t
        self.name = name
        self.is_dram = is_dram
        self.is_psum = False
        self.recs = {None: Rec()}

    def key(self, *keys):
        return TK(self, tuple(keys))

    def __getitem__(self, sl):
        return V(self.t[sl], self, None)


class TK:
    def __init__(self, tile, keys):
        self.tile = tile
        self.keys = keys

    def __getitem__(self, sl):
        return V(self.tile.t[sl], self.tile, self.keys)


class V:
    def __init__(self, ap, tile, keys):
        self.ap = ap
        self.tile = tile
        self.keys = keys

    def w(self, fn):
        return V(fn(self.ap), self.tile, self.keys)

    def rearrange(self, s, **kw):
        return V(self.ap.rearrange(s, **kw), self.tile, self.keys)

    def __getitem__(self, sl):
        return V(self.ap[sl], self.tile, self.keys)

    def bitcast(self, dt):
        return V(self.ap.bitcast(dt), self.tile, self.keys)


WRITE_KW = ("out", "accum_out")


class K:
    def __init__(self, nc, es, same_engine_sync=True):
        self.nc = nc
        self.es = es
        self.same = same_engine_sync
        self.eng = {"pe": nc.tensor, "act": nc.scalar, "dve": nc.vector, "pool": nc.gpsimd, "sp": nc.sync}
        self.sem = {}
        self.cnt = {}
        self.EPOCH = 30000
        for e in self.eng:
            self.sem[e] = [es.enter_context(nc.semaphore("s_" + e + "_0"))]
            self.cnt[e] = 0
        self.seen = {e: {} for e in self.eng}
        self.ndsem = 0
        self.ninstr = 0
        self.nwait = 0
        self.free_dsems = []
        self.alloc_es = es
        self.phase_tiles = None

    def sb(self, name, shape, dt):
        self.uid = getattr(self, "uid", 0) + 1
        name = "%s_u%d" % (name, self.uid)
        t = T(self, self.alloc_es.enter_context(self.nc.sbuf_tensor(name, list(shape), dt)), name)
        if self.phase_tiles is not None:
            self.phase_tiles.append(t)
        return t

    def ps(self, name, shape, dt=F32):
        self.uid = getattr(self, "uid", 0) + 1
        name = "%s_u%d" % (name, self.uid)
        t = T(self, self.alloc_es.enter_context(self.nc.psum_tensor(name, list(shape), dt)), name)
        t.is_psum = True
        if self.phase_tiles is not None:
            self.phase_tiles.append(t)
        return t

    def dram(self, name, shape, dt, kind="Internal"):
        return T(self, self.nc.dram_tensor(name, list(shape), dt, kind=kind).ap(), name, is_dram=True)

    @contextmanager
    def phase(self, name=""):
        old_es, old_tiles = self.alloc_es, self.phase_tiles
        with ExitStack() as pes:
            self.alloc_es = pes
            self.phase_tiles = []
            yield
            self.barrier()
            for t in self.phase_tiles:
                for r in t.recs.values():
                    if r.dsem is not None:
                        if self.cnt[r.dsem] < 40000:
                            self.free_dsems.append(r.dsem)
                        r.dsem = None
        self.alloc_es, self.phase_tiles = old_es, old_tiles

    def barrier(self):
        for e in self.eng:
            needs = {sk: v for sk, v in self.cnt.items() if v > 0}
            self._emit_waits(e, needs, force_same=False)

    def _recs_for(self, v):
        tile, keys = v.tile, v.keys
        if keys is None:
            return list(tile.recs.values())
        out = [tile.recs[None]]
        for kk in keys:
            if kk not in tile.recs:
                tile.recs[kk] = Rec()
            out.append(tile.recs[kk])
        return out

    def _own(self, v):
        tile, keys = v.tile, v.keys
        if keys is None:
            return [tile.recs[None]]
        out = []
        for kk in keys:
            if kk not in tile.recs:
                tile.recs[kk] = Rec()
            out.append(tile.recs[kk])
        return out

    @staticmethod
    def _need(needs, semkey, val):
        if val > needs.get(semkey, 0):
            needs[semkey] = val

    def _collect(self, reads, writes):
        needs = {}
        for v in reads:
            for r in self._recs_for(v):
                if r.w is not None:
                    self._need(needs, *r.w)
                if v.tile.is_psum:
                    for sk, val in r.r.items():
                        self._need(needs, sk, val)
        for v in writes:
            for r in self._recs_for(v):
                if r.w is not None:
                    self._need(needs, *r.w)
                for sk, val in r.r.items():
                    self._need(needs, sk, val)
        return needs

    def _emit_waits(self, e, needs, force_same=None):
        eng = self.eng[e]
        seen = self.seen[e]
        for sk, val in needs.items():
            if sk == e and (e == "pe" or not self.same or force_same is False):
                continue
            if seen.get(sk, 0) >= val:
                continue
            if sk in self.eng:
                p = (val - 1) // self.EPOCH
                eng.wait_ge(self.sem[sk][p], val - p * self.EPOCH)
            else:
                eng.wait_ge(self.sem[sk], val)
            self.nwait += 1
            seen[sk] = val

    def _commit(self, reads, writes, semkey, val):
        for v in writes:
            for r in self._own(v):
                r.w = (semkey, val)
                r.r = {}
            if v.keys is None:
                for kk, rr in v.tile.recs.items():
                    if kk is not None:
                        rr.w = (semkey, val)
                        rr.r = {}
        for v in reads:
            for r in self._own(v):
                if val > r.r.get(semkey, 0):
                    r.r[semkey] = val

    def op(self, e, method, *args, extra_reads=(), extra_writes=(), **kw):
        reads = list(extra_reads)
        writes = list(extra_writes)
        real = {}
        for name, a in kw.items():
            if isinstance(a, V):
                (writes if name in WRITE_KW else reads).append(a)
                real[name] = a.ap
            else:
                real[name] = a
        needs = self._collect(reads, writes)
        self._emit_waits(e, needs)
        ins = getattr(self.eng[e], method)(*args, **real)
        self.cnt[e] += 1
        p = (self.cnt[e] - 1) // self.EPOCH
        if p >= len(self.sem[e]):
            self.sem[e].append(self.es.enter_context(self.nc.semaphore("s_%s_%d" % (e, p))))
        ins.then_inc(self.sem[e][p], 1)
        self.ninstr += 1
        self._commit(reads, writes, e, self.cnt[e])
        return ins

    def _dsem_for(self, v):
        r = self._own(v)[0]
        if r.dsem is not None and self.cnt[r.dsem] > 50000:
            r.dsem = None
        if r.dsem is None:
            if self.free_dsems:
                r.dsem = self.free_dsems.pop()
            else:
                name = "d%d" % self.ndsem
                self.ndsem += 1
                self.sem[name] = self.es.enter_context(self.nc.semaphore(name))
                self.cnt[name] = 0
                r.dsem = name
        return r.dsem

    def dma(self, q, out, in_, **kw):
        reads = [in_]
        writes = [out]
        needs = self._collect(reads, writes)
        self._emit_waits(q, needs)
        owner = out if not out.tile.is_dram else (in_ if not in_.tile.is_dram else out)
        sk = self._dsem_for(owner)
        ins = self.eng[q].dma_start(out=out.ap, in_=in_.ap, **kw)
        self.cnt[sk] += 16
        ins.then_inc(self.sem[sk], 16)
        self.ninstr += 1
        self._commit(reads, writes, sk, self.cnt[sk])
        return ins

    def wait_all(self, e, views):
        needs = self._collect(views, [])
        self._emit_waits(e, needs)

    def mm(self, out, lhsT, rhs, start=True, stop=True, skip=False):
        if skip:
            return self.op("pe", "matmul", out=out, lhsT=lhsT, rhs=rhs, start=start, stop=stop, skip_group_check=True)
        return self.op("pe", "matmul", out=out, lhsT=lhsT, rhs=rhs, start=start, stop=stop)

    def tr(self, out, in_, ident):
        return self.op("pe", "transpose", out=out, in_=in_, identity=ident)

    def act(self, out, in_, func, **kw):
        return self.op("act", "activation", out=out, in_=in_, func=func, **kw)

    def tt(self, e, out, in0, in1, op):
        return self.op(e, "tensor_tensor", out=out, in0=in0, in1=in1, op=op)

    def ts(self, e, out, in0, s1, op0, s2=None, op1=None, **kw):
        if op1 is None:
            return self.op(e, "tensor_scalar", out=out, in0=in0, scalar1=s1, scalar2=None, op0=op0, **kw)
        return self.op(e, "tensor_scalar", out=out, in0=in0, scalar1=s1, scalar2=s2, op0=op0, op1=op1, **kw)

    def stt(self, e, out, in0, scalar, in1, op0, op1):
        return self.op(e, "scalar_tensor_tensor", out=out, in0=in0, scalar=scalar, in1=in1, op0=op0, op1=op1)

    def copy(self, e, out, in_):
        if e == "act":
            return self.act(out, in_, AF.Copy)
        return self.op(e, "tensor_copy", out=out, in_=in_)

    def memset(self, e, out, val):
        return self.op(e, "memset", out.ap, val, extra_writes=[out])


def rope_tables(S):
    GRID_W = 64
    rows = S // GRID_W
    row = np.repeat(np.arange(rows), GRID_W).astype(np.float32)
    col = np.tile(np.arange(GRID_W), rows).astype(np.float32)
    nf = HD // 4
    inv = (10000.0 ** (-np.arange(nf, dtype=np.float32) / nf)).astype(np.float32)
    ar, ac = row[:, None] * inv, col[:, None] * inv
    ang = np.concatenate([ar, ar, ac, ac], axis=-1).astype(np.float32)
    cos, sin = np.cos(ang).astype(np.float32), np.sin(ang).astype(np.float32)
    sgn = np.ones((64,), np.float32)
    sgn[0:16] = -1.0
    sgn[32:48] = -1.0
    return cos, (sin * sgn).astype(np.float32)


def make_consts(S):
    c = {}
    cos, ssin = rope_tables(S)
    c["rope_cos"] = cos
    c["rope_sin"] = ssin
    c["ident"] = np.eye(128, dtype=np.float32)
    kp = np.arange(128)[:, None]
    qp = np.arange(128)[None, :]
    mP = np.where(qp <= kp, 0.0, NEG).astype(np.float32)
    mN = np.where(kp <= qp, 0.0, NEG).astype(np.float32)
    c["maskP"] = np.tile(mP, (1, 4))
    c["maskN"] = np.tile(mN, (1, 4))
    c["pool_band"] = pool_band_consts()
    c.update(rwkv_consts())
    return c


class Prog:
    def __init__(self, NB, S, C, layers, depth_total=4):
        self.NB, self.S, self.C = NB, S, C
        self.TS, self.TC = S // 128, C // 128
        self.TB = self.TS + self.TC
        self.NT = NB * self.TB
        self.layers = layers
        self.lmap = {l: i for i, l in enumerate(layers)}
        self.depth_total = depth_total

    def cond_of(self, tt):
        b, j = divmod(tt, self.TB)
        return b if j < self.TS else 2

    def is_lat(self, tt):
        return (tt % self.TB) < self.TS

    def build(self, weight_shapes, const_shapes):
        nc = bass.Bass("TRN2", target_bir_lowering=False)
        self.nc = nc
        es = ExitStack()
        with es:
            k = K(nc, es)
            self.k = k
            NT = self.NT
            self.xin = k.dram("xin", [NT * 128, D], F32, kind="ExternalInput")
            self.cc = k.dram("cc", [3, D], F32, kind="ExternalInput")
            self.W = {n: k.dram(n, list(s), F32, kind="ExternalInput") for n, s in weight_shapes.items()}
            self.Cn = {n: k.dram(n, list(s), F32, kind="ExternalInput") for n, s in const_shapes.items()}
            self.y = k.dram("y", [self.NB * self.S, D], F32, kind="ExternalOutput")
            self.xs = k.dram("xs", [NT * 128, D], F32)
            self.attn_d = k.dram("attn_d", [NT * 128, D], BF16)
            self.modrows = k.dram("modrows", [self.depth_total, 3, 6 * D], F32)
            self.kstop = os.environ.get("KSTOP", "")
            self.mod_prep()
            src = self.xin
            for li, l in enumerate(self.layers):
                if self.kstop == "mod":
                    break
                m = l % 4
                last = l == self.depth_total - 1
                final = li == len(self.layers) - 1
                if m == 0:
                    self.win_attention(l, src, last)
                elif m == 1:
                    self.diff_attention(l, src, last)
                elif m == 2:
                    self.pool_mixer(l, src, last)
                else:
                    self.rwkv_features(l, src)
                    if self.kstop in ("rwA", "rwB"):
                        break
                    self.rwkv_scan(l, src)
                    if self.kstop in ("rwC", "rwD"):
                        break
                if self.kstop in ("p1", "p2"):
                    break
                self.post_and_moe(l, src, last, final, pool=(m == 2))
                src = self.xs
            self.dbg = []
            if os.environ.get("KDBG", ""):
                dl = [("attn_d", self.attn_d, [NT * 128, D], BF16), ("xs", self.xs, [NT * 128, D], F32),
                      ("modrows", self.modrows, [self.depth_total * 3, 6 * D], F32)]
                if hasattr(self, "o_d"):
                    dl += [("rw_v", self.v_d, [NT * 128, D], BF16), ("rw_g", self.g_d, [NT * 128, D], BF16),
                           ("rw_bs", self.bs_d, [NT * 128, 16], F32), ("rw_o0", self.o_d, [NT * 128, D], F32), ("rw_o1", self.o_d, [NT * 128, D], F32)]
                if hasattr(self, "o_d"):
                    dl += [("rw_F", self.F_d, [self.NB * 2 * self.TB * 128, 6 * 1024], BF16), ("rw_Wc", self.Wc_d, [self.NB * 2 * self.TB * 128, 8], F32)]
                for nm, t, shp, dt in dl:
                    o = k.dram("dbg_" + nm, shp, dt, kind="ExternalOutput")
                    if nm == "modrows":
                        src_v = t[:, :, :].rearrange("a b c -> (a b) c")
                    elif nm in ("rw_F", "rw_Wc"):
                        src_v = t[:, :, :, :, :].rearrange("a b c p f -> (a b c p) f")
                    elif nm in ("rw_o0", "rw_o1"):
                        src_v = t[int(nm[-1]), :, :]
                    else:
                        src_v = t[:, :]
                    k.dma("sp", o[:, :], src_v)
                    self.dbg.append(("dbg_" + nm, o))
                k.wait_all("sp", [o[:, :] for _, o in self.dbg])
            k.wait_all("sp", [self.y[:, :]])
            print("build: ninstr", k.ninstr, "nwait", k.nwait, "ndsem", k.ndsem)
        return nc

    def mod_prep(self):
        k = self.k
        with k.phase("modprep"):
            csT = k.sb("csT", [128, 8, 3], F32)
            for c_ in range(3):
                k.dma("sp", csT[:, :, c_:c_ + 1], self.cc[c_:c_ + 1, :].rearrange("c (k p) -> p k c", p=128), allow_slow_non_contiguous=True)
            k.act(csT[:], csT[:], AF.Silu)
            wbuf = [k.sb("modw%d" % i, [128, 8, 512], F32) for i in range(2)]
            bb = [k.sb("modb%d" % i, [3, 512], F32) for i in range(2)]
            ob = [k.sb("modo%d" % i, [3, 512], F32) for i in range(2)]
            ps = [k.ps("modps%d" % i, [128, 512], F32) for i in range(2)]
            it = 0
            for l in self.layers:
                for n in range(12):
                    i = it % 2
                    it += 1
                    k.dma("sp", wbuf[i][:], self.W["mod_w"][self.lmap[l], :, n * 512:(n + 1) * 512].rearrange("(k p) n -> p k n", p=128))
                    k.dma("sp", bb[i][:], self.W["mod_b"][self.lmap[l]:self.lmap[l] + 1, n * 512:(n + 1) * 512].w(lambda a: a.partition_broadcast(3)))
                    for kc in range(8):
                        k.mm(ps[i][0:3, :], csT[:, kc, :], wbuf[i][:, kc, :], start=(kc == 0), stop=(kc == 7))
                    k.tt("dve", ob[i][:], ps[i][0:3, :], bb[i][:], ALU.add)
                    k.dma("sp", self.modrows[l, :, n * 512:(n + 1) * 512], ob[i][:])

    def alloc_mod(self, l, sub, norm=True, gate=True):
        k = self.k
        st = {"cond": None, "l": l, "sub": sub}
        if norm:
            st["Gp"] = k.sb("modGp%d" % sub, [128, D], F32)
            st["sh"] = k.sb("modsh%d" % sub, [128, D], F32)
            st["g"] = k.sb("modg%d" % sub, [128, D], F32)
            k.dma("sp", st["g"][:], self.W["norm_g"][self.lmap[l], sub:sub + 1, :].w(lambda a: a.partition_broadcast(128)))
        if gate:
            st["gate"] = k.sb("modgt%d" % sub, [128, D], F32)
        return st

    def set_cond(self, st, cond, need_gate=True, need_norm=True):
        k = self.k
        l, sub = st["l"], st["sub"]
        need_norm = need_norm and st.get("cond_n") != cond
        need_gate = need_gate and st.get("cond_g") != cond
        if need_norm:
            st["cond_n"] = cond
        if need_gate:
            st["cond_g"] = cond
        base = sub * 3 * D

        def row(i):
            return self.modrows[l, cond:cond + 1, base + i * D: base + (i + 1) * D].w(lambda a: a.partition_broadcast(128))
        if need_norm:
            k.dma("sp", st["sh"][:], row(0))
            k.dma("sp", st["Gp"][:], row(1))
            k.stt("dve", st["Gp"][:], st["Gp"][:], 1.0, st["g"][:], ALU.add, ALU.mult)
        if need_gate:
            k.dma("sp", st["gate"][:], row(2))

    def norm_mod(self, xt, Gp, sh, out, scr, ss, eng2="pool"):
        k = self.k
        k.act(scr, xt, AF.Square, accum_out=ss)
        k.ts("dve", ss, ss, 1.0 / D, ALU.mult, NORM_EPS, ALU.add)
        k.act(ss, ss, AF.Sqrt)
        k.op("dve", "reciprocal", out=ss, in_=ss)
        if sh is None:
            k.stt("dve", out, xt, ss, Gp, ALU.mult, ALU.mult)
        else:
            k.stt("dve", scr, xt, ss, Gp, ALU.mult, ALU.mult)
            k.tt(eng2, out, scr, sh, ALU.add)

    def load_w_bf16(self, name, src_v, kchunks, n):
        k = self.k
        t = k.sb(name, [128, kchunks, n], BF16)
        for kc in range(kchunks):
            k.dma("pool", t[:, kc, :], src_v[kc * 128:(kc + 1) * 128, :])
        return t

    def transpose_tile(self, src, dst, ps, ident, nblk=8, eng="act"):
        k = self.k
        for kc in range(nblk):
            k.tr(ps[:, kc * 128:(kc + 1) * 128], src[:, kc * 128:(kc + 1) * 128], ident)
        k.copy(eng, dst, ps)

    def rope(self, ps_v, nh, cos, sin, scr_v, out_v):
        k = self.k
        X3 = ps_v.rearrange("p (h d) -> p h d", d=64)
        S3 = scr_v.rearrange("p (h d) -> p h d", d=64)
        cb = cos.w(lambda a: a.unsqueeze(1).to_broadcast([128, nh, 64]))
        k.tt("dve", S3, X3, cb, ALU.mult)
        X5 = ps_v.rearrange("p (h a r i) -> p h a r i", a=2, r=2, i=16)
        s4 = sin.rearrange("p (a r i) -> p a r i", a=2, r=2, i=16)
        O5 = out_v.rearrange("p h (a r i) -> p h a r i", a=2, r=2, i=16)
        for r in range(2):
            sb_ = s4[:, :, r, :].w(lambda a: a.unsqueeze(1).to_broadcast([128, nh, 2, 16]))
            k.tt("dve", O5[:, :, :, r, :], X5[:, :, :, 1 - r, :], sb_, ALU.mult)
        k.tt("pool", out_v, out_v, S3, ALU.add)

    def win_attention(self, l, src, last):
        k = self.k
        NB, TS, TC, TB, NT = self.NB, self.TS, self.TC, self.TB, self.NT
        W_ = TB * 128
        G = 4
        qT_d = k.dram("win_qT", [NB, 16, 65, W_], BF16)
        kT_d = k.dram("win_kT", [NB, 4, 65, W_], BF16)
        with k.phase("win"):
            v_all = k.sb("v_all", [128, NT, 4, 65], BF16)
            qn_all = k.sb("qn_all", [128, NT, 16], F32)
            nk8 = k.sb("nk8", [128, 1], F32)
            ident_bf = k.sb("ident_bf", [128, 128], BF16)
            k.dma("pool", ident_bf[:], self.Cn["ident"][:, :])
            k.memset("pool", v_all[:, :, :, 64:65], 1.0)
            with k.phase("winP1"):
                ident_f = k.sb("ident_f", [128, 128], F32)
                k.dma("sp", ident_f[:], self.Cn["ident"][:, :])
                wqkv = self.load_w_bf16("wqkv", self.W["win_w_qkv"][0], 8, 1536)
                st1 = self.alloc_mod(l, 0, gate=False)
                xt = [k.sb("xt%d" % i, [128, D], F32) for i in range(2)]
                cs = [k.sb("cos%d" % i, [128, 64], F32) for i in range(2)]
                sn = [k.sb("sin%d" % i, [128, 64], F32) for i in range(2)]
                scr = k.sb("scr", [128, D], F32)
                scr2 = k.sb("scr2", [128, D], F32)
                ss = k.sb("ss", [128, 1], F32)
                hb = k.sb("hb", [128, D], BF16)
                hT = k.sb("hT", [128, 8, 128], BF16)
                q_aug = [k.sb("q_aug%d" % i, [128, 16, 65], BF16) for i in range(2)]
                k_aug = [k.sb("k_aug%d" % i, [128, 4, 65], BF16) for i in range(2)]
                for i in range(2):
                    k.memset("pool", k_aug[i][:, :, 64:65], 0.0)
                qn2 = k.sb("qn2", [128, 16], F32)
                qnb = k.sb("qnb", [128, 16], BF16)
                k2 = k.sb("k2", [128, 4], F32)
                kmax2 = k.sb("kmax2", [128, 4], F32)
                k.memset("dve", kmax2[:], 0.0)
                qT_st = [k.sb("qT_st%d" % i, [65, 16, 128], BF16) for i in range(2)]
                kT_st = [k.sb("kT_st%d" % i, [65, 4, 128], BF16) for i in range(2)]
                ps_tr = k.ps("ps_tr", [128, D], BF16)
                ps_q = k.ps("ps_q", [128, D], F32)
                ps_kv = k.ps("ps_kv", [128, 512], F32)
                ps_qT = k.ps("ps_qT", [128, 2048], BF16)
                ps_kT = k.ps("ps_kT", [128, 1024], BF16)
                def p1_load(tt):
                    b, j = divmod(tt, TB)
                    i = tt % 2
                    k.dma("sp", xt[i][:], src.key(tt)[tt * 128:(tt + 1) * 128, :])
                    if j < TS:
                        k.dma("sp", cs[i][:], self.Cn["rope_cos"][j * 128:(j + 1) * 128, :])
                        k.dma("sp", sn[i][:], self.Cn["rope_sin"][j * 128:(j + 1) * 128, :])
                p1_load(0)
                for tt in range(NT):
                    b, j = divmod(tt, TB)
                    lat = j < TS
                    i = tt % 2
                    if tt + 1 < NT:
                        p1_load(tt + 1)
                    self.set_cond(st1, self.cond_of(tt), need_gate=False)
                    self.norm_mod(xt[i][:], st1["Gp"][:], st1["sh"][:], hb[:], scr[:], ss[:])
                    self.transpose_tile(hb[:], hT[:].rearrange("p k t -> p (k t)"), ps_tr[:], ident_bf[:])
                    for n in range(3):
                        o = ps_q[:, n * 512:(n + 1) * 512] if n < 2 else ps_kv[:, :]
                        for kc in range(8):
                            k.mm(o, hT[:, kc, :], wqkv[:, kc, n * 512:(n + 1) * 512], start=(kc == 0), stop=(kc == 7))
                    k.act(scr[:], ps_q[:], AF.Square)
                    k.op("dve", "tensor_reduce", out=qn2[:], in_=scr[:].rearrange("p (h d) -> p h d", d=64), axis=AX.X, op=ALU.add)
                    k.act(qn2[:], qn2[:], AF.Sqrt)
                    k.copy("dve", qnb[:], qn2[:])
                    k.copy("dve", qn_all[:, tt, :], qnb[:])
                    k.ts("dve", q_aug[i][:, :, 64:65], qnb[:].w(lambda a: a.unsqueeze(2)), -1.0, ALU.mult)
                    k.act(scr2[:, 0:256], ps_kv[:, 0:256], AF.Square)
                    k.op("dve", "tensor_reduce", out=k2[:], in_=scr2[:, 0:256].rearrange("p (h d) -> p h d", d=64), axis=AX.X, op=ALU.add)
                    k.tt("dve", kmax2[:], kmax2[:], k2[:], ALU.max)
                    if lat:
                        self.rope(ps_q[:], 16, cs[i][:], sn[i][:], scr[:], q_aug[i][:, :, 0:64])
                        self.rope(ps_kv[:, 0:256], 4, cs[i][:], sn[i][:], scr2[:, 0:256], k_aug[i][:, :, 0:64])
                    else:
                        k.copy("dve", q_aug[i][:, :, 0:64], ps_q[:].rearrange("p (h d) -> p h d", d=64))
                        k.copy("dve", k_aug[i][:, :, 0:64], ps_kv[:, 0:256].rearrange("p (h d) -> p h d", d=64))
                    k.copy("act", v_all[:, tt, :, 0:64], ps_kv[:, 256:512].rearrange("p (h d) -> p h d", d=64))
                    for h in range(16):
                        k.tr(ps_qT[0:65, h * 128:(h + 1) * 128], q_aug[i][:, h, :], ident_bf[:])
                    k.copy("act", qT_st[i][:].rearrange("r h t -> r (h t)"), ps_qT[0:65, :])
                    for g in range(4):
                        k.tr(ps_kT[0:65, g * 128:(g + 1) * 128], k_aug[i][:, g, :], ident_bf[:])
                    k.copy("dve", kT_st[i][:].rearrange("r h t -> r (h t)"), ps_kT[0:65, 0:512])
                    k.dma("sp", qT_d.key(tt)[b, :, :, j * 128:(j + 1) * 128].rearrange("h r t -> r h t"), qT_st[i][:])
                    k.dma("sp", kT_d.key(tt)[b, :, :, j * 128:(j + 1) * 128].rearrange("h r t -> r h t"), kT_st[i][:])
                km1 = k.sb("km1", [128, 1], F32)
                k.op("dve", "tensor_reduce", out=km1[:], in_=kmax2[:], axis=AX.X, op=ALU.max)
                k.tr(ps_kv[0:1, 0:128], km1[:], ident_f[:])
                kmx = k.sb("kmx", [1, 1], F32)
                kmxb = k.sb("kmxb", [1, 1], BF16)
                k.op("dve", "tensor_reduce", out=kmx[:], in_=ps_kv[0:1, 0:128], axis=AX.X, op=ALU.max)
                k.act(kmx[:], kmx[:], AF.Sqrt)
                k.copy("dve", kmxb[:], kmx[:])
                k.copy("dve", kmx[:], kmxb[:])
                ones_f = k.sb("ones_f", [1, 128], F32)
                k.memset("dve", ones_f[:], 1.0)
                k.mm(ps_kv[:, 128:129], ones_f[0:1, :], kmx[0:1, 0:1])
                k.ts("dve", nk8[:], ps_kv[:, 128:129], -0.125, ALU.mult)
                kmrow = k.sb("kmrow", [1, W_], BF16)
                k.memset("pool", kmrow[:], 1.0)
                k.ts("dve", kmrow[:], kmrow[:], kmx[0:1, 0:1], ALU.mult)
                for b in range(NB):
                    for g in range(4):
                        k.dma("sp", kT_d[b, g, 64:65, :], kmrow[:])
            if self.kstop == "p1":
                return
            with k.phase("winP2"):
                maskP = k.sb("maskP", [128, 512], BF16)
                maskN = k.sb("maskN", [128, 512], BF16)
                k.dma("pool", maskP[:], self.Cn["maskP"][:, :])
                k.dma("pool", maskN[:], self.Cn["maskN"][:, :])
                masks = {"P": maskP, "N": maskN}
                sinkb = k.sb("sinkb", [128, 16], F32)
                k.dma("sp", sinkb[:], self.W["win_sink"][0:1, :].w(lambda a: a.partition_broadcast(128)))
                qT_g = [k.sb("qT_g%d" % i, [65, 4, W_], BF16) for i in range(2)]
                kT_g = [k.sb("kT_g%d" % i, [65, W_], BF16) for i in range(2)]
                sk = [k.sb("sk%d" % i, [128, TB, 4], F32) for i in range(2)]
                ET = [k.sb("ET%d" % i, [128, 512], BF16) for i in range(3)]
                den = [k.sb("den%d" % i, [128, 4], F32) for i in range(2)]
                ao = [k.sb("ao%d" % i, [128, 4, 64], BF16) for i in range(2)]
                ps_s = [k.ps("ps_s%d" % i, [128, 512], F32) for i in range(4)]
                ps_o = [k.ps("ps_o%d" % i, [128, 512], F32) for i in range(2)]
                it = 0
                def w2_load(b, g):
                    bi = (b * G + g) % 2
                    k.dma("sp", qT_g[bi][:], qT_d[b, 4 * g:4 * g + 4, :, :].rearrange("h r t -> r h t"))
                    k.dma("sp", kT_g[bi][:], kT_d[b, g, :, :])
                bgs = [(b, g) for b in range(NB) for g in range(G)]
                w2_load(*bgs[0])
                for bgi, (b, g) in enumerate(bgs):
                    if True:
                        bi = (b * G + g) % 2
                        if bgi + 1 < len(bgs):
                            w2_load(*bgs[bgi + 1])
                        k.ts("dve", sk[bi][:], qn_all[:, b * TB:(b + 1) * TB, 4 * g:4 * g + 4], nk8[:, 0:1], ALU.mult)
                        k.tt("dve", sk[bi][:], sk[bi][:], sinkb[:, 4 * g:4 * g + 4].w(lambda a: a.unsqueeze(1).to_broadcast([128, TB, 4])), ALU.add)
                        k.act(sk[bi][:], sk[bi][:], AF.Exp)
                        for n in range(TB):
                            lat = n < TS
                            kbs = []
                            if lat:
                                if n >= 1:
                                    kbs.append((n - 1, "P"))
                                kbs.append((n, None))
                                if n + 1 < TS:
                                    kbs.append((n + 1, "N"))
                            kbs += [(TS + c, None) for c in range(TC)]
                            po = ps_o[n % 2]

                            def w_qk(i_):
                                jb, mk = kbs[i_]
                                pss = ps_s[(it + i_) % 4]
                                k.mm(pss[:], kT_g[bi][:, jb * 128:(jb + 1) * 128], qT_g[bi][:, :, n * 128:(n + 1) * 128], start=True, stop=(mk is None))
                                if mk is not None:
                                    k.mm(pss[:], ident_bf[:], masks[mk][:], start=False, stop=True)

                            def w_rest(i_):
                                jb, mk = kbs[i_]
                                pss = ps_s[(it + i_) % 4]
                                et = ET[(it + i_) % 3]
                                k.act(et[:], pss[:], AF.Exp, scale=0.125)
                                for h in range(4):
                                    k.mm(po[:, h * 65:(h + 1) * 65], et[:, h * 128:(h + 1) * 128], v_all[:, b * TB + jb, g, :],
                                         start=(i_ == 0 and h == 0), stop=(i_ == len(kbs) - 1), skip=True)
                            for i_ in range(min(2, len(kbs))):
                                w_qk(i_)
                            for i_ in range(len(kbs)):
                                if i_ + 2 < len(kbs):
                                    w_qk(i_ + 2)
                                w_rest(i_)
                            it += len(kbs)
                            po3 = po[:, 0:260].rearrange("p (h c) -> p h c", c=65)
                            dn = den[n % 2]
                            k.tt("dve", dn[:], po3[:, :, 64:65].rearrange("p h c -> p (h c)"), sk[bi][:, n, :], ALU.add)
                            k.op("dve", "reciprocal", out=dn[:], in_=dn[:])
                            k.tt("dve", ao[n % 2][:], po3[:, :, 0:64], dn[:].w(lambda a: a.unsqueeze(2).to_broadcast([128, 4, 64])), ALU.mult)
                            tt = b * TB + n
                            k.dma("sp", self.attn_d.key(tt)[tt * 128:(tt + 1) * 128, g * 256:(g + 1) * 256], ao[n % 2][:].rearrange("p h d -> p (h d)"))

    def post_and_moe(self, l, src, last, final, pool=False, wo_name=None):
        k = self.k
        NB, TS, TC, TB, NT = self.NB, self.TS, self.TC, self.TB, self.NT
        m = l % 4
        wo_src = {0: "win_w_o", 1: "diff_w_o", 3: "rwkv_w_o"}.get(m)
        tiles = [tt for tt in range(NT) if (not last) or self.is_lat(tt)]
        GSZ = 12
        groups = [tiles[i:i + GSZ] for i in range(0, len(tiles), GSZ)]
        with k.phase("post"):
            ident_bf = k.sb("ident_bf", [128, 128], BF16)
            k.dma("pool", ident_bf[:], self.Cn["ident"][:, :])
            ident_f = k.sb("ident_f", [128, 128], F32)
            k.dma("sp", ident_f[:], self.Cn["ident"][:, :])
            wo = None if pool else self.load_w_bf16("wo", self.W[wo_src][0], 8, D)
            yt_ = [k.sb("yt%d" % i, [128, D], F32) for i in range(2)] if pool else None
            wr = k.sb("wr", [128, 8, 36], F32)
            k.dma("sp", wr[:, :, 0:4], self.W["moe_wg"][self.lmap[l]].rearrange("(k p) n -> p k n", p=128))
            k.dma("sp", wr[:, :, 4:36], self.W["moe_we"][self.lmap[l]].rearrange("(k p) n -> p k n", p=128))
            rb = k.sb("rb", [128, 36], F32)
            k.dma("sp", rb[:, 0:4], self.W["moe_bg"][self.lmap[l]:self.lmap[l] + 1, :].w(lambda a: a.partition_broadcast(128)))
            k.dma("sp", rb[:, 4:36], self.W["moe_be"][self.lmap[l]:self.lmap[l] + 1, :].w(lambda a: a.partition_broadcast(128)))
            st1 = self.alloc_mod(l, 0, norm=False)
            st2 = self.alloc_mod(l, 1)
            if final:
                fg = k.sb("fg", [128, D], F32)
                k.dma("sp", fg[:], self.W["final_g"][:].w(lambda a: a.unsqueeze(0).partition_broadcast(128)))
            at = [k.sb("at%d" % i, [128, D], BF16) for i in range(2)]
            xt = [k.sb("xt%d" % i, [128, D], F32) for i in range(2)]
            aT = k.sb("aT", [128, 8, 128], BF16)
            scr = k.sb("scr", [128, D], F32)
            h2 = k.sb("h2", [128, D], F32)
            ss = k.sb("ss", [128, 1], F32)
            h2T32 = k.sb("h2T32", [128, 8, 128], F32)
            h2T = k.sb("h2T", [128, 8, GSZ * 128], BF16)
            wt = k.sb("wt", [128, GSZ, 32], F32)
            acc = k.sb("acc", [128, GSZ, D], F32)
            lg = k.sb("lg", [128, 36], F32)
            sm = {n: k.sb("r_" + n, [128, w], F32) for n, w in
                  [("gmax", 1), ("ngmax", 1), ("oh", 4), ("eg", 4), ("gsum", 1), ("les", 32), ("lsel", 8), ("emax", 1), ("nemax", 1),
                   ("ee", 8), ("mk1", 8), ("ee2", 8), ("m2", 1), ("mk2", 8), ("wsel", 8), ("fac", 1)]}
            wup = [k.sb("wup%d" % i, [128, 8, D], BF16) for i in range(2)]
            wdn = [k.sb("wdn%d" % i, [128, 4, D], BF16) for i in range(2)]
            sa = [k.sb("sa%d" % i, [128, 512], F32) for i in range(2)]
            actT = [k.sb("actT%d" % i, [128, 4, 512], BF16) for i in range(2)]
            ps_tr = k.ps("ps_tr", [128, D], BF16)
            ps_y = k.ps("ps_y", [128, D], F32)
            ps_ab = [k.ps("ps_ab%d" % i, [128, 512], F32) for i in range(3)]
            ps_d = [k.ps("ps_d%d" % i, [128, 512], F32) for i in range(2)]

            def load_expert(e):
                wu, wd = wup[e % 2], wdn[e % 2]
                for kc in range(8):
                    k.dma("pool", wu[:, kc, :], self.W["moe_w_up"][self.lmap[l], e, kc * 128:(kc + 1) * 128, :])
                for kc in range(4):
                    k.dma("pool", wd[:, kc, :], self.W["moe_w_down"][self.lmap[l], e, kc * 128:(kc + 1) * 128, :])

            for G in groups:
                ng = len(G)
                if self.kstop == "post0":
                    return
                load_expert(0)

                def g_load(ti):
                    tt = G[ti]
                    if pool:
                        k.dma("sp", yt_[ti % 2][:], self.y_d.key(tt)[tt * 128:(tt + 1) * 128, :])
                    else:
                        k.dma("sp", at[ti % 2][:], self.attn_d.key(tt)[tt * 128:(tt + 1) * 128, :])
                    k.dma("sp", xt[ti % 2][:], src.key(tt)[tt * 128:(tt + 1) * 128, :])
                g_load(0)
                for ti, tt in enumerate(G):
                    if ti + 1 < ng:
                        g_load(ti + 1)
                    cond = self.cond_of(tt)
                    self.set_cond(st1, cond, need_norm=False)
                    self.set_cond(st2, cond, need_gate=False)
                    a_ = at[ti % 2]
                    x_ = xt[ti % 2]
                    if pool:
                        k.tt("dve", scr[:], yt_[ti % 2][:], st1["gate"][:], ALU.mult)
                    else:
                        self.transpose_tile(a_[:], aT[:].rearrange("p k t -> p (k t)"), ps_tr[:], ident_bf[:])
                        for n in range(2):
                            for kc in range(8):
                                k.mm(ps_y[:, n * 512:(n + 1) * 512], aT[:, kc, :], wo[:, kc, n * 512:(n + 1) * 512], start=(kc == 0), stop=(kc == 7))
                        k.tt("dve", scr[:], ps_y[:], st1["gate"][:], ALU.mult)
                    k.tt("pool", x_[:], scr[:], x_[:], ALU.add)
                    k.dma("sp", self.xs.key(tt)[tt * 128:(tt + 1) * 128, :], x_[:])
                    if self.kstop == "post1a":
                        return
                    self.norm_mod(x_[:], st2["Gp"][:], st2["sh"][:], h2[:], scr[:], ss[:])
                    if self.kstop == "post1b1":
                        return
                    for kc in range(8):
                        k.tr(ps_y[:, kc * 128:(kc + 1) * 128], h2[:, kc * 128:(kc + 1) * 128], ident_f[:])
                    if self.kstop == "post1b2":
                        return
                    k.copy("act", h2T32[:].rearrange("p k t -> p (k t)"), ps_y[:])
                    if self.kstop == "post1b3":
                        return
                    k.copy("pool", h2T[:, :, ti * 128:(ti + 1) * 128], h2T32[:])
                    if self.kstop == "post1b":
                        return
                    pr = ps_ab[2]
                    for kc in range(8):
                        k.mm(pr[:, 0:36], h2T32[:, kc, :], wr[:, kc, :], start=(kc == 0), stop=(kc == 7))
                    if self.kstop == "post1c":
                        return
                    self.router(pr[:, 0:36], rb, lg, sm, wt[:, ti, :])
                if self.kstop == "post1":
                    return
                ntok = ng * 128
                nsb = (ng + 3) // 4
                cnt_ab = 0
                cnt_d = 0
                for e in range(NEXP):
                    wu, wd = wup[e % 2], wdn[e % 2]
                    if e + 1 < NEXP:
                        load_expert(e + 1)
                    for sb_ in range(nsb):
                        t0 = sb_ * 4
                        nt_ = min(4, ng - t0)
                        w_ = nt_ * 128
                        aT_ = actT[(e * nsb + sb_) % 2]
                        for j in range(4):
                            pa = ps_ab[cnt_ab % 3]
                            pb = ps_ab[(cnt_ab + 1) % 3]
                            cnt_ab += 2
                            for kc in range(8):
                                k.mm(pa[:, 0:w_], wu[:, kc, j * 128:(j + 1) * 128], h2T[:, kc, t0 * 128:t0 * 128 + w_], start=(kc == 0), stop=(kc == 7))
                            for kc in range(8):
                                k.mm(pb[:, 0:w_], wu[:, kc, 512 + j * 128:512 + (j + 1) * 128], h2T[:, kc, t0 * 128:t0 * 128 + w_], start=(kc == 0), stop=(kc == 7))
                            s_ = sa[j % 2]
                            k.act(s_[:, 0:w_], pa[:, 0:w_], AF.Silu)
                            k.tt("dve", aT_[:, j, 0:w_], s_[:, 0:w_], pb[:, 0:w_], ALU.mult)
                        for t in range(nt_):
                            ti = t0 + t
                            for half in range(2):
                                pd = ps_d[cnt_d % 2]
                                cnt_d += 1
                                for j in range(4):
                                    k.mm(pd[:], aT_[:, j, t * 128:(t + 1) * 128], wd[:, j, half * 512:(half + 1) * 512], start=(j == 0), stop=(j == 3))
                                av = acc[:, ti, half * 512:(half + 1) * 512]
                                if e == 0:
                                    k.ts("dve", av, pd[:], wt[:, ti, e:e + 1], ALU.mult)
                                else:
                                    k.stt("dve", av, pd[:], wt[:, ti, e:e + 1], av, ALU.mult, ALU.add)
                if self.kstop == "post2":
                    return
                def f_load(ti):
                    tt = G[ti]
                    k.dma("sp", xt[ti % 2][:], self.xs.key(tt)[tt * 128:(tt + 1) * 128, :])
                f_load(0)
                for ti, tt in enumerate(G):
                    if ti + 1 < ng:
                        f_load(ti + 1)
                    cond = self.cond_of(tt)
                    self.set_cond(st2, cond, need_norm=False)
                    x_ = xt[ti % 2]
                    k.tt("pool", scr[:], acc[:, ti, :], st2["gate"][:], ALU.mult)
                    k.tt("pool", x_[:], scr[:], x_[:], ALU.add)
                    if not final:
                        k.dma("sp", self.xs.key(tt)[tt * 128:(tt + 1) * 128, :], x_[:])
                    elif self.is_lat(tt):
                        b, j = divmod(tt, TB)
                        self.norm_mod(x_[:], fg[:], None, h2[:], scr[:], ss[:])
                        r0 = (b * TS + j) * 128
                        k.dma("sp", self.y.key(tt)[r0:r0 + 128, :], h2[:])

    def router(self, pr, rb, lg, sm, wt_out):
        k = self.k
        k.tt("dve", lg[:], pr, rb[:], ALU.add)
        lgg = lg[:, 0:4]
        le = lg[:, 4:36]
        k.op("dve", "tensor_reduce", out=sm["gmax"][:], in_=lgg, axis=AX.X, op=ALU.max)
        k.ts("dve", sm["ngmax"][:], sm["gmax"][:], -1.0, ALU.mult)
        k.ts("dve", sm["oh"][:], lgg, sm["gmax"][:, 0:1], ALU.is_equal)
        k.act(sm["eg"][:], lgg, AF.Exp, bias=sm["ngmax"][:, 0:1], accum_out=sm["gsum"][:])
        k.tt("dve", sm["les"][:].rearrange("p (g e) -> p g e", e=8), le.rearrange("p (g e) -> p g e", e=8),
             sm["oh"][:].w(lambda a: a.unsqueeze(2).to_broadcast([128, 4, 8])), ALU.mult)
        k.op("dve", "tensor_reduce", out=sm["lsel"][:], in_=sm["les"][:].rearrange("p (g e) -> p e g", e=8), axis=AX.X, op=ALU.add)
        k.op("dve", "tensor_reduce", out=sm["emax"][:], in_=sm["lsel"][:], axis=AX.X, op=ALU.max)
        k.ts("dve", sm["nemax"][:], sm["emax"][:], -1.0, ALU.mult)
        k.act(sm["ee"][:], sm["lsel"][:], AF.Exp, bias=sm["nemax"][:, 0:1])
        k.ts("dve", sm["mk1"][:], sm["lsel"][:], sm["emax"][:, 0:1], ALU.is_equal)
        k.stt("dve", sm["ee2"][:], sm["mk1"][:], -2.0, sm["ee"][:], ALU.mult, ALU.add)
        k.op("dve", "tensor_reduce", out=sm["m2"][:], in_=sm["ee2"][:], axis=AX.X, op=ALU.max)
        k.ts("dve", sm["mk2"][:], sm["ee2"][:], sm["m2"][:, 0:1], ALU.is_equal)
        k.stt("dve", sm["wsel"][:], sm["mk2"][:], sm["m2"][:, 0:1], sm["mk1"][:], ALU.mult, ALU.add)
        k.ts("dve", sm["fac"][:], sm["m2"][:], 1.0, ALU.add, sm["gsum"][:, 0:1], ALU.mult)
        k.op("dve", "reciprocal", out=sm["fac"][:], in_=sm["fac"][:])
        k.ts("dve", sm["wsel"][:], sm["wsel"][:], sm["fac"][:, 0:1], ALU.mult)
        k.tt("dve", wt_out.rearrange("p (g e) -> p g e", e=8),
             sm["oh"][:].w(lambda a: a.unsqueeze(2).to_broadcast([128, 4, 8])),
             sm["wsel"][:].w(lambda a: a.unsqueeze(1).to_broadcast([128, 4, 8])), ALU.mult)


WEIGHT_NAMES = ["mod_w", "mod_b", "norm_g", "final_g", "win_w_qkv", "win_sink", "win_w_o",
                "diff_w_qkv", "diff_lambda", "diff_subln_g", "diff_w_o",
                "pool_w_group", "pool_b_group", "pool_scale",
                "rwkv_mu", "rwkv_w_rkv", "rwkv_w0", "rwkv_w_a1", "rwkv_w_a2", "rwkv_a0", "rwkv_a_a1", "rwkv_a_a2",
                "rwkv_g1", "rwkv_g2", "rwkv_k_k", "rwkv_k_a", "rwkv_r_k", "rwkv_ln_w", "rwkv_ln_b", "rwkv_w_o",
                "moe_wg", "moe_bg", "moe_we", "moe_be", "moe_w_up", "moe_w_down"]


def run_model(inputs, layers, n_cores, NB):
    x = np.asarray(inputs["x"], np.float32)
    ctx = np.asarray(inputs["ctx"], np.float32)
    c = np.asarray(inputs["c"], np.float32)
    c_ctx = np.asarray(inputs["c_ctx"], np.float32)
    B, S, _ = x.shape
    C = ctx.shape[1]
    assert B == n_cores * NB
    weights = {n: np.asarray(inputs[n], np.float32) for n in WEIGHT_NAMES}
    if list(layers) != [0, 1, 2, 3]:
        for n in ["mod_w", "mod_b", "norm_g", "moe_wg", "moe_we", "moe_bg", "moe_be", "moe_w_up", "moe_w_down"]:
            weights[n] = weights[n][list(layers)]
    weights = {n: np.ascontiguousarray(w) for n, w in weights.items()}
    consts = make_consts(S)
    prog = Prog(NB, S, C, layers)
    nc = prog.build({n: w.shape for n, w in weights.items()}, {n: v.shape for n, v in consts.items()})
    in_maps = []
    for i in range(n_cores):
        bs = range(i * NB, (i + 1) * NB)
        xin = np.concatenate([np.concatenate([x[b], ctx[b]], axis=0) for b in bs], axis=0)
        cc = np.stack([c[b] for b in bs] + [c_ctx] * (3 - NB), axis=0) if NB == 2 else None
        m = {"xin": np.ascontiguousarray(xin), "cc": np.ascontiguousarray(cc)}
        m.update(weights)
        m.update(consts)
        in_maps.append(m)
    res = run_bass_kernel_spmd(nc, in_maps, core_ids=list(range(n_cores)))
    global LAST_DBG
    LAST_DBG = [{n: np.asarray(r[n]) for n, _ in (prog.dbg + getattr(prog, "dbg2", []))} for r in res.results]
    out = np.concatenate([r["y"].reshape(NB, S, D) for r in res.results], axis=0)
    return out.astype(np.float32)


def kernel(**inputs):
    return run_model(inputs, [0, 1, 2, 3], 8, 2)


def diff_attention(self, l, src, last):
    k = self.k
    NB, TS, TC, TB, NT = self.NB, self.TS, self.TC, self.TB, self.NT
    W_ = TB * 128
    lam_init = 0.8 - 0.6 * math.exp(-0.3 * l)
    qT_d = k.dram("dif_qT", [NB, 16, 65, W_], BF16)
    kT_d = k.dram("dif_kT", [NB, 16, 65, W_], BF16)
    v_d = k.dram("dif_v", [NB, 8, 128, TB, 129], BF16)
    with k.phase("diffP1"):
        ident_bf = k.sb("ident_bf", [128, 128], BF16)
        k.dma("pool", ident_bf[:], self.Cn["ident"][:, :])
        ident_f = k.sb("ident_f", [128, 128], F32)
        k.dma("sp", ident_f[:], self.Cn["ident"][:, :])
        wqkv = self.load_w_bf16("wqkv", self.W["diff_w_qkv"][0], 8, 3072)
        st1 = self.alloc_mod(l, 0, gate=False)
        xt = [k.sb("xt%d" % i, [128, D], F32) for i in range(2)]
        cs = [k.sb("cos%d" % i, [128, 64], F32) for i in range(2)]
        sn = [k.sb("sin%d" % i, [128, 64], F32) for i in range(2)]
        scr = k.sb("scr", [128, D], F32)
        ss = k.sb("ss", [128, 1], F32)
        hb = k.sb("hb", [128, D], BF16)
        hT = k.sb("hT", [128, 8, 128], BF16)
        qk_aug = [k.sb("qk_aug%d" % i, [128, 16, 65], BF16) for i in range(2)]
        k.memset("pool", qk_aug[1][:, :, 64:65], 0.0)
        v_aug = [k.sb("v_aug%d" % i, [128, 8, 129], BF16) for i in range(2)]
        for i in range(2):
            k.memset("pool", v_aug[i][:, :, 128:129], 1.0)
        n2 = k.sb("n2", [128, 16], F32)
        nb_ = k.sb("nb_", [128, 16], BF16)
        kmax2 = k.sb("kmax2", [128, 16], F32)
        k.memset("dve", kmax2[:], 0.0)
        T_st = [k.sb("T_st%d" % i, [65, 16, 128], BF16) for i in range(2)]
        ps_tr = k.ps("ps_tr", [128, D], BF16)
        ps_a = [k.ps("ps_a%d" % i, [128, D], F32) for i in range(2)]
        ps_T = k.ps("ps_T", [128, 2048], BF16)

        def p1_load(tt):
            b, j = divmod(tt, TB)
            i = tt % 2
            k.dma("sp", xt[i][:], src.key(tt)[tt * 128:(tt + 1) * 128, :])
            if j < TS:
                k.dma("sp", cs[i][:], self.Cn["rope_cos"][j * 128:(j + 1) * 128, :])
                k.dma("sp", sn[i][:], self.Cn["rope_sin"][j * 128:(j + 1) * 128, :])
        p1_load(0)
        for tt in range(NT):
            b, j = divmod(tt, TB)
            lat = j < TS
            i = tt % 2
            if tt + 1 < NT:
                p1_load(tt + 1)
            self.set_cond(st1, self.cond_of(tt), need_gate=False)
            self.norm_mod(xt[i][:], st1["Gp"][:], st1["sh"][:], hb[:], scr[:], ss[:])
            self.transpose_tile(hb[:], hT[:].rearrange("p k t -> p (k t)"), ps_tr[:], ident_bf[:])
            for part in range(3):
                pa = ps_a[part % 2]
                for n in range(2):
                    c0 = part * 1024 + n * 512
                    for kc in range(8):
                        k.mm(pa[:, n * 512:(n + 1) * 512], hT[:, kc, :], wqkv[:, kc, c0:c0 + 512], start=(kc == 0), stop=(kc == 7))
                if part == 2:
                    k.copy("act", v_aug[i][:, :, 0:128], pa[:].rearrange("p (h d) -> p h d", d=128))
                    k.dma("sp", v_d.key(tt)[b, :, :, j, :].rearrange("h p c -> p h c"), v_aug[i][:])
                    continue
                aug = qk_aug[part]
                k.act(scr[:], pa[:], AF.Square)
                k.op("dve", "tensor_reduce", out=n2[:], in_=scr[:].rearrange("p (h d) -> p h d", d=64), axis=AX.X, op=ALU.add)
                if part == 0:
                    k.act(n2[:], n2[:], AF.Sqrt)
                    k.copy("dve", nb_[:], n2[:])
                    k.ts("dve", aug[:, :, 64:65], nb_[:].w(lambda a: a.unsqueeze(2)), -1.0, ALU.mult)
                else:
                    k.tt("dve", kmax2[:], kmax2[:], n2[:], ALU.max)
                if lat:
                    self.rope(pa[:], 16, cs[i][:], sn[i][:], scr[:], aug[:, :, 0:64])
                else:
                    k.copy("dve", aug[:, :, 0:64], pa[:].rearrange("p (h d) -> p h d", d=64))
                for h in range(16):
                    k.tr(ps_T[0:65, h * 128:(h + 1) * 128], aug[:, h, :], ident_bf[:])
                ts_ = T_st[part]
                k.copy("act", ts_[:].rearrange("r h t -> r (h t)"), ps_T[0:65, :])
                dst = qT_d if part == 0 else kT_d
                k.dma("sp", dst.key(tt)[b, :, :, j * 128:(j + 1) * 128].rearrange("h r t -> r h t"), ts_[:])
        km1 = k.sb("km1", [128, 1], F32)
        k.op("dve", "tensor_reduce", out=km1[:], in_=kmax2[:], axis=AX.X, op=ALU.max)
        k.tr(ps_a[0][0:1, 0:128], km1[:], ident_f[:])
        kmx = k.sb("kmx", [1, 1], F32)
        kmxb = k.sb("kmxb", [1, 1], BF16)
        k.op("dve", "tensor_reduce", out=kmx[:], in_=ps_a[0][0:1, 0:128], axis=AX.X, op=ALU.max)
        k.act(kmx[:], kmx[:], AF.Sqrt)
        k.copy("dve", kmxb[:], kmx[:])
        k.copy("dve", kmx[:], kmxb[:])
        kmrow = k.sb("kmrow", [1, W_], BF16)
        k.memset("pool", kmrow[:], 1.0)
        k.ts("dve", kmrow[:], kmrow[:], kmx[0:1, 0:1], ALU.mult)
        for b in range(NB):
            for g in range(16):
                k.dma("sp", kT_d[b, g, 64:65, :], kmrow[:])
    if self.kstop == "p1":
        return
    with k.phase("diffP2"):
        lamt = k.sb("lamt", [128, 256], F32)
        k.dma("sp", lamt[:], self.W["diff_lambda"][0:1, :, :].rearrange("o a d -> o (a d)").w(lambda a: a.partition_broadcast(128)))
        lp = k.sb("lp", [128, 128], F32)
        l2 = k.sb("l2", [128, 2], F32)
        l4 = lamt[:].rearrange("p (a d) -> p a d", d=64)
        k.tt("dve", lp[:, 0:64], l4[:, 0, :], l4[:, 1, :], ALU.mult)
        k.tt("dve", lp[:, 64:128], l4[:, 2, :], l4[:, 3, :], ALU.mult)
        k.op("dve", "tensor_reduce", out=l2[:], in_=lp[:].rearrange("p (a d) -> p a d", d=64), axis=AX.X, op=ALU.add)
        k.act(l2[:], l2[:], AF.Exp)
        nlam = k.sb("nlam", [128, 1], F32)
        k.tt("dve", nlam[:], l2[:, 1:2], l2[:, 0:1], ALU.subtract)
        k.ts("dve", nlam[:], nlam[:], -lam_init, ALU.add)
        gsub = k.sb("gsub", [128, 128], F32)
        k.dma("sp", gsub[:], self.W["diff_subln_g"][0:1, :].w(lambda a: a.partition_broadcast(128)))
        k.ts("dve", gsub[:], gsub[:], 1.0 - lam_init, ALU.mult)
        qT_g = [k.sb("qT_g%d" % i, [65, 2, W_], BF16) for i in range(2)]
        kT_g = [k.sb("kT_g%d" % i, [65, 2, W_], BF16) for i in range(2)]
        v_g = [k.sb("v_g%d" % i, [128, TB, 129], BF16) for i in range(2)]
        ET = [k.sb("ET%d" % i, [128, 512], BF16) for i in range(3)]
        o1 = [k.sb("o1_%d" % i, [128, 128], F32) for i in range(2)]
        osq = k.sb("osq", [128, 128], F32)
        rr = [k.sb("rr%d" % i, [128, 2], F32) for i in range(2)]
        sso = [k.sb("sso%d" % i, [128, 1], F32) for i in range(2)]
        ao = [k.sb("ao%d" % i, [128, 128], BF16) for i in range(2)]
        ps_s = [k.ps("ps_s%d" % i, [128, 512], F32) for i in range(4)]
        ps_o = [[k.ps("ps_o%d_%d" % (m, i), [128, 512], F32) for i in range(2)] for m in range(2)]
        sblocks = [(q0, min(4, TS - q0), list(range(TB))) for q0 in range(0, TS, 4)]
        sblocks += [(TS + q0, min(4, TC - q0), list(range(TS, TB))) for q0 in range(0, TC, 4)]
        it = 0
        cnt = 0
        def d2_load(b, h):
            bi = (b * 8 + h) % 2
            k.dma("sp", qT_g[bi][:], qT_d[b, 2 * h:2 * h + 2, :, :].rearrange("m r t -> r m t"))
            k.dma("sp", kT_g[bi][:], kT_d[b, 2 * h:2 * h + 2, :, :].rearrange("m r t -> r m t"))
            k.dma("sp", v_g[bi][:], v_d[b, h, :, :, :])
        bhs = [(b, h) for b in range(NB) for h in range(8)]
        d2_load(*bhs[0])
        for bhi, (b, h) in enumerate(bhs):
            if True:
                bi = (b * 8 + h) % 2
                if bhi + 1 < len(bhs):
                    d2_load(*bhs[bhi + 1])
                for (q0, nq, kbs) in sblocks:
                    wq = nq * 128
                    items = [(m, idx, jb) for m in range(2) for idx, jb in enumerate(kbs)]

                    def d_qk(i_):
                        m, idx, jb = items[i_]
                        pss = ps_s[(it + i_) % 4]
                        k.mm(pss[:, 0:wq], kT_g[bi][:, m, jb * 128:(jb + 1) * 128], qT_g[bi][:, m, q0 * 128:q0 * 128 + wq])

                    def d_rest(i_):
                        m, idx, jb = items[i_]
                        pss = ps_s[(it + i_) % 4]
                        et = ET[(it + i_) % 3]
                        k.act(et[:, 0:wq], pss[:, 0:wq], AF.Exp, scale=0.125)
                        for qb in range(nq):
                            po = ps_o[m][qb // 2]
                            c0 = (qb % 2) * 129
                            k.mm(po[:, c0:c0 + 129], et[:, qb * 128:(qb + 1) * 128], v_g[bi][:, jb, :],
                                 start=(idx == 0 and qb % 2 == 0), stop=(idx == len(kbs) - 1), skip=True)
                    for i_ in range(min(2, len(items))):
                        d_qk(i_)
                    for i_ in range(len(items)):
                        if i_ + 2 < len(items):
                            d_qk(i_ + 2)
                        d_rest(i_)
                    it += len(items)
                    for qb in range(nq):
                        c0 = (qb % 2) * 129
                        p1 = ps_o[0][qb // 2]
                        p2 = ps_o[1][qb // 2]
                        ci = cnt % 2
                        cnt += 1
                        r_ = rr[ci]
                        k.copy("dve", r_[:, 0:1], p1[:, c0 + 128:c0 + 129])
                        k.copy("dve", r_[:, 1:2], p2[:, c0 + 128:c0 + 129])
                        k.op("dve", "reciprocal", out=r_[:], in_=r_[:])
                        k.ts("dve", r_[:, 1:2], r_[:, 1:2], nlam[:, 0:1], ALU.mult)
                        k.ts("dve", o1[ci][:], p1[:, c0:c0 + 128], r_[:, 0:1], ALU.mult)
                        k.stt("dve", o1[ci][:], p2[:, c0:c0 + 128], r_[:, 1:2], o1[ci][:], ALU.mult, ALU.add)
                        k.act(osq[:], o1[ci][:], AF.Square, accum_out=sso[ci][:])
                        k.ts("dve", sso[ci][:], sso[ci][:], 1.0 / 128, ALU.mult, NORM_EPS, ALU.add)
                        k.act(sso[ci][:], sso[ci][:], AF.Sqrt)
                        k.op("dve", "reciprocal", out=sso[ci][:], in_=sso[ci][:])
                        k.stt("dve", ao[ci][:], o1[ci][:], sso[ci][:, 0:1], gsub[:], ALU.mult, ALU.mult)
                        tt = b * TB + q0 + qb
                        k.dma("sp", self.attn_d.key(tt)[tt * 128:(tt + 1) * 128, h * 128:(h + 1) * 128], ao[ci][:])


Prog.diff_attention = diff_attention


POOL_WINDOWS = (2, 4, 8, 16)


def pool_band_consts():
    out = np.zeros((20, 128, 128), np.float32)
    Lbig = 128 * 8
    for wi, w in enumerate(POOL_WINDOWS):
        for vi, tile in enumerate([0, 3, 7]):
            for i in range(128):
                p = tile * 128 + i
                lo = min(max(p - w // 2, 0), Lbig)
                hi = min(max(p + w - w // 2, 0), Lbig)
                cnt = hi - lo
                for q in range(lo, hi):
                    j = q - tile * 128
                    if 0 <= j < 128:
                        out[wi * 3 + vi, j, i] += 1.0 / cnt
                    elif j < 0 and vi == 1:
                        out[12 + wi, j + 128, i] += 1.0 / cnt
                    elif j >= 128 and vi == 1:
                        out[16 + wi, j - 128, i] += 1.0 / cnt
                out[wi * 3 + vi, i, i] -= 1.0
    return out


def pool_mixer(self, l, src, last):
    k = self.k
    NB, TS, TC, TB, NT = self.NB, self.TS, self.TC, self.TB, self.NT
    self.y_d = getattr(self, "y_d", None) or k.dram("pool_y", [NT * 128, D], F32)
    with k.phase("pool"):
        band = k.sb("band", [128, 20, 128], F32)
        k.dma("sp", band[:], self.Cn["pool_band"][:, :, :].rearrange("n j i -> j n i"))
        wg = k.sb("wg", [128, 4, 2, 256], BF16)
        for g in range(4):
            for cc in range(2):
                k.dma("pool", wg[:, g, cc, :], self.W["pool_w_group"][0, g, cc * 128:(cc + 1) * 128, :])
        bgb = k.sb("bgb", [128, D], F32)
        lsb = k.sb("lsb", [128, D], F32)
        k.dma("sp", bgb[:], self.W["pool_b_group"][0:1, :].w(lambda a: a.partition_broadcast(128)))
        k.dma("sp", lsb[:], self.W["pool_scale"][0:1, :].w(lambda a: a.partition_broadcast(128)))
        st1 = self.alloc_mod(l, 0, gate=False)
        xt = [k.sb("xt%d" % i, [128, D], F32) for i in range(2)]
        hh = [k.sb("hh%d" % i, [128, D], F32) for i in range(3)]
        scr = k.sb("scr", [128, D], F32)
        ss = k.sb("ss", [128, 1], F32)
        yT = k.sb("yT", [128, 8, 128], BF16)
        yo = [k.sb("yo%d" % i, [128, D], F32) for i in range(2)]
        ps_p = k.ps("ps_p", [128, D], F32)
        ps_y = k.ps("ps_y", [128, D], F32)
        seqs = []
        for b in range(NB):
            seqs.append([b * TB + j for j in range(TS)])
            seqs.append([b * TB + TS + j for j in range(TC)])
        cnt = 0
        for seq in seqs:
            n = len(seq)
            assert n >= 2

            def mk_h(t):
                nonlocal cnt
                tt = seq[t]
                x_ = xt[cnt % 2]
                cnt += 1
                k.dma("sp", x_[:], src.key(tt)[tt * 128:(tt + 1) * 128, :])
                self.set_cond(st1, self.cond_of(tt), need_gate=False)
                self.norm_mod(x_[:], st1["Gp"][:], st1["sh"][:], hh[t % 3][:], scr[:], ss[:])
            mk_h(0)
            for t in range(n):
                tt = seq[t]
                if t + 1 < n:
                    mk_h(t + 1)
                vi = 0 if t == 0 else (2 if t == n - 1 else 1)
                for c in range(8):
                    wi = c // 2
                    srcs = [(hh[t % 3], band[:, wi * 3 + vi, :])]
                    if t > 0:
                        srcs.append((hh[(t - 1) % 3], band[:, 12 + wi, :]))
                    if t + 1 < n:
                        srcs.append((hh[(t + 1) % 3], band[:, 16 + wi, :]))
                    for si, (hsrc, bm) in enumerate(srcs):
                        k.mm(ps_p[:, c * 128:(c + 1) * 128], hsrc[:, c * 128:(c + 1) * 128], bm, start=(si == 0), stop=(si == len(srcs) - 1))
                k.copy("act", yT[:].rearrange("p c t -> p (c t)"), ps_p[:])
                for g in range(4):
                    for cc in range(2):
                        k.mm(ps_y[:, g * 256:(g + 1) * 256], yT[:, 2 * g + cc, :], wg[:, g, cc, :], start=(cc == 0), stop=(cc == 1))
                yo_ = yo[t % 2]
                k.tt("dve", yo_[:], ps_y[:], bgb[:], ALU.add)
                k.tt("pool", yo_[:], yo_[:], lsb[:], ALU.mult)
                k.dma("sp", self.y_d.key(tt)[tt * 128:(tt + 1) * 128, :], yo_[:])


Prog.pool_mixer = pool_mixer


def rwkv_consts():
    c = {}
    row = np.arange(128)[:, None]
    col = np.arange(128)[None, :]
    ms = {"U": row < col, "Lo": row > col, "UI": row <= col, "LI": row >= col}
    c["rw_masks"] = np.stack([np.tile(ms["U"], (1, 4)), np.tile(ms["Lo"], (1, 4)),
                              -np.tile(ms["UI"], (1, 4)).astype(np.float32), -np.tile(ms["LI"], (1, 4)).astype(np.float32),
                              np.tile(ms["UI"], (1, 4)), np.tile(ms["LI"], (1, 4))], axis=0).astype(np.float32)
    bo = np.zeros((128, 128), np.float32)
    bo[:64, :64] = 1.0
    bo[64:, 64:] = 1.0
    c["rw_bones"] = bo
    sel = np.zeros((128, 2), np.float32)
    sel[:64, 0] = 1.0
    sel[64:, 1] = 1.0
    c["rw_sel2"] = sel
    c["rw_i2"] = np.concatenate([np.eye(64), np.eye(64)], axis=0).astype(np.float32)
    return c


def rwkv_features(self, l, src):
    k = self.k
    NB, TS, TC, TB, NT = self.NB, self.TS, self.TC, self.TB, self.NT
    W_ = TB * 128
    hT_d = k.dram("rw_hT", [NB, D, W_], F32)
    self.F_d = k.dram("rw_F", [NB, 2, TB, 128, 6 * 1024], BF16)
    self.Wc_d = k.dram("rw_Wc", [NB, 2, TB, 128, 8], F32)
    self.v_d = k.dram("rw_v", [NT * 128, D], BF16)
    self.g_d = k.dram("rw_g", [NT * 128, D], BF16)
    self.bs_d = k.dram("rw_bs", [NT * 128, 16], F32)
    with k.phase("rwA"):
        ident_f = k.sb("ident_f", [128, 128], F32)
        k.dma("sp", ident_f[:], self.Cn["ident"][:, :])
        st1 = self.alloc_mod(l, 0, gate=False)
        xt = [k.sb("xt%d" % i, [128, D], F32) for i in range(2)]
        scr = k.sb("scr", [128, D], F32)
        ss = k.sb("ss", [128, 1], F32)
        h = k.sb("h", [128, D], F32)
        hT = [k.sb("hT%d" % i, [128, 8, 128], F32) for i in range(2)]
        ps = [k.ps("psA%d" % i, [128, D], F32) for i in range(2)]
        k.dma("sp", xt[0][:], src.key(0)[0:128, :])
        for tt in range(NT):
            b, j = divmod(tt, TB)
            i = tt % 2
            if tt + 1 < NT:
                k.dma("sp", xt[1 - i][:], src.key(tt + 1)[(tt + 1) * 128:(tt + 2) * 128, :])
            self.set_cond(st1, self.cond_of(tt), need_gate=False)
            self.norm_mod(xt[i][:], st1["Gp"][:], st1["sh"][:], h[:], scr[:], ss[:])
            for kc in range(8):
                k.tr(ps[i][:, kc * 128:(kc + 1) * 128], h[:, kc * 128:(kc + 1) * 128], ident_f[:])
            k.copy("act", hT[i][:].rearrange("p c t -> p (c t)"), ps[i][:])
            k.dma("sp", hT_d.key(tt)[b, :, j * 128:(j + 1) * 128].rearrange("(c p) t -> p c t", p=128), hT[i][:])
    if self.kstop == "rwA":
        return
    with k.phase("rwB"):
        def wload(name, srcv, kch, n):
            return self.load_w_bf16(name, srcv, kch, n)
        Wr = wload("Wr", self.W["rwkv_w_rkv"][0, 0], 8, D)
        Wk = wload("Wk", self.W["rwkv_w_rkv"][0, 1], 8, D)
        Wv = wload("Wv", self.W["rwkv_w_rkv"][0, 2], 8, D)
        wa1 = k.sb("wa1", [128, 8, 128], BF16)
        aa1 = k.sb("aa1", [128, 8, 128], BF16)
        wa2 = k.sb("wa2", [128, D], BF16)
        aa2 = k.sb("aa2", [128, D], BF16)
        for d in range(2):
            for kc in range(8):
                k.dma("pool", wa1[:, kc, d * 64:(d + 1) * 64], self.W["rwkv_w_a1"][0, d, kc * 128:(kc + 1) * 128, :])
                k.dma("pool", aa1[:, kc, d * 64:(d + 1) * 64], self.W["rwkv_a_a1"][0, d, kc * 128:(kc + 1) * 128, :])
            k.dma("pool", wa2[d * 64:(d + 1) * 64, :], self.W["rwkv_w_a2"][0, d, :, :])
            k.dma("pool", aa2[d * 64:(d + 1) * 64, :], self.W["rwkv_a_a2"][0, d, :, :])
        g1 = wload("g1", self.W["rwkv_g1"][0], 8, 128)
        g2 = k.sb("g2", [128, D], BF16)
        k.dma("pool", g2[:], self.W["rwkv_g2"][0, :, :])
        sc = k.sb("sc", [128, 8, 24], F32)

        def scload(col, v1d):
            k.dma("sp", sc[:, :, col:col + 1], v1d.rearrange("(c p o) -> p c o", p=128, o=1), allow_slow_non_contiguous=True)
        for d in range(2):
            for i in range(6):
                scload(d * 6 + i, self.W["rwkv_mu"][0, d, i, :])
        scload(12, self.W["rwkv_k_k"][0, :])
        scload(13, self.W["rwkv_k_a"][0, :])
        scload(14, self.W["rwkv_r_k"][0, :, :].rearrange("h d -> (h d)"))
        scload(15, self.W["rwkv_w0"][0, 0, :])
        scload(16, self.W["rwkv_w0"][0, 1, :])
        scload(17, self.W["rwkv_a0"][0, 0, :])
        scload(18, self.W["rwkv_a0"][0, 1, :])
        c0 = k.sb("c0", [128, 8, 6], F32)
        k.tt("dve", c0[:], sc[:, :, 0:6], sc[:, :, 6:12], ALU.add)
        k.ts("dve", c0[:], c0[:], -1.0, ALU.mult, 1.0, ALU.add)
        oka = k.sb("oka", [128, 8, 1], F32)
        k.ts("dve", oka[:], sc[:, :, 13:14], -1.0, ALU.mult, 1.0, ALU.add)
        bones = k.sb("bones", [128, 128], F32)
        k.dma("sp", bones[:], self.Cn["rw_bones"][:, :])
        sel2 = k.sb("sel2", [128, 2], F32)
        k.dma("sp", sel2[:], self.Cn["rw_sel2"][:, :])
        WB = 256
        blk = k.sb("blk", [128, 8, WB + 2], F32)
        mix = [k.sb("mix%d" % i, [128, 8, WB], BF16) for i in range(2)]
        t_a = k.sb("t_a", [128, 8, WB], F32)
        t_b = k.sb("t_b", [128, 8, WB], F32)
        kT = k.sb("kT", [128, 8, WB], F32)
        rT = k.sb("rT", [128, 8, WB], F32)
        kkT = k.sb("kkT", [128, 8, WB], F32)
        aT = k.sb("aT_", [128, 8, WB], F32)
        kdT = k.sb("kdT", [128, 8, WB], F32)
        bT = k.sb("bT", [128, 8, WB], F32)
        lwT = k.sb("lwT", [128, 8, WB], F32)
        cA = k.sb("cA", [128, 8, WB], F32)
        cB = k.sb("cB", [128, 8, WB], F32)
        ex = k.sb("ex", [128, 8, WB], F32)
        fo = [k.sb("fo%d" % i, [128, 2, 6 * 1024], BF16) for i in range(1)]
        wc = k.sb("wc", [128, 2, 8], F32)
        hid = k.sb("hid", [128, WB], BF16)
        vt = [k.sb("vt%d" % i, [128, D], BF16) for i in range(2)]
        bsv = k.sb("bsv", [128, 16], F32)
        ps_m = [k.ps("ps_m%d" % i, [128, 512], F32) for i in range(4)]
        ps_v = k.ps("ps_v", [128, D], F32)
        ps_b = k.ps("ps_b", [128, 512], F32)
        pc = [0]

        def nps():
            pc[0] += 1
            return ps_m[pc[0] % 4]

        def bc(v, n):
            return v.w(lambda a: a.to_broadcast([128, 8, n]))

        blocks = []
        for b in range(NB):
            for (s0, n_) in [(0, TS), (TS, TC)]:
                for t0 in range(0, n_, 2):
                    blocks.append((b, s0, n_, t0, min(2, n_ - t0)))
        mi = [0]
        for (b, s0, n_, t0, nt) in blocks:
            wb = nt * 128
            lat = s0 == 0
            g0 = (s0 + t0) * 128
            first, lastb = t0 == 0, t0 + nt == n_
            lo = g0 - (0 if first else 1)
            hi = g0 + wb + (0 if lastb else 1)
            if first:
                k.memset("pool", blk[:, :, 0:1], 0.0)
            if lastb:
                k.memset("pool", blk[:, :, wb + 1:wb + 2], 0.0)
            k.dma("sp", blk[:, :, (1 if first else 0):(1 if first else 0) + (hi - lo)],
                  hT_d[b, :, lo:hi].rearrange("(c p) t -> p c t", p=128))
            cur, prv, nxt = blk[:, :, 1:wb + 1], blk[:, :, 0:wb], blk[:, :, 2:wb + 2]

            def mk_mix(i):
                m_ = mix[mi[0] % 2]
                mi[0] += 1
                k.tt("dve", t_a[:, :, 0:wb], prv, bc(sc[:, :, i:i + 1], wb), ALU.mult)
                k.tt("pool", t_b[:, :, 0:wb], nxt, bc(sc[:, :, 6 + i:7 + i], wb), ALU.mult)
                k.tt("pool", t_a[:, :, 0:wb], t_a[:, :, 0:wb], t_b[:, :, 0:wb], ALU.add)
                k.tt("dve", t_b[:, :, 0:wb], cur, bc(c0[:, :, i:i + 1], wb), ALU.mult)
                k.tt("pool", m_[:, :, 0:wb], t_a[:, :, 0:wb], t_b[:, :, 0:wb], ALU.add)
                return m_

            def proj_fm(m_, Wt, dst):
                for oc in range(8):
                    p_ = nps()
                    for kc in range(8):
                        k.mm(p_[:, 0:wb], Wt[:, kc, oc * 128:(oc + 1) * 128], m_[:, kc, 0:wb], start=(kc == 0), stop=(kc == 7))
                    k.copy("act", dst[:, oc, 0:wb], p_[:, 0:wb])
            m2 = mk_mix(2)
            proj_fm(m2, Wk, kT)
            k.tt("dve", kkT[:, :, 0:wb], kT[:, :, 0:wb], bc(sc[:, :, 12:13], wb), ALU.mult)
            k.tt("pool", t_a[:, :, 0:wb], kkT[:, :, 0:wb], kkT[:, :, 0:wb], ALU.mult)
            for oc in range(8):
                p_ = nps()
                k.mm(p_[:, 0:wb], bones[:], t_a[:, oc, 0:wb])
                k.act(t_b[:, oc, 0:wb], p_[:, 0:wb], AF.Sqrt)
            k.ts("dve", t_b[:, :, 0:wb], t_b[:, :, 0:wb], 1e-12, ALU.max)
            k.op("dve", "reciprocal", out=t_b[:, :, 0:wb], in_=t_b[:, :, 0:wb])
            k.tt("dve", kkT[:, :, 0:wb], kkT[:, :, 0:wb], t_b[:, :, 0:wb], ALU.mult)
            m3 = mk_mix(3)
            for t in range(nt):
                tt = b * TB + s0 + t0 + t
                for n in range(2):
                    for kc in range(8):
                        k.mm(ps_v[:, n * 512:(n + 1) * 512], m3[:, kc, t * 128:(t + 1) * 128], Wv[:, kc, n * 512:(n + 1) * 512], start=(kc == 0), stop=(kc == 7))
                k.copy("act", vt[t % 2][:], ps_v[:])
                k.dma("sp", self.v_d.key(tt)[tt * 128:(tt + 1) * 128, :], vt[t % 2][:])
            if lat:
                m0 = mk_mix(0)
                proj_fm(m0, Wr, rT)
                m5 = mk_mix(5)
                p_ = nps()
                for kc in range(8):
                    k.mm(p_[:, 0:wb], g1[:, kc, :], m5[:, kc, 0:wb], start=(kc == 0), stop=(kc == 7))
                k.act(hid[:, 0:wb], p_[:, 0:wb], AF.Sigmoid)
                for t in range(nt):
                    tt = b * TB + s0 + t0 + t
                    for n in range(2):
                        k.mm(ps_v[:, n * 512:(n + 1) * 512], hid[:, t * 128:(t + 1) * 128], g2[:, n * 512:(n + 1) * 512])
                    k.copy("act", vt[t % 2][:], ps_v[:])
                    k.dma("sp", self.g_d.key(tt)[tt * 128:(tt + 1) * 128, :], vt[t % 2][:])
            m1 = mk_mix(1)
            p_ = nps()
            for kc in range(8):
                k.mm(p_[:, 0:wb], wa1[:, kc, :], m1[:, kc, 0:wb], start=(kc == 0), stop=(kc == 7))
            hw = k_hw = hid
            k.act(hw[:, 0:wb], p_[:, 0:wb], AF.Tanh)
            lws = []
            for d in range(2):
                dst = lwT if d == 0 else cA
                for oc in range(8):
                    p_ = nps()
                    k.mm(p_[:, 0:wb], wa2[d * 64:(d + 1) * 64, oc * 128:(oc + 1) * 128], hw[d * 64:(d + 1) * 64, 0:wb])
                    k.act(dst[:, oc, 0:wb], p_[:, 0:wb], AF.Sigmoid, bias=sc[:, oc, 15 + d:16 + d])
            m4 = mk_mix(4)
            p_ = nps()
            for kc in range(8):
                k.mm(p_[:, 0:wb], aa1[:, kc, :], m4[:, kc, 0:wb], start=(kc == 0), stop=(kc == 7))
            ha = k.sb if False else None
            k.copy("act", hid[:, 0:wb], p_[:, 0:wb])
            first_bs = True
            for d in range(2):
                for oc in range(8):
                    p_ = nps()
                    k.mm(p_[:, 0:wb], aa2[d * 64:(d + 1) * 64, oc * 128:(oc + 1) * 128], hid[d * 64:(d + 1) * 64, 0:wb])
                    k.act(aT[:, oc, 0:wb], p_[:, 0:wb], AF.Sigmoid, bias=sc[:, oc, 17 + d:18 + d])
                k.tt("dve", t_a[:, :, 0:wb], aT[:, :, 0:wb], bc(sc[:, :, 13:14], wb), ALU.mult)
                k.tt("pool", t_a[:, :, 0:wb], t_a[:, :, 0:wb], bc(oka[:, :, 0:1], wb), ALU.add)
                k.tt("dve", kdT[:, :, 0:wb], kT[:, :, 0:wb], t_a[:, :, 0:wb], ALU.mult)
                k.tt("pool", bT[:, :, 0:wb], kkT[:, :, 0:wb], aT[:, :, 0:wb], ALU.mult)
                if lat:
                    k.tt("dve", t_a[:, :, 0:wb], rT[:, :, 0:wb], kdT[:, :, 0:wb], ALU.mult)
                    k.tt("pool", t_a[:, :, 0:wb], t_a[:, :, 0:wb], bc(sc[:, :, 14:15], wb), ALU.mult)
                    for t in range(nt):
                        for oc in range(8):
                            k.mm(ps_b[:, t * 16 + 2 * oc:t * 16 + 2 * oc + 2], t_a[:, oc, t * 128:(t + 1) * 128], sel2[:],
                                 start=first_bs, stop=(d == 1), skip=True)
                            first_bs = False
                lsrc = lwT if d == 0 else cA
                if d == 1:
                    k.copy("act", lwT[:, :, 0:wb], cA[:, :, 0:wb])
                k.ts("dve", lwT[:, :, 0:wb], lwT[:, :, 0:wb], -0.6065306597126334, ALU.mult)
                src_c, dst_c = lwT, cB
                bufs = [cB, ex]
                bi_ = 0
                cur_c = lwT
                for s_ in (1, 2, 4, 8, 16, 32, 64):
                    nb_ = bufs[bi_ % 2]
                    bi_ += 1
                    c4 = cur_c[:, :, 0:wb].rearrange("p c (t k) -> p c t k", k=128)
                    n4 = nb_[:, :, 0:wb].rearrange("p c (t k) -> p c t k", k=128)
                    if d == 0:
                        k.copy("act", n4[:, :, :, 0:s_], c4[:, :, :, 0:s_])
                        k.tt("dve", n4[:, :, :, s_:128], c4[:, :, :, s_:128], c4[:, :, :, 0:128 - s_], ALU.add)
                    else:
                        k.copy("act", n4[:, :, :, 128 - s_:128], c4[:, :, :, 128 - s_:128])
                        k.tt("dve", n4[:, :, :, 0:128 - s_], c4[:, :, :, 0:128 - s_], c4[:, :, :, s_:128], ALU.add)
                    cur_c = nb_
                cum = cur_c
                cum4 = cum[:, :, 0:wb].rearrange("p c (t k) -> p c t k", k=128)
                cend = cum4[:, :, :, 127:128] if d == 0 else cum4[:, :, :, 0:1]
                fo_ = fo[0]

                def fq(q):
                    return fo_[:, 0:nt, q * 1024:(q + 1) * 1024].rearrange("p t (c k) -> p c t k", k=128)

                def v4(x):
                    return x[:, :, 0:wb].rearrange("p c (t k) -> p c t k", k=128)
                k.tt("pool", t_a[:, :, 0:wb], cum[:, :, 0:wb], lwT[:, :, 0:wb], ALU.subtract)
                k.act(ex[:, :, 0:wb], t_a[:, :, 0:wb], AF.Exp)
                k.tt("dve", fq(0), v4(kkT), v4(ex), ALU.mult)
                k.act(ex[:, :, 0:wb], cum[:, :, 0:wb], AF.Exp)
                if lat:
                    k.tt("dve", fq(1), v4(rT), v4(ex), ALU.mult)
                k.act(ex[:, :, 0:wb], cum[:, :, 0:wb], AF.Exp, scale=-1.0)
                k.tt("dve", fq(2), v4(bT), v4(ex), ALU.mult)
                k.tt("pool", fq(3), v4(kdT), v4(ex), ALU.mult)
                k.tt("dve", v4(t_a), cend.w(lambda a: a.to_broadcast([128, 8, nt, 128])), v4(cum), ALU.subtract)
                k.act(ex[:, :, 0:wb], t_a[:, :, 0:wb], AF.Exp)
                k.tt("dve", fq(4), v4(bT), v4(ex), ALU.mult)
                k.tt("pool", fq(5), v4(kdT), v4(ex), ALU.mult)
                k.act(wc[:, 0:nt, :].rearrange("p t c -> p c t"), cend.rearrange("p c t o -> p c (t o)"), AF.Exp)
                tl0 = s0 + t0
                k.dma("sp", self.F_d.key((b, d, tl0))[b, d, tl0:tl0 + nt, :, :].rearrange("t p f -> p t f"), fo_[:, 0:nt, :])
                k.dma("sp", self.Wc_d.key((b, d, tl0))[b, d, tl0:tl0 + nt, :, :].rearrange("t p c -> p t c"), wc[:, 0:nt, :])
            if lat:
                for t in range(nt):
                    tt = b * TB + s0 + t0 + t
                    k.copy("dve", bsv[:], ps_b[:, t * 16:(t + 1) * 16])
                    k.dma("sp", self.bs_d.key(tt)[tt * 128:(tt + 1) * 128, :], bsv[:])


Prog.rwkv_features = rwkv_features


def rwkv_scan(self, l, src):
    k = self.k
    NB, TS, TC, TB, NT = self.NB, self.TS, self.TC, self.TB, self.NT
    o_d = k.dram("rw_o", [2, NT * 128, D], F32)
    self.o_d = o_d
    with k.phase("rwC"):
        ident_bf = k.sb("ident_bf", [128, 128], BF16)
        k.dma("pool", ident_bf[:], self.Cn["ident"][:, :])
        i2 = k.sb("i2", [128, 64], F32)
        k.dma("sp", i2[:], self.Cn["rw_i2"][:, :])
        masks = k.sb("masks", [128, 6, 512], F32)
        k.dma("sp", masks[:], self.Cn["rw_masks"][:, :, :].rearrange("m p f -> p m f"))
        f = [k.sb("f%d" % i, [128, 6 * 1024], BF16) for i in range(2)]
        wct = [k.sb("wct%d" % i, [128, 8], F32) for i in range(2)]
        vv = [k.sb("vv%d" % i, [128, D], BF16) for i in range(2)]
        Z = k.sb("Z", [128, 8, 64], BF16)
        NBh = k.sb("NBh", [128, 16, 64], BF16)
        Kh = k.sb("Kh", [128, 16, 64], BF16)
        LT = [k.sb("LT%d" % i, [128, 16, 128], F32) for i in range(2)]
        Lm = [k.sb("Lm%d" % i, [128, 16, 128], F32) for i in range(2)]
        LqkT = k.sb("LqkT", [128, 16, 128], BF16)
        NLrbT = k.sb("NLrbT", [128, 16, 128], BF16)
        LrkT = k.sb("LrkT", [128, 16, 128], BF16)
        Y32 = k.sb("Y32", [128, 16, 128], F32)
        Ybf = k.sb("Ybf", [128, 16, 128], BF16)
        RhT = k.sb("RhT", [64, 16, 128], BF16)
        Pt = k.sb("Pt", [64, 16, 64], BF16)
        G32 = k.sb("G32", [64, 16, 64], F32)
        M32 = k.sb("M32", [64, 16, 64], F32)
        Mbf = k.sb("Mbf", [64, 16, 64], BF16)
        ot = [k.sb("ot%d" % i, [128, D], F32) for i in range(2)]
        psw = [k.ps("psw%d" % i, [128, 512], F32) for i in range(7)]
        pst = k.ps("pst", [128, D], BF16)
        pc = [0]

        def nps():
            pc[0] += 1
            return psw[pc[0] % 7]
        lc = 0
        for b in range(NB):
            for d in range(2):
                order = (list(range(TS, TB)) + list(range(TS))) if d == 0 else (list(range(TB - 1, TS - 1, -1)) + list(range(TS - 1, -1, -1)))
                mA, mB, mNAI, mAI = (0, 1, 2, 4) if d == 0 else (1, 0, 3, 5)
                k.memset("dve", M32[:], 0.0)
                k.memset("pool", Mbf[:], 0.0)

                def c_load(tl, i):
                    tt = b * TB + tl
                    k.dma("sp", f[i][:], self.F_d[b, d, tl, :, :])
                    k.dma("sp", wct[i][:], self.Wc_d[b, d, tl, :, :])
                    k.dma("sp", vv[i][:], self.v_d[tt * 128:(tt + 1) * 128, :])
                c_load(order[0], lc % 2)
                for oi, tl in enumerate(order):
                    tt = b * TB + tl
                    lat = tl < TS
                    fi = f[lc % 2]
                    wc_ = wct[lc % 2]
                    V = vv[lc % 2]
                    lc += 1
                    if oi + 1 < len(order):
                        c_load(order[oi + 1], lc % 2)

                    def X(q, h):
                        c_, hp = divmod(h, 2)
                        return fi[hp * 64:(hp + 1) * 64, q * 1024 + c_ * 128:q * 1024 + (c_ + 1) * 128]

                    def Vh(h):
                        return V[:, h * 64:(h + 1) * 64]
                    def hsel4(x, gg, hp):
                        return x.rearrange("p (g i q) t -> p g i q t", g=2, i=4, q=2)[:, gg, :, hp, :]

                    def hsel8(x, hp):
                        return x.rearrange("p (i q) t -> p i q t", q=2)[:, :, hp, :]
                    G4 = [(gg, hp) for hp in range(2) for gg in range(2)]
                    for q, dst in ((0, None), (4, NBh), (5, Kh)):
                        for c_ in range(8):
                            k.tr(pst[:, c_ * 128:(c_ + 1) * 128], fi[:, q * 1024 + c_ * 128:q * 1024 + (c_ + 1) * 128], ident_bf[:])
                        pv = pst[:].rearrange("p (h k) -> p h k", k=64)
                        if q == 0:
                            k.copy("act", Y32[:, :, 0:64], pv)
                        elif q == 4:
                            k.ts("dve", dst[:], pv, -1.0, ALU.mult)
                        else:
                            k.copy("act", dst[:], pv)
                    k.tt("dve", Z[:], i2[:].w(lambda a: a.unsqueeze(1).to_broadcast([128, 8, 64])),
                         wc_[:].w(lambda a: a.unsqueeze(2).to_broadcast([128, 8, 64])), ALU.mult)
                    if self.kstop == "rwC0":
                        return
                    jobs = [(2, 0, LT[0], mA), (0, 2, Lm[0], mB), (3, 0, LqkT, mA)]
                    if lat:
                        jobs += [(2, 1, NLrbT, mNAI), (3, 1, LrkT, mAI)]
                    ev = 0
                    for (qa, qb, dst, mi_) in jobs:
                        for (gg, hp) in G4:
                            p_ = nps()
                            for hh in range(4):
                                h = 8 * gg + 2 * hh + hp
                                k.mm(p_[:, hh * 128:(hh + 1) * 128], X(qa, h), X(qb, h))
                            k.tt("dve", hsel4(dst[:], gg, hp), p_[:].rearrange("p (h t) -> p h t", t=128),
                                 masks[:, mi_, :].rearrange("p (h t) -> p h t", t=128), ALU.mult)
                    if self.kstop == "rwC1":
                        return
                    for g8 in range(2):
                        p_ = nps()
                        for hh in range(8):
                            h = 8 * g8 + hh
                            k.mm(p_[:, hh * 64:(hh + 1) * 64], LqkT[:, h, :], Vh(h))
                        k.copy("act", Y32[:, 8 * g8:8 * g8 + 8, 64:128], p_[:].rearrange("p (h k) -> p h k", k=64))
                    if self.kstop == "rwC2":
                        return
                    cur = 0
                    for lev in range(7):
                        if lev > 0:
                            nxt = 1 - cur
                            for g4 in range(4):
                                if lev < 6:
                                    p_ = nps()
                                    for hh in range(4):
                                        h = 4 * g4 + hh
                                        k.mm(p_[:, hh * 128:(hh + 1) * 128], LT[cur][:, h, :], Lm[cur][:, h, :])
                                    k.copy("act", Lm[nxt][:, 4 * g4:4 * g4 + 4, :].rearrange("p h t -> p (h t)"), p_[:])
                                p_ = nps()
                                for hh in range(4):
                                    h = 4 * g4 + hh
                                    k.mm(p_[:, hh * 128:(hh + 1) * 128], Lm[cur][:, h, :], LT[cur][:, h, :])
                                k.copy("dve", LT[nxt][:, 4 * g4:4 * g4 + 4, :].rearrange("p h t -> p (h t)"), p_[:])
                            cur = nxt
                        for g4 in range(4):
                            p_ = nps()
                            for hh in range(4):
                                h = 4 * g4 + hh
                                k.mm(p_[:, hh * 128:(hh + 1) * 128], LT[cur][:, h, :], Y32[:, h, :])
                            ysl = Y32[:, 4 * g4:4 * g4 + 4, :].rearrange("p h t -> p (h t)")
                            k.tt("dve", ysl, ysl, p_[:], ALU.subtract if lev == 0 else ALU.add)
                            if lev == 6:
                                k.copy("act", Ybf[:, 4 * g4:4 * g4 + 4, :].rearrange("p h t -> p (h t)"), ysl)
                    if self.kstop == "rwC3":
                        return
                    if lat:
                        for (gg, hp) in G4:
                            p_ = nps()
                            for hh in range(4):
                                h = 8 * gg + 2 * hh + hp
                                o_ = p_[0:64, hh * 128:(hh + 1) * 128]
                                k.mm(o_, Ybf[:, h, 0:64], NLrbT[:, h, :], start=True, stop=False)
                                k.mm(o_, ident_bf[hp * 64:(hp + 1) * 64, hp * 64:(hp + 1) * 64], X(1, h), start=False, stop=True)
                            k.copy("act", hsel4(RhT[:], gg, hp), p_[0:64, :].rearrange("p (h t) -> p h t", t=128))
                    for hp in range(2):
                        p_ = nps()
                        for hh in range(8):
                            h = 2 * hh + hp
                            c_ = hh
                            o_ = p_[0:64, hh * 64:(hh + 1) * 64]
                            k.mm(o_, ident_bf[hp * 64:(hp + 1) * 64, hp * 64:(hp + 1) * 64], Z[hp * 64:(hp + 1) * 64, c_, :], start=True, stop=False)
                            k.mm(o_, Ybf[:, h, 0:64], NBh[:, h, :], start=False, stop=True)
                        k.copy("act", hsel8(Pt[:], hp), p_[0:64, :].rearrange("p (h t) -> p h t", t=64))
                    for g8 in range(2):
                        p_ = nps()
                        for hh in range(8):
                            h = 8 * g8 + hh
                            o_ = p_[0:64, hh * 64:(hh + 1) * 64]
                            k.mm(o_, Kh[:, h, :], Vh(h), start=True, stop=False)
                            k.mm(o_, NBh[:, h, :], Ybf[:, h, 64:128], start=False, stop=True)
                        k.copy("dve", G32[:, 8 * g8:8 * g8 + 8, :].rearrange("p h t -> p (h t)"), p_[0:64, :])
                    if self.kstop == "rwC4":
                        self.dbg2 = []
                        for nm, tl_, shp, dt in [("LT0", LT[0], [128, 2048], BF16), ("Lm0", Lm[0], [128, 2048], BF16), ("LqkT", LqkT, [128, 2048], BF16),
                                                 ("Y32", Y32, [128, 2048], F32), ("Pt", Pt, [64, 1024], BF16), ("G32", G32, [64, 1024], F32),
                                                 ("NBh", NBh, [128, 1024], BF16), ("Kh", Kh, [128, 1024], BF16), ("LTc", LT[cur], [128, 2048], BF16)]:
                            o = k.dram("dbg2_" + nm, shp, dt, kind="ExternalOutput")
                            k.dma("sp", o[:, :], tl_[:].rearrange("p h t -> p (h t)"))
                            self.dbg2.append(("dbg2_" + nm, o))
                        k.wait_all("sp", [o[:, :] for _, o in self.dbg2])
                        return
                    if lat:
                        o_t = ot[oi % 2]
                        for g8 in range(2):
                            p_ = nps()
                            for hh in range(8):
                                h = 8 * g8 + hh
                                o_ = p_[:, hh * 64:(hh + 1) * 64]
                                k.mm(o_, LrkT[:, h, :], Vh(h), start=True, stop=False)
                                k.mm(o_, NLrbT[:, h, :], Ybf[:, h, 64:128], start=False, stop=False)
                                k.mm(o_, RhT[:, h, :], Mbf[:, h, :], start=False, stop=True)
                            k.copy("act", o_t[:, g8 * 512:(g8 + 1) * 512], p_[:])
                        k.dma("sp", o_d.key((d, tt))[d, tt * 128:(tt + 1) * 128, :], o_t[:])
                    for g8 in range(2):
                        p_ = nps()
                        for hh in range(8):
                            h = 8 * g8 + hh
                            k.mm(p_[0:64, hh * 64:(hh + 1) * 64], Pt[:, h, :], Mbf[:, h, :])
                        msl = M32[:, 8 * g8:8 * g8 + 8, :].rearrange("p h t -> p (h t)")
                        k.tt("dve", msl, p_[0:64, :], G32[:, 8 * g8:8 * g8 + 8, :].rearrange("p h t -> p (h t)"), ALU.add)
                        k.copy("act", Mbf[:, 8 * g8:8 * g8 + 8, :].rearrange("p h t -> p (h t)"), msl)
    if self.kstop == "rwC":
        return
    with k.phase("rwD"):
        lnw = k.sb("lnw", [128, D], F32)
        lnb = k.sb("lnb", [128, D], F32)
        k.dma("sp", lnw[:], self.W["rwkv_ln_w"][0:1, :].w(lambda a: a.partition_broadcast(128)))
        k.dma("sp", lnb[:], self.W["rwkv_ln_b"][0:1, :].w(lambda a: a.partition_broadcast(128)))
        o0 = [k.sb("o0_%d" % i, [128, D], F32) for i in range(2)]
        o1 = [k.sb("o1_%d" % i, [128, D], F32) for i in range(2)]
        vb = [k.sb("vb%d" % i, [128, D], BF16) for i in range(2)]
        gb = [k.sb("gb%d" % i, [128, D], BF16) for i in range(2)]
        bs = [k.sb("bs%d" % i, [128, 16], F32) for i in range(2)]
        sq = k.sb("sq", [128, D], F32)
        mean = k.sb("mean", [128, 16], F32)
        var = k.sb("var", [128, 16], F32)
        res = [k.sb("res%d" % i, [128, D], BF16) for i in range(2)]
        lat_tiles = [tt for tt in range(NT) if self.is_lat(tt)]

        def d_load(ix):
            tt = lat_tiles[ix]
            i = ix % 2
            k.dma("sp", o0[i][:], o_d[0, tt * 128:(tt + 1) * 128, :])
            k.dma("sp", o1[i][:], o_d[1, tt * 128:(tt + 1) * 128, :])
            k.dma("sp", vb[i][:], self.v_d[tt * 128:(tt + 1) * 128, :])
            k.dma("sp", gb[i][:], self.g_d[tt * 128:(tt + 1) * 128, :])
            k.dma("sp", bs[i][:], self.bs_d[tt * 128:(tt + 1) * 128, :])
        d_load(0)
        for ix, tt in enumerate(lat_tiles):
            i = ix % 2
            if ix + 1 < len(lat_tiles):
                d_load(ix + 1)
            o = o0[i]

            def h3(x):
                return x.rearrange("p (h k) -> p h k", k=64)

            def b16(x):
                return x.w(lambda a: a.unsqueeze(2).to_broadcast([128, 16, 64]))
            k.tt("pool", o[:], o[:], o1[i][:], ALU.add)
            k.op("dve", "tensor_reduce", out=mean[:], in_=h3(o[:]), axis=AX.X, op=ALU.add)
            k.ts("dve", mean[:], mean[:], 1.0 / 64, ALU.mult)
            k.tt("dve", h3(o[:]), h3(o[:]), b16(mean[:]), ALU.subtract)
            k.tt("pool", sq[:], o[:], o[:], ALU.mult)
            k.op("dve", "tensor_reduce", out=var[:], in_=h3(sq[:]), axis=AX.X, op=ALU.add)
            k.ts("dve", var[:], var[:], 1.0 / 64, ALU.mult, 64e-5, ALU.add)
            k.act(var[:], var[:], AF.Sqrt)
            k.op("dve", "reciprocal", out=var[:], in_=var[:])
            k.tt("dve", h3(o[:]), h3(o[:]), b16(var[:]), ALU.mult)
            k.tt("pool", o[:], o[:], lnw[:], ALU.mult)
            k.tt("pool", o[:], o[:], lnb[:], ALU.add)
            k.tt("dve", h3(sq[:]), h3(vb[i][:]), b16(bs[i][:]), ALU.mult)
            k.tt("pool", o[:], o[:], sq[:], ALU.add)
            k.tt("dve", res[i][:], o[:], gb[i][:], ALU.mult)
            k.dma("sp", self.attn_d.key(tt)[tt * 128:(tt + 1) * 128, :], res[i][:])


Prog.rwkv_scan = rwkv_scan
```
